# Optimizing a Trainium2 kernel written in Bass

```python
import jax, jax.numpy as jnp
from jax import lax
import numpy as np

D_MODEL = 4096
BATCH = 4
SEQ = 2048
DEPTH = 4

N_MIXERS = 2
MEM_TOKENS = 256
MIX_WIDTH = D_MODEL
MEM_HEADS = 4
MEM_HEAD_DIM = MIX_WIDTH // 4 // MEM_HEADS
TOKEN_MIX_WIDTH = MIX_WIDTH - MEM_HEADS * MEM_HEAD_DIM
MLSTM_HEAD_V = 512
MLSTM_HEADS = TOKEN_MIX_WIDTH // MLSTM_HEAD_V
MLSTM_HEAD_QK = MLSTM_HEAD_V // 2
MLSTM_CHUNK = 64
GATE_SOFT_CAP = 15.0
SB_HEAD_DIM = 128
SB_HEADS = TOKEN_MIX_WIDTH // SB_HEAD_DIM
SB_BLOCK = 128
D_FF = 3 * D_MODEL // 2
RMS_EPS = 1e-6
N_A_LAYERS = (DEPTH + 1) // 2
N_B_LAYERS = DEPTH // 2

A_SPLITS = (MLSTM_HEADS * MLSTM_HEAD_QK, MLSTM_HEADS * MLSTM_HEAD_QK, TOKEN_MIX_WIDTH,
            TOKEN_MIX_WIDTH, MLSTM_HEADS, MLSTM_HEADS, MEM_HEADS * MEM_HEAD_DIM)
A_IN_COLS = sum(A_SPLITS)
B_SPLITS = (TOKEN_MIX_WIDTH, TOKEN_MIX_WIDTH, TOKEN_MIX_WIDTH, MEM_HEADS * MEM_HEAD_DIM)
B_IN_COLS = sum(B_SPLITS)

kernel_name = "hybrid_mlstm_stickbreaking_macaron_memory"


def rmsnorm(x, g):
    xf = x.astype(jnp.float32)
    y = xf * lax.rsqrt(jnp.mean(xf * xf, axis=-1, keepdims=True) + RMS_EPS)
    return (y * g.astype(jnp.float32)).astype(x.dtype)


def _split(t, sizes):
    idx = np.cumsum(sizes)[:-1].tolist()
    return jnp.split(t, idx, axis=-1)


def _heads(t, n_heads):
    b, s, _ = t.shape
    return t.reshape(b, s, n_heads, -1).transpose(0, 2, 1, 3)


def _merge_heads(t):
    b, h, s, d = t.shape
    return t.transpose(0, 2, 1, 3).reshape(b, s, h * d)


def swiglu(x, w_in, w_out):
    g, u = jnp.split(x @ w_in, 2, axis=-1)
    return (jax.nn.silu(g) * u) @ w_out


def softcap(t):
    return GATE_SOFT_CAP * jnp.tanh(t / GATE_SOFT_CAP)


def memory_attention(mq, mem_k, mem_v):
    b, s, _ = mq.shape
    q = mq.reshape(b, s, MEM_HEADS, MEM_HEAD_DIM)
    scores = jnp.einsum('bshd,bmhd->bhsm', q, mem_k).astype(jnp.float32) * (MEM_HEAD_DIM ** -0.5)
    p = jax.nn.softmax(scores, axis=-1).astype(mem_v.dtype)
    o = jnp.einsum('bhsm,bmhd->bshd', p, mem_v)
    return o.reshape(b, s, MEM_HEADS * MEM_HEAD_DIM)


def _mlstm_chunk_step(carry, inp):
    c_prev, n_prev, m_prev = carry
    q, k, v, ig, lf = inp
    L = q.shape[2]
    b = jnp.cumsum(lf, axis=-1)
    causal = jnp.tril(jnp.ones((L, L), dtype=bool))
    d_log = jnp.where(causal, b[..., :, None] - b[..., None, :] + ig[..., None, :], -jnp.inf)
    inter_log = b + m_prev[..., None]
    m_t = jnp.maximum(inter_log, d_log.max(axis=-1))
    d_w = jnp.exp(d_log - m_t[..., None])
    inter_w = jnp.exp(inter_log - m_t)
    s = jnp.einsum('bhjd,bhsd->bhjs', q, k) * d_w
    num = jnp.einsum('bhjs,bhsv->bhjv', s, v) + inter_w[..., None] * jnp.einsum('bhjd,bhvd->bhjv', q, c_prev)
    den = s.sum(axis=-1) + inter_w * jnp.einsum('bhjd,bhd->bhj', q, n_prev)
    h = num / jnp.maximum(jnp.abs(den), jnp.exp(-m_t))[..., None]
    b_last = b[..., -1]
    w_log = b_last[..., None] - b + ig
    m_new = jnp.maximum(b_last + m_prev, w_log.max(axis=-1))
    decay = jnp.exp(b_last + m_prev - m_new)
    w = jnp.exp(w_log - m_new[..., None])
    c_new = decay[..., None, None] * c_prev + jnp.einsum('bhs,bhsv,bhsd->bhvd', w, v, k)
    n_new = decay[..., None] * n_prev + jnp.einsum('bhs,bhsd->bhd', w, k)
    return (c_new, n_new, m_new), h


def mlstm_chunkwise(q, k, v, ig, lf):
    b, h, s, dk = q.shape
    dv = v.shape[-1]
    nc = s // MLSTM_CHUNK

    def chunks(t):
        return jnp.moveaxis(t.reshape(b, h, nc, MLSTM_CHUNK, *t.shape[3:]), 2, 0)

    init = (jnp.zeros((b, h, dv, dk), jnp.float32),
            jnp.zeros((b, h, dk), jnp.float32),
            jnp.zeros((b, h), jnp.float32))
    _, hs = lax.scan(_mlstm_chunk_step, init, (chunks(q), chunks(k), chunks(v), chunks(ig), chunks(lf)))
    return jnp.moveaxis(hs, 0, 2).reshape(b, h, s, dv)


def mlstm_mixer(xn, w_in, b_igate, b_fgate, head_gain, mem_k, mem_v):
    q, k, v, o, ig, fg, mq = _split(xn @ w_in, A_SPLITS)
    f32 = jnp.float32
    q = _heads(q, MLSTM_HEADS).astype(f32) * (MLSTM_HEAD_QK ** -0.5)
    k = _heads(k, MLSTM_HEADS).astype(f32)
    v = _heads(v, MLSTM_HEADS).astype(f32)
    ig = softcap((ig + b_igate).astype(f32)).transpose(0, 2, 1)
    lf = jax.nn.log_sigmoid(softcap((fg + b_fgate).astype(f32))).transpose(0, 2, 1)
    h = mlstm_chunkwise(q, k, v, ig, lf)
    h = h.transpose(0, 2, 1, 3)
    h = rmsnorm(h, head_gain.reshape(MLSTM_HEADS, MLSTM_HEAD_V))
    b_, s_ = h.shape[:2]
    h = (jax.nn.sigmoid(o.astype(f32)) * h.reshape(b_, s_, TOKEN_MIX_WIDTH)).astype(xn.dtype)
    return jnp.concatenate([h, memory_attention(mq, mem_k, mem_v)], axis=-1)


def stick_breaking_attention(q, k, v):
    _, _, s_len, d = q.shape
    scale = d ** -0.5
    outs = []
    for blk in range(s_len // SB_BLOCK):
        q0 = blk * SB_BLOCK
        q1 = q0 + SB_BLOCK
        z = jnp.einsum('bhtd,bhsd->bhts', q[:, :, q0:q1], k[:, :, :q1]).astype(jnp.float32) * scale
        t_pos = q0 + jnp.arange(SB_BLOCK)
        s_pos = jnp.arange(q1)
        causal = s_pos[None, :] < t_pos[:, None]
        log_keep = jnp.where(causal, jax.nn.log_sigmoid(-z), 0.0)
        later = lax.cumsum(log_keep, axis=3, reverse=True) - log_keep
        log_a = jax.nn.log_sigmoid(z) + later
        a = jnp.where(causal, jnp.exp(log_a), 0.0).astype(v.dtype)
        outs.append(jnp.einsum('bhts,bhsd->bhtd', a, v[:, :, :q1]))
    return jnp.concatenate(outs, axis=2)


def stick_breaking_mixer(xn, w_in, mem_k, mem_v):
    q, k, v, mq = _split(xn @ w_in, B_SPLITS)
    o = stick_breaking_attention(_heads(q, SB_HEADS), _heads(k, SB_HEADS), _heads(v, SB_HEADS))
    return jnp.concatenate([_merge_heads(o), memory_attention(mq, mem_k, mem_v)], axis=-1)


def setup_inputs(seed: int = 0) -> dict:
    key = jax.random.key(seed)
    ks = jax.random.split(key, 20)

    def nrm(k, shape, scale):
        return jax.random.normal(k, shape, jnp.float32) * scale

    return {
        "x": nrm(ks[0], (BATCH, SEQ, D_MODEL), 1.0),
        "mem": nrm(ks[1], (BATCH, MEM_TOKENS, D_MODEL), 1.0),
        "norm_ffn1": 1.0 + nrm(ks[2], (DEPTH, D_MODEL), 0.02),
        "ffn1_w_in": nrm(ks[3], (DEPTH, D_MODEL, 2 * D_FF), D_MODEL ** -0.5),
        "ffn1_w_out": nrm(ks[4], (DEPTH, D_FF, D_MODEL), D_FF ** -0.5),
        "norm_mix": 1.0 + nrm(ks[5], (DEPTH, D_MODEL), 0.02),
        "a_w_in": nrm(ks[6], (N_A_LAYERS, D_MODEL, A_IN_COLS), D_MODEL ** -0.5),
        "a_b_igate": nrm(ks[7], (N_A_LAYERS, MLSTM_HEADS), 0.1),
        "a_b_fgate": 3.0 + nrm(ks[8], (N_A_LAYERS, MLSTM_HEADS), 0.5),
        "a_head_gain": 1.0 + nrm(ks[9], (N_A_LAYERS, TOKEN_MIX_WIDTH), 0.02),
        "b_w_in": nrm(ks[10], (N_B_LAYERS, D_MODEL, B_IN_COLS), D_MODEL ** -0.5),
        "mem_norm": 1.0 + nrm(ks[11], (D_MODEL,), 0.02),
        "w_mem_kv": nrm(ks[12], (D_MODEL, 2 * MEM_HEADS * MEM_HEAD_DIM), D_MODEL ** -0.5),
        "w_out": nrm(ks[13], (DEPTH, MIX_WIDTH, D_MODEL), MIX_WIDTH ** -0.5),
        "norm_ffn2": 1.0 + nrm(ks[14], (DEPTH, D_MODEL), 0.02),
        "ffn2_w_in": nrm(ks[15], (DEPTH, D_MODEL, 2 * D_FF), D_MODEL ** -0.5),
        "ffn2_w_out": nrm(ks[16], (DEPTH, D_FF, D_MODEL), D_FF ** -0.5),
        "norm_final": 1.0 + nrm(ks[17], (D_MODEL,), 0.02),
    }


def reference(x, mem, norm_ffn1, ffn1_w_in, ffn1_w_out, norm_mix, a_w_in, a_b_igate, a_b_fgate,
              a_head_gain, b_w_in, mem_norm, w_mem_kv, w_out, norm_ffn2, ffn2_w_in, ffn2_w_out,
              norm_final):
    b, m_len, _ = mem.shape
    mem_k, mem_v = jnp.split(rmsnorm(mem, mem_norm) @ w_mem_kv, 2, axis=-1)
    mem_k = mem_k.reshape(b, m_len, MEM_HEADS, MEM_HEAD_DIM)
    mem_v = mem_v.reshape(b, m_len, MEM_HEADS, MEM_HEAD_DIM)

    h = x
    for i in range(DEPTH):
        h = h + 0.5 * swiglu(rmsnorm(h, norm_ffn1[i]), ffn1_w_in[i], ffn1_w_out[i])
        xn = rmsnorm(h, norm_mix[i])
        j = i // N_MIXERS
        if i % N_MIXERS == 0:
            mixed = mlstm_mixer(xn, a_w_in[j], a_b_igate[j], a_b_fgate[j], a_head_gain[j], mem_k, mem_v)
        else:
            mixed = stick_breaking_mixer(xn, b_w_in[j], mem_k, mem_v)
        h = h + mixed @ w_out[i]
        h = h + 0.5 * swiglu(rmsnorm(h, norm_ffn2[i]), ffn2_w_in[i], ffn2_w_out[i])
    return rmsnorm(h, norm_final)
```

```python
import numpy as np
import ml_dtypes
from contextlib import ExitStack
import concourse.bass as bass
import concourse.mybir as mybir
from concourse.bass_utils import run_bass_kernel_spmd

F32 = mybir.dt.float32
BF16 = mybir.dt.bfloat16
AF = mybir.ActivationFunctionType
ALU = mybir.AluOpType

D_MODEL = 4096
BATCH = 4
SEQ = 2048
DEPTH = 4
MEM_TOKENS = 256
D_FF = 6144
RMS_EPS = 1e-6
NCORES = 8
NT = 1024
KC = D_MODEL // 128


class Buf:
    __slots__ = ("name", "last_write", "reads")

    def __init__(self, name):
        self.name = name
        self.last_write = None
        self.reads = []


class Prog:
    ENGS = ("pe", "act", "dve", "pool", "sp")

    def __init__(self):
        self.nc = bass.Bass("TRN2", target_bir_lowering=False)
        self.es = ExitStack()
        self.ops = {e: [] for e in self.ENGS}
        self.count = {e: 0 for e in self.ENGS}
        self.sem = {}
        for e in ("pe", "act", "dve", "pool"):
            self.sem[e] = self.es.enter_context(self.nc.semaphore("sem_" + e))
        self.seen = {}
        self.dma_sems = []
        self.free_dma_sems = []
        self.live_dma_sems = []
        self.nbuf = 0
        self.tensors = 0

    def sbuf(self, shape, dtype, name=None, stack=None):
        self.tensors += 1
        name = name or f"sb{self.tensors}"
        t = (stack or self.es).enter_context(self.nc.sbuf_tensor(name, list(shape), dtype))
        return t

    def psum(self, shape, dtype, name=None, stack=None):
        self.tensors += 1
        name = name or f"ps{self.tensors}"
        t = (stack or self.es).enter_context(self.nc.psum_tensor(name, list(shape), dtype))
        return t

    def dram(self, name, shape, dtype, kind=None):
        if kind is None:
            return self.nc.dram_tensor(name, list(shape), dtype)
        return self.nc.dram_tensor(name, list(shape), dtype, kind=kind)

    def buf(self, name=None):
        self.nbuf += 1
        return Buf(name or f"b{self.nbuf}")

    def new_dma_sem(self):
        if self.free_dma_sems:
            ent = self.free_dma_sems.pop()
        else:
            s = self.es.enter_context(self.nc.semaphore(f"dsem{len(self.dma_sems)}"))
            ent = [s, 0]
            self.dma_sems.append(ent)
        self.live_dma_sems.append(ent)
        return ent

    def _wait(self, eng, tok):
        if tok is None:
            return
        kind, key, val = tok
        if kind == "eng":
            if key == eng and eng == "pe":
                return
            sem = self.sem[key]
            skey = (eng, "e" + key)
        else:
            sem = key[0]
            skey = (eng, id(key))
        if self.seen.get(skey, 0) >= val:
            return
        self.seen[skey] = val
        self.ops[eng].append(lambda e, sem=sem, val=val: e.wait_ge(sem, val))

    def _deps(self, eng, reads, writes, dsem=None):
        toks = []
        for b in reads:
            toks.append(b.last_write)
        for b in writes:
            lw = b.last_write
            if not (dsem is not None and lw is not None and lw[0] == "dma" and lw[1] is dsem):
                toks.append(lw)
            toks.extend(b.reads)
        for t in toks:
            self._wait(eng, t)

    def _commit(self, tok, reads, writes):
        for b in reads:
            b.reads.append(tok)
        for b in writes:
            b.last_write = tok
            b.reads = []

    def op(self, eng, fn, reads=(), writes=(), signal=True):
        self._deps(eng, reads, writes)
        if signal:
            self.count[eng] += 1
            n = self.count[eng]
            sem = self.sem[eng]
            self.ops[eng].append(lambda e, fn=fn, sem=sem: fn(e).then_inc(sem, 1))
        else:
            n = self.count[eng] + 1
            self.ops[eng].append(lambda e, fn=fn: fn(e))
        self._commit(("eng", eng, n), reads, writes)

    def dma(self, eng, out, in_, dsem, reads=(), writes=(), **kw):
        self._deps(eng, reads, writes, dsem)
        dsem[1] += 16
        sem = dsem[0]
        self.ops[eng].append(
            lambda e, out=out, in_=in_, sem=sem, kw=kw: e.dma_start(out=out, in_=in_, **kw).then_inc(sem, 16))
        self._commit(("dma", dsem, dsem[1]), reads, writes)

    def barrier(self):
        for eng in self.ENGS:
            for other in ("pe", "act", "dve", "pool"):
                if other != eng and self.count[other] > 0:
                    self._wait(eng, ("eng", other, self.count[other]))
            for ent in self.dma_sems:
                if ent[1] > 0:
                    self._wait(eng, ("dma", ent, ent[1]))
        self.free_dma_sems.extend(self.live_dma_sems)
        self.live_dma_sems = []

    def finish(self):
        self.barrier()
        nc = self.nc
        with nc.Block() as block:
            @block.tensor
            def _(e):
                for f in self.ops["pe"]:
                    f(e)

            @block.scalar
            def _(e):
                for f in self.ops["act"]:
                    f(e)

            @block.vector
            def _(e):
                for f in self.ops["dve"]:
                    f(e)

            @block.gpsimd
            def _(e):
                for f in self.ops["pool"]:
                    f(e)

            @block.sync
            def _(e):
                for f in self.ops["sp"]:
                    f(e)
        self.es.close()
        return nc


KG = 8
CONST_NAMES_IDX_ONES = 5


class Ctx:
    def __init__(self, p, consts_dram):
        self.p = p
        nc = p.nc
        self.ps = [p.psum([128, 512], F32, name=f"psb{i}") for i in range(8)]
        self.psb = [p.buf(f"psb{i}") for i in range(8)]
        self.ones_bf = p.sbuf([128, 128], BF16, name="ones_bf")
        self.ones_bf_b = p.buf("ones_bf")
        self.csem = p.new_dma_sem()
        p.dma("pool", self.ones_bf[:], consts_dram[:, CONST_NAMES_IDX_ONES, :], self.csem, writes=[self.ones_bf_b])


def tile_w(W, ncols, col_blocks):
    K = W.shape[0]
    kcn = K // 128
    kgn = kcn // KG
    MT = len(col_blocks)
    out = np.zeros((MT, kgn, 128, KG, ncols), np.float32)
    Wr = W.reshape(kgn, KG, 128, W.shape[1])
    for m, blocks in enumerate(col_blocks):
        c0 = 0
        for (s, w) in blocks:
            out[m, :, :, :, c0:c0 + w] = Wr[:, :, :, s:s + w].transpose(0, 2, 1, 3)
            c0 += w
    return out


def emit_rmsnorm(p, cx, hT_dram, g_sb, g_buf, gcol, xnT, xnT_buf, nt, kcn=KC):
    with ExitStack() as st:
        hs = [p.sbuf([128, nt], F32, stack=st) for _ in range(2)]
        hsb = [p.buf() for _ in range(2)]
        hsem = [p.new_dma_sem() for _ in range(2)]
        sq = [p.sbuf([128, nt], BF16, stack=st) for _ in range(2)]
        sqb = [p.buf() for _ in range(2)]
        rstd = p.sbuf([128, nt], F32, stack=st)
        rstdb = p.buf()
        tw = min(512, nt)
        ntt = nt // tw
        for kc in range(kcn):
            s = kc % 2
            p.dma("sp", hs[s][:], hT_dram[kc * 128:(kc + 1) * 128, :], hsem[s], writes=[hsb[s]])
            p.op("act", lambda e, s=s: e.activation(out=sq[s][:], in_=hs[s][:], func=AF.Square),
                 reads=[hsb[s]], writes=[sqb[s]])
            for tt in range(ntt):
                last = kc == kcn - 1
                p.op("pe", lambda e, s=s, tt=tt, kc=kc, last=last: e.matmul(
                    cx.ps[tt][:, 0:tw], lhsT=cx.ones_bf[:], rhs=sq[s][:, tt * tw:(tt + 1) * tw],
                    start=(kc == 0), stop=last),
                    reads=[sqb[s], cx.ones_bf_b], writes=[cx.psb[tt]], signal=(last or tt == ntt - 1))
        for tt in range(ntt):
            p.op("act", lambda e, tt=tt: e.activation(
                out=rstd[:, tt * tw:(tt + 1) * tw], in_=cx.ps[tt][:, 0:tw], func=AF.Sqrt,
                bias=RMS_EPS, scale=1.0 / (kcn * 128)),
                reads=[cx.psb[tt]], writes=[rstdb])
        p.op("dve", lambda e: e.reciprocal(out=rstd[:], in_=rstd[:]), reads=[rstdb], writes=[rstdb])
        for kc in range(kcn):
            s = kc % 2
            p.dma("sp", hs[s][:], hT_dram[kc * 128:(kc + 1) * 128, :], hsem[s], writes=[hsb[s]])
            p.op("dve", lambda e, s=s, kc=kc: e.scalar_tensor_tensor(
                out=xnT[:, kc, :], in0=hs[s][:], scalar=g_sb[:, gcol + kc:gcol + kc + 1], in1=rstd[:],
                op0=ALU.mult, op1=ALU.mult),
                reads=[hsb[s], rstdb, g_buf], writes=[xnT_buf])
        p.barrier()


class WStream:
    def __init__(self, p, nslots, ncols, stack):
        self.p = p
        self.ncols = ncols
        self.nslots = nslots
        self.slots = [p.sbuf([128, KG, ncols], BF16, stack=stack) for _ in range(nslots)]
        self.bufs = [p.buf() for _ in range(nslots)]
        self.sems = [p.new_dma_sem() for _ in range(nslots)]
        self.i = 0

    def load(self, src_ap):
        s = self.i % self.nslots
        self.i += 1
        self.p.dma("pool", self.slots[s][:], src_ap, self.sems[s], writes=[self.bufs[s]])
        return self.slots[s], self.bufs[s]


def emit_lin_fm(p, cx, ws, xT, xT_buf, kcn, w_dram, mt, nblk, nt, evac, ps_base=0, nsets=2):
    tw = min(512, nt)
    ntt = nt // tw
    per_set = nblk * ntt
    kgn = kcn // KG
    for m in range(mt):
        st_ = m % nsets
        banks = [[ps_base + st_ * per_set + b * ntt + tt for tt in range(ntt)] for b in range(nblk)]
        for kg in range(kgn):
            wt, wb = ws.load(w_dram[m, kg])
            for ki in range(KG):
                kc = kg * KG + ki
                for b in range(nblk):
                    for tt in range(ntt):
                        last = kc == kcn - 1
                        bk = banks[b][tt]
                        p.op("pe", lambda e, wt=wt, ki=ki, b=b, tt=tt, kc=kc, bk=bk, last=last: e.matmul(
                            cx.ps[bk][:, 0:tw], lhsT=wt[:, ki, b * 128:(b + 1) * 128],
                            rhs=xT[:, kc, tt * tw:(tt + 1) * tw], start=(kc == 0), stop=last),
                            reads=[wb, xT_buf], writes=[cx.psb[bk]],
                            signal=(last or (ki == KG - 1 and b == nblk - 1 and tt == ntt - 1)))
        evac(m, banks)


def emit_lin_tm(p, cx, ws, xT, xT_buf, kcn, w_dram, ct_n, ncols, nt, evac, ps_base=0):
    kgn = kcn // KG
    ntg = nt // 128
    i = 0
    for ct in range(ct_n):
        tiles = [ws.load(w_dram[ct, kg]) for kg in range(kgn)]
        for tg in range(ntg):
            bk = ps_base + (i % 2)
            i += 1
            for kc in range(kcn):
                wt, wb = tiles[kc // KG]
                last = kc == kcn - 1
                p.op("pe", lambda e, wt=wt, kc=kc, tg=tg, bk=bk, last=last: e.matmul(
                    cx.ps[bk][:, 0:ncols], lhsT=xT[:, kc, tg * 128:(tg + 1) * 128],
                    rhs=wt[:, kc % KG, :], start=(kc == 0), stop=last),
                    reads=[wb, xT_buf], writes=[cx.psb[bk]], signal=(last or kc % KG == KG - 1))
            evac(ct, tg, bk)


def emit_ffn(p, cx, hT_in, hT_out, g_sb, g_buf, gcol, w_in_dram, w_out_dram, nt, kcn=KC, fcn=D_FF // 128,
             h_in_buf=None, h_out_buf=None):
    h_in_buf = h_in_buf or p.buf()
    h_out_buf = h_out_buf or p.buf()
    with ExitStack() as st:
        xnT = p.sbuf([128, kcn, nt], BF16, stack=st)
        xnT_b = p.buf()
        emit_rmsnorm(p, cx, hT_in, g_sb, g_buf, gcol, xnT, xnT_b, nt, kcn)
        aT = p.sbuf([128, fcn, nt], BF16, stack=st)
        aT_b = p.buf()
        ntt = nt // 512
        with ExitStack() as st2:
            ws = WStream(p, 3, 256, st2)
            sg = [p.sbuf([128, 512], F32, stack=st2) for _ in range(2)]
            sgb = [p.buf() for _ in range(2)]
            cnt = [0]

            def evac_in(j, banks):
                for tt in range(ntt):
                    i = cnt[0] % 2
                    cnt[0] += 1
                    bg, bu = banks[0][tt], banks[1][tt]
                    p.op("act", lambda e, i=i, bg=bg: e.activation(out=sg[i][:], in_=cx.ps[bg][:], func=AF.Silu),
                         reads=[cx.psb[bg]], writes=[sgb[i]])
                    p.op("dve", lambda e, i=i, bu=bu, j=j, tt=tt: e.tensor_tensor(
                        out=aT[:, j, tt * 512:(tt + 1) * 512], in0=sg[i][:], in1=cx.ps[bu][:], op=ALU.mult),
                        reads=[sgb[i], cx.psb[bu]], writes=[aT_b])
            emit_lin_fm(p, cx, ws, xnT, xnT_b, kcn, w_in_dram, fcn, 2, nt, evac_in)
            p.barrier()
        with ExitStack() as st3:
            ws = WStream(p, 4, 128, st3)
            hs = [p.sbuf([128, nt], F32, stack=st3) for _ in range(2)]
            hsb = [p.buf() for _ in range(2)]
            hsem = [p.new_dma_sem() for _ in range(2)]
            osem = p.new_dma_sem()

            def evac_out(m, banks):
                s = m % 2
                p.dma("pool", hs[s][:], hT_in[m * 128:(m + 1) * 128, :], hsem[s],
                      reads=[h_in_buf], writes=[hsb[s]])
                for tt in range(ntt):
                    bk = banks[0][tt]
                    p.op("dve", lambda e, s=s, bk=bk, tt=tt: e.scalar_tensor_tensor(
                        out=hs[s][:, tt * 512:(tt + 1) * 512], in0=cx.ps[bk][:], scalar=0.5,
                        in1=hs[s][:, tt * 512:(tt + 1) * 512], op0=ALU.mult, op1=ALU.add),
                        reads=[cx.psb[bk], hsb[s]], writes=[hsb[s]])
                p.dma("sp", hT_out[m * 128:(m + 1) * 128, :], hs[s][:], osem, reads=[hsb[s]], writes=[h_out_buf])
            emit_lin_fm(p, cx, ws, aT, aT_b, fcn, w_out_dram, kcn, 1, nt, evac_out)
            p.barrier()
    return h_out_buf


def emit_inproj(p, cx, hT_in, g_sb, g_buf, gcol, w_fm_dram, n_fm, fmT_out, w_tm_dram, n_tm, tm_out, nt,
                kcn=KC):
    fm_b, tm_b = p.buf(), p.buf()
    with ExitStack() as st:
        xnT = p.sbuf([128, kcn, nt], BF16, stack=st)
        xnT_b = p.buf()
        emit_rmsnorm(p, cx, hT_in, g_sb, g_buf, gcol, xnT, xnT_b, nt, kcn)
        ntt = nt // 512
        with ExitStack() as st2:
            ws = WStream(p, 4, 128, st2)
            og = [p.sbuf([128, nt], BF16, stack=st2) for _ in range(2)]
            ogb = [p.buf() for _ in range(2)]
            osem = p.new_dma_sem()

            def evac_fm(m, banks):
                s = m % 2
                for tt in range(ntt):
                    bk = banks[0][tt]
                    eng = "act" if tt % 2 == 0 else "dve"
                    if eng == "act":
                        p.op("act", lambda e, s=s, bk=bk, tt=tt: e.activation(
                            out=og[s][:, tt * 512:(tt + 1) * 512], in_=cx.ps[bk][:], func=AF.Copy),
                            reads=[cx.psb[bk]], writes=[ogb[s]])
                    else:
                        p.op("dve", lambda e, s=s, bk=bk, tt=tt: e.tensor_copy(
                            out=og[s][:, tt * 512:(tt + 1) * 512], in_=cx.ps[bk][:]),
                            reads=[cx.psb[bk]], writes=[ogb[s]])
                p.dma("sp", fmT_out[m * 128:(m + 1) * 128, :], og[s][:], osem, reads=[ogb[s]], writes=[fm_b])
            emit_lin_fm(p, cx, ws, xnT, xnT_b, kcn, w_fm_dram, n_fm, 1, nt, evac_fm)
            p.barrier()
        with ExitStack() as st3:
            kgn = kcn // KG
            ws = WStream(p, 2 * kgn, 512, st3)
            ot = [p.sbuf([128, 512], F32, stack=st3) for _ in range(3)]
            otb = [p.buf() for _ in range(3)]
            osem = p.new_dma_sem()
            cnt = [0]

            def evac_tm(ct, tg, bk):
                s = cnt[0] % 3
                eng = "act" if cnt[0] % 2 == 0 else "dve"
                cnt[0] += 1
                if eng == "act":
                    p.op("act", lambda e, s=s, bk=bk: e.activation(out=ot[s][:], in_=cx.ps[bk][:], func=AF.Copy),
                         reads=[cx.psb[bk]], writes=[otb[s]])
                else:
                    p.op("dve", lambda e, s=s, bk=bk: e.tensor_copy(out=ot[s][:], in_=cx.ps[bk][:]),
                         reads=[cx.psb[bk]], writes=[otb[s]])
                p.dma("sp", tm_out[tg * 128:(tg + 1) * 128, ct * 512:(ct + 1) * 512], ot[s][:], osem,
                      reads=[otb[s]], writes=[tm_b])
            emit_lin_tm(p, cx, ws, xnT, xnT_b, kcn, w_tm_dram, n_tm, 512, nt, evac_tm)
            p.barrier()
    return fm_b, tm_b


def emit_mixout(p, cx, hT_in, hT_out, mixT_dram, w_dram, nt, kcn=KC):
    with ExitStack() as st:
        mT = p.sbuf([128, kcn, nt], BF16, stack=st)
        mT_b = p.buf()
        msem = p.new_dma_sem()
        for kc in range(kcn):
            p.dma("sp", mT[:, kc, :], mixT_dram[kc * 128:(kc + 1) * 128, :], msem, writes=[mT_b])
        ws = WStream(p, 4, 128, st)
        hs = [p.sbuf([128, nt], F32, stack=st) for _ in range(2)]
        hsb = [p.buf() for _ in range(2)]
        hsem = [p.new_dma_sem() for _ in range(2)]
        osem = p.new_dma_sem()
        ob = p.buf()
        ntt = nt // 512

        def evac(m, banks):
            s = m % 2
            p.dma("pool", hs[s][:], hT_in[m * 128:(m + 1) * 128, :], hsem[s], writes=[hsb[s]])
            for tt in range(ntt):
                bk = banks[0][tt]
                p.op("dve", lambda e, s=s, bk=bk, tt=tt: e.tensor_tensor(
                    out=hs[s][:, tt * 512:(tt + 1) * 512], in0=cx.ps[bk][:],
                    in1=hs[s][:, tt * 512:(tt + 1) * 512], op=ALU.add),
                    reads=[cx.psb[bk], hsb[s]], writes=[hsb[s]])
            p.dma("sp", hT_out[m * 128:(m + 1) * 128, :], hs[s][:], osem, reads=[hsb[s]], writes=[ob])
        emit_lin_fm(p, cx, ws, mT, mT_b, kcn, w_dram, kcn, 1, nt, evac)
        p.barrier()


def emit_final_norm(p, cx, hT_in, g_sb, g_buf, gcol, outT, nt, kcn=KC):
    with ExitStack() as st:
        hs = [p.sbuf([128, nt], F32, stack=st) for _ in range(2)]
        hsb = [p.buf() for _ in range(2)]
        hsem = [p.new_dma_sem() for _ in range(2)]
        sq = [p.sbuf([128, nt], F32, stack=st) for _ in range(2)]
        sqb = [p.buf() for _ in range(2)]
        rstd = p.sbuf([128, nt], F32, stack=st)
        rstdb = p.buf()
        ones32 = p.sbuf([128, 128], F32, stack=st)
        o32b = p.buf()
        p.op("pool", lambda e: e.memset(ones32[:], 1.0), writes=[o32b])
        ntt = nt // 512
        for kc in range(kcn):
            s = kc % 2
            p.dma("sp", hs[s][:], hT_in[kc * 128:(kc + 1) * 128, :], hsem[s], writes=[hsb[s]])
            p.op("act", lambda e, s=s: e.activation(out=sq[s][:], in_=hs[s][:], func=AF.Square),
                 reads=[hsb[s]], writes=[sqb[s]])
            for tt in range(ntt):
                last = kc == kcn - 1
                p.op("pe", lambda e, s=s, tt=tt, kc=kc, last=last: e.matmul(
                    cx.ps[tt][:], lhsT=ones32[:], rhs=sq[s][:, tt * 512:(tt + 1) * 512],
                    start=(kc == 0), stop=last),
                    reads=[sqb[s], o32b], writes=[cx.psb[tt]], signal=(last or tt == ntt - 1))
        for tt in range(ntt):
            p.op("act", lambda e, tt=tt: e.activation(
                out=rstd[:, tt * 512:(tt + 1) * 512], in_=cx.ps[tt][:], func=AF.Sqrt,
                bias=RMS_EPS, scale=1.0 / (kcn * 128)), reads=[cx.psb[tt]], writes=[rstdb])
        p.op("dve", lambda e: e.reciprocal(out=rstd[:], in_=rstd[:]), reads=[rstdb], writes=[rstdb])
        osem = p.new_dma_sem()
        ob = p.buf()
        for kc in range(kcn):
            s = kc % 2
            p.dma("sp", hs[s][:], hT_in[kc * 128:(kc + 1) * 128, :], hsem[s], writes=[hsb[s]])
            p.op("dve", lambda e, s=s, kc=kc: e.scalar_tensor_tensor(
                out=hs[s][:], in0=hs[s][:], scalar=g_sb[:, gcol + kc:gcol + kc + 1], in1=rstd[:],
                op0=ALU.mult, op1=ALU.mult), reads=[hsb[s], rstdb, g_buf], writes=[hsb[s]])
            p.dma("sp", outT[kc * 128:(kc + 1) * 128, :], hs[s][:], osem, reads=[hsb[s]], writes=[ob])
        p.barrier()


NEG = -30000.0
LN16 = float(np.log(16.0))
CONST_NAMES = ["U", "Tri", "negU", "maskS", "negS", "ones", "ident"]


def host_consts():
    i = np.arange(128)[:, None]
    j = np.arange(128)[None, :]
    c = {
        "U": (i <= j),
        "Tri": (i > j),
        "negU": np.where(i <= j, 0.0, NEG),
        "maskS": (j > i),
        "negS": np.where(j > i, 0.0, NEG),
        "ones": np.ones((128, 128)),
        "ident": (i == j),
    }
    return np.ascontiguousarray(
        np.stack([c[n].astype(np.float32) for n in CONST_NAMES], axis=1))


class HConsts:
    def __init__(self, p, cdram):
        n = len(CONST_NAMES)
        self.f32 = p.sbuf([128, n, 128], F32, name="c32")
        self.bf = p.sbuf([128, n, 128], BF16, name="cbf")
        self.b = p.buf("consts")
        sem = p.new_dma_sem()
        p.dma("sp", self.f32[:], cdram[:, :, :], sem, writes=[self.b])
        p.dma("pool", self.bf[:], cdram[:, :, :], sem, writes=[self.b])

    def c32(self, name):
        return self.f32[:, CONST_NAMES.index(name), :]

    def cbf(self, name):
        return self.bf[:, CONST_NAMES.index(name), :]


def emit_memkv(p, cx, memT_dram, g_sb, g_buf, w_k_dram, w_v_dram, mkT, mkT_b, mv, mv_b, nmh=2, gcol=0):
    with ExitStack() as st:
        mnT = p.sbuf([128, KC, MEM_TOKENS], BF16, stack=st)
        mnT_b = p.buf()
        emit_rmsnorm(p, cx, memT_dram, g_sb, g_buf, gcol, mnT, mnT_b, MEM_TOKENS)
        ws = WStream(p, 4, 128, st)

        def evac_k(m, banks):
            bk = banks[0][0]
            p.op("act", lambda e, m=m, bk=bk: e.activation(out=mkT[:, m, :], in_=cx.ps[bk][:, 0:MEM_TOKENS],
                                                          func=AF.Copy),
                 reads=[cx.psb[bk]], writes=[mkT_b])
        emit_lin_fm(p, cx, ws, mnT, mnT_b, KC, w_k_dram, 2 * nmh, 1, MEM_TOKENS, evac_k)
        ws2 = WStream(p, 2 * (KC // KG), 512, st)

        def evac_v(ct, tg, bk):
            p.op("act", lambda e, ct=ct, tg=tg, bk=bk: e.activation(
                out=mv[:, tg, ct * 512:(ct + 1) * 512], in_=cx.ps[bk][:], func=AF.Copy),
                 reads=[cx.psb[bk]], writes=[mv_b])
        emit_lin_tm(p, cx, ws2, mnT, mnT_b, KC, w_v_dram, nmh // 2, 512, MEM_TOKENS, evac_v)
        p.barrier()


def emit_memattn(p, cx, hc, mqT_dram, mkT, mkT_b, mv, mv_b, mixT_out, row0, mix_b, nmh=2):
    with ExitStack() as st:
        mq = p.sbuf([128, 2 * nmh, SEQ], BF16, stack=st)
        mq_b = p.buf()
        sem = p.new_dma_sem()
        for i in range(2 * nmh):
            p.dma("sp", mq[:, i, :], mqT_dram[i * 128:(i + 1) * 128, :], sem, writes=[mq_b])
        pT = [p.sbuf([128, 2, 512], BF16, stack=st) for _ in range(2)]
        pTb = [p.buf() for _ in range(2)]
        rd = [p.sbuf([128, 512], F32, stack=st) for _ in range(2)]
        rdb = [p.buf() for _ in range(2)]
        oT = p.sbuf([128, 2 * nmh, SEQ], BF16, stack=st)
        oT_b = p.buf()
        it = 0
        for mh in range(nmh):
            for tq in range(SEQ // 512):
                s = it % 2
                it += 1
                tok = slice(tq * 512, (tq + 1) * 512)
                for mc in range(2):
                    for dc in range(2):
                        p.op("pe", lambda e, mc=mc, dc=dc, mh=mh, tok=tok: e.matmul(
                            cx.ps[mc][:], lhsT=mkT[:, mh * 2 + dc, mc * 128:(mc + 1) * 128],
                            rhs=mq[:, mh * 2 + dc, tok], start=(dc == 0), stop=(dc == 1)),
                            reads=[mkT_b, mq_b], writes=[cx.psb[mc]], signal=(dc == 1))
                    p.op("act", lambda e, mc=mc, s=s: e.activation(
                        out=pT[s][:, mc, :], in_=cx.ps[mc][:], func=AF.Exp, scale=1.0 / 16.0),
                        reads=[cx.psb[mc]], writes=[pTb[s]])
                for mc in range(2):
                    p.op("pe", lambda e, mc=mc, s=s: e.matmul(
                        cx.ps[2][:], lhsT=hc.cbf("ones"), rhs=pT[s][:, mc, :], start=(mc == 0), stop=(mc == 1)),
                        reads=[hc.b, pTb[s]], writes=[cx.psb[2]], signal=(mc == 1))
                p.op("dve", lambda e, s=s: e.reciprocal(out=rd[s][:], in_=cx.ps[2][:]),
                     reads=[cx.psb[2]], writes=[rdb[s]])
                for dvc in range(2):
                    bk = 3 + dvc
                    for mc in range(2):
                        p.op("pe", lambda e, mc=mc, dvc=dvc, mh=mh, s=s, bk=bk: e.matmul(
                            cx.ps[bk][:], lhsT=mv[:, mc, mh * 256 + dvc * 128:mh * 256 + (dvc + 1) * 128],
                            rhs=pT[s][:, mc, :], start=(mc == 0), stop=(mc == 1)),
                            reads=[mv_b, pTb[s]], writes=[cx.psb[bk]], signal=(mc == 1))
                    p.op("dve", lambda e, bk=bk, s=s, mh=mh, dvc=dvc, tok=tok: e.tensor_tensor(
                        out=oT[:, mh * 2 + dvc, tok], in0=cx.ps[bk][:], in1=rd[s][:], op=ALU.mult),
                        reads=[cx.psb[bk], rdb[s]], writes=[oT_b])
        osem = p.new_dma_sem()
        for i in range(2 * nmh):
            p.dma("sp", mixT_out[row0 + i * 128:row0 + (i + 1) * 128, :], oT[:, i, :], osem,
                  reads=[oT_b], writes=[mix_b])
        p.barrier()


def emit_mlstm(p, cx, hc, d, mixT_out, mix_b, nheads=3):
    NCH = SEQ // 128
    NI = nheads * NCH
    with ExitStack() as st:
        G = p.sbuf([128, NCH, 2 * nheads], F32, stack=st)
        gb = p.sbuf([128, 2 * nheads], F32, stack=st)
        gb15 = p.sbuf([128, 2 * nheads], F32, stack=st)
        gain = p.sbuf([128, nheads * 512], F32, stack=st)
        setup_b = p.buf()
        sem0 = p.new_dma_sem()
        p.dma("sp", G[:], d["gates"].rearrange("(c p) j -> p c j", p=128), sem0, writes=[setup_b])
        p.dma("sp", gb[:], d["gb"][:, :], sem0, writes=[setup_b])
        p.dma("sp", gain[:], d["gain"][:, :], sem0, writes=[setup_b])
        T1 = p.sbuf([128, NI], F32, stack=st)
        T2 = p.sbuf([128, NI], F32, stack=st)
        IG = p.sbuf([128, NI], F32, stack=st)
        LF = p.sbuf([128, NI], F32, stack=st)
        WB = p.sbuf([128, NI], F32, stack=st)
        DB = p.sbuf([128, NI], F32, stack=st)
        gt_b = p.buf()
        p.op("dve", lambda e: e.tensor_scalar_mul(out=gb15[:], in0=gb[:], scalar1=1.0 / 15.0),
             reads=[setup_b], writes=[gt_b])
        for h in range(nheads):
            hs_ = slice(h * NCH, (h + 1) * NCH)
            p.op("act", lambda e, h=h, hs_=hs_: e.activation(
                out=T1[:, hs_], in_=G[:, :, h], func=AF.Tanh, bias=gb15[:, h:h + 1], scale=1.0 / 15.0),
                reads=[setup_b, gt_b], writes=[gt_b])
            p.op("act", lambda e, h=h, hs_=hs_: e.activation(
                out=T2[:, hs_], in_=G[:, :, nheads + h], func=AF.Tanh,
                bias=gb15[:, nheads + h:nheads + h + 1], scale=1.0 / 15.0),
                reads=[setup_b, gt_b], writes=[gt_b])
        p.op("dve", lambda e: e.tensor_scalar_mul(out=IG[:], in0=T1[:], scalar1=15.0), reads=[gt_b], writes=[gt_b])
        p.op("act", lambda e: e.activation(out=T2[:], in_=T2[:], func=AF.Exp, scale=-15.0),
             reads=[gt_b], writes=[gt_b])
        p.op("act", lambda e: e.activation(out=T2[:], in_=T2[:], func=AF.Ln, bias=1.0),
             reads=[gt_b], writes=[gt_b])
        p.op("dve", lambda e: e.tensor_scalar_mul(out=LF[:], in0=T2[:], scalar1=-1.0), reads=[gt_b], writes=[gt_b])
        p.op("pe", lambda e: e.matmul(cx.ps[7][:, 0:NI], lhsT=hc.c32("U"), rhs=LF[:], start=True, stop=True),
             reads=[gt_b, hc.b], writes=[cx.psb[7]])
        p.op("dve", lambda e: e.tensor_tensor(out=WB[:], in0=IG[:], in1=cx.ps[7][:, 0:NI], op=ALU.subtract),
             reads=[gt_b, cx.psb[7]], writes=[gt_b])
        p.op("dve", lambda e: e.tensor_scalar_add(out=DB[:], in0=WB[:], scalar1=-LN16), reads=[gt_b], writes=[gt_b])

        qTs = p.sbuf([128, 2, SEQ], BF16, stack=st)
        kTs = p.sbuf([128, 2, SEQ], BF16, stack=st)
        ktm = p.sbuf([128, NCH, 256], BF16, stack=st)
        V = p.sbuf([128, NCH, 512], BF16, stack=st)
        O = p.sbuf([128, NCH, 512], F32, stack=st)
        hd_b = p.buf()
        hsem = p.new_dma_sem()
        C32 = p.sbuf([128, 2, 512], F32, stack=st)
        Cbf = p.sbuf([128, 2, 512], BF16, stack=st)
        n32 = p.sbuf([128, 2], F32, stack=st)
        nbf = p.sbuf([128, 2], BF16, stack=st)
        C32_b, Cbf_b, n32_b, nbf_b = p.buf(), p.buf(), p.buf(), p.buf()
        mTh = p.sbuf([128, 4, SEQ], BF16, stack=st)
        mTh_b = p.buf()
        osem = p.new_dma_sem()
        lfb = p.sbuf([128, 128], F32, stack=st); lfb_b = p.buf()
        bl = p.sbuf([128, 1], F32, stack=st); bl_b = p.buf()
        tt_ = p.sbuf([128, 128], F32, stack=st); tt_b = p.buf()
        DT = p.sbuf([128, 128], F32, stack=st); DT_b = p.buf()
        EB = p.sbuf([128, 128], F32, stack=st); EB_b = p.buf()
        qp = p.sbuf([128, 2, 128], BF16, stack=st); qp_b = p.buf()
        ST = p.sbuf([128, 128], BF16, stack=st); ST_b = p.buf()
        w_ = p.sbuf([128, 1], F32, stack=st); w_b = p.buf()
        eL = p.sbuf([128, 1], F32, stack=st); eL_b = p.buf()
        kw = p.sbuf([128, 256], BF16, stack=st); kw_b = p.buf()
        den = p.sbuf([128, 1], F32, stack=st); den_b = p.buf()
        hn = p.sbuf([128, 512], F32, stack=st); hn_b = p.buf()
        junk = p.sbuf([128, 512], F32, stack=st); junk_b = p.buf()
        ss = p.sbuf([128, 1], F32, stack=st); ss_b = p.buf()
        y = p.sbuf([128, 512], F32, stack=st); y_b = p.buf()
        sig = p.sbuf([128, 512], F32, stack=st); sig_b = p.buf()
        y2 = p.sbuf([128, 512], BF16, stack=st); y2_b = p.buf()
        ps3n_b = p.buf()
        ones_c = hc.cbf("ones")[:, 0:1]
        P = cx.ps
        B = cx.psb

        for h in range(nheads):
            for dc in range(2):
                r0 = h * 256 + dc * 128
                p.dma("sp", qTs[:, dc, :], d["qT"][r0:r0 + 128, :], hsem, writes=[hd_b])
                p.dma("sp", kTs[:, dc, :], d["kT"][r0:r0 + 128, :], hsem, writes=[hd_b])
            p.dma("pool", ktm[:], d["k_tm"][:, h * 256:(h + 1) * 256].rearrange("(c p) d -> p c d", p=128),
                  hsem, writes=[hd_b])
            p.dma("pool", V[:], d["v_tm"][:, h * 512:(h + 1) * 512].rearrange("(c p) d -> p c d", p=128),
                  hsem, writes=[hd_b])
            p.dma("sp", O[:], d["o_tm"][:, h * 512:(h + 1) * 512].rearrange("(c p) d -> p c d", p=128),
                  hsem, writes=[hd_b])
            p.op("pool", lambda e: e.memset(C32[:], 0.0), writes=[C32_b])
            p.op("pool", lambda e: e.memset(Cbf[:], 0.0), writes=[Cbf_b])
            p.op("pool", lambda e: e.memset(n32[:], 0.0), writes=[n32_b])
            p.op("pool", lambda e: e.memset(nbf[:], 0.0), writes=[nbf_b])
            for c in range(NCH):
                idx = h * NCH + c
                cols = slice(c * 128, (c + 1) * 128)
                p.op("pool", lambda e, idx=idx: e.tensor_scalar_mul(
                    out=lfb[:], in0=hc.c32("ones"), scalar1=LF[:, idx:idx + 1]),
                    reads=[gt_b, hc.b], writes=[lfb_b])
                p.op("pe", lambda e: e.matmul(P[0][:, 0:128], lhsT=lfb[:], rhs=hc.c32("U"), start=True, stop=True),
                     reads=[lfb_b, hc.b], writes=[B[0]])
                p.op("dve", lambda e: e.tensor_copy(out=bl[:], in_=P[0][:, 127:128]), reads=[B[0]], writes=[bl_b])
                p.op("dve", lambda e: e.tensor_tensor(out=tt_[:], in0=P[0][:, 0:128], in1=hc.c32("negU"),
                                                      op=ALU.add), reads=[B[0], hc.b], writes=[tt_b])
                p.op("act", lambda e, idx=idx: e.activation(out=DT[:], in_=tt_[:], func=AF.Exp,
                                                            bias=DB[:, idx:idx + 1]),
                     reads=[tt_b, gt_b], writes=[DT_b])
                p.op("act", lambda e: e.activation(out=EB[:], in_=P[0][:, 0:128], func=AF.Exp, bias=-LN16),
                     reads=[B[0]], writes=[EB_b])
                for dc in range(2):
                    p.op("dve", lambda e, dc=dc, cols=cols: e.tensor_tensor(
                        out=qp[:, dc, :], in0=qTs[:, dc, cols], in1=EB[:], op=ALU.mult),
                        reads=[hd_b, EB_b], writes=[qp_b])
                for dc in range(2):
                    p.op("pe", lambda e, dc=dc, cols=cols: e.matmul(
                        P[1][:, 0:128], lhsT=kTs[:, dc, cols], rhs=qTs[:, dc, cols], start=(dc == 0), stop=(dc == 1)),
                        reads=[hd_b], writes=[B[1]], signal=(dc == 1))
                p.op("dve", lambda e: e.tensor_tensor(out=ST[:], in0=P[1][:, 0:128], in1=DT[:], op=ALU.mult),
                     reads=[B[1], DT_b], writes=[ST_b])
                p.op("pe", lambda e, c=c: e.matmul(P[2][:], lhsT=ST[:], rhs=V[:, c, :], start=True, stop=False),
                     reads=[ST_b, hd_b], writes=[B[2]], signal=False)
                for dc in range(2):
                    p.op("pe", lambda e, dc=dc: e.matmul(P[2][:], lhsT=qp[:, dc, :], rhs=Cbf[:, dc, :],
                                                         start=False, stop=(dc == 1)),
                         reads=[qp_b, Cbf_b], writes=[B[2]], signal=(dc == 1))
                p.op("pe", lambda e: e.matmul(P[3][:, 0:1], lhsT=ST[:], rhs=ones_c, start=True, stop=False),
                     reads=[ST_b, hc.b], writes=[B[3]], signal=False)
                for dc in range(2):
                    p.op("pe", lambda e, dc=dc: e.matmul(P[3][:, 0:1], lhsT=qp[:, dc, :], rhs=nbf[:, dc:dc + 1],
                                                         start=False, stop=(dc == 1)),
                         reads=[qp_b, nbf_b], writes=[B[3]], signal=(dc == 1))
                p.op("act", lambda e, idx=idx: e.activation(out=w_[:], in_=WB[:, idx:idx + 1], func=AF.Exp,
                                                            bias=bl[:, 0:1]),
                     reads=[gt_b, bl_b], writes=[w_b])
                p.op("act", lambda e: e.activation(out=eL[:], in_=bl[:], func=AF.Exp), reads=[bl_b], writes=[eL_b])
                p.op("dve", lambda e, c=c: e.tensor_scalar_mul(out=kw[:], in0=ktm[:, c, :], scalar1=w_[:, 0:1]),
                     reads=[hd_b, w_b], writes=[kw_b])
                for dc in range(2):
                    p.op("pe", lambda e, dc=dc, c=c: e.matmul(
                        P[4 + dc][:], lhsT=kw[:, dc * 128:(dc + 1) * 128], rhs=V[:, c, :], start=True, stop=True),
                        reads=[kw_b, hd_b], writes=[B[4 + dc]])
                    p.op("pe", lambda e, dc=dc: e.matmul(
                        P[3][:, 1 + dc:2 + dc], lhsT=kw[:, dc * 128:(dc + 1) * 128], rhs=ones_c,
                        start=True, stop=True), reads=[kw_b, hc.b], writes=[ps3n_b])
                p.op("act", lambda e: e.activation(out=den[:], in_=P[3][:, 0:1], func=AF.Abs),
                     reads=[B[3]], writes=[den_b])
                p.op("dve", lambda e: e.tensor_scalar_max(out=den[:], in0=den[:], scalar1=1.0),
                     reads=[den_b], writes=[den_b])
                p.op("dve", lambda e: e.reciprocal(out=den[:], in_=den[:]), reads=[den_b], writes=[den_b])
                p.op("act", lambda e: e.activation(out=hn[:], in_=P[2][:], func=AF.Copy, scale=den[:, 0:1]),
                     reads=[B[2], den_b], writes=[hn_b])
                p.op("act", lambda e: e.activation(out=junk[:], in_=hn[:], func=AF.Square, accum_out=ss[:, 0:1]),
                     reads=[hn_b], writes=[junk_b, ss_b])
                p.op("act", lambda e: e.activation(out=ss[:], in_=ss[:], func=AF.Sqrt, bias=RMS_EPS,
                                                   scale=1.0 / 512.0), reads=[ss_b], writes=[ss_b])
                p.op("dve", lambda e: e.reciprocal(out=ss[:], in_=ss[:]), reads=[ss_b], writes=[ss_b])
                p.op("dve", lambda e, h=h: e.scalar_tensor_tensor(
                    out=y[:], in0=hn[:], scalar=ss[:, 0:1], in1=gain[:, h * 512:(h + 1) * 512],
                    op0=ALU.mult, op1=ALU.mult), reads=[hn_b, ss_b, setup_b], writes=[y_b])
                p.op("act", lambda e, c=c: e.activation(out=sig[:], in_=O[:, c, :], func=AF.Sigmoid),
                     reads=[hd_b], writes=[sig_b])
                p.op("pool", lambda e: e.tensor_tensor(out=y2[:], in0=y[:], in1=sig[:], op=ALU.mult),
                     reads=[y_b, sig_b], writes=[y2_b])
                for blk in range(4):
                    p.op("pe", lambda e, blk=blk: e.matmul(
                        P[6][:, blk * 128:(blk + 1) * 128], lhsT=y2[:, blk * 128:(blk + 1) * 128],
                        rhs=hc.cbf("ident"), start=True, stop=True),
                        reads=[y2_b, hc.b], writes=[B[6]], signal=(blk == 3))
                p.op("act", lambda e, cols=cols: e.activation(
                    out=mTh[:, :, cols], in_=P[6][:].rearrange("p (b j) -> p b j", b=4), func=AF.Copy),
                    reads=[B[6]], writes=[mTh_b])
                for dc in range(2):
                    p.op("dve", lambda e, dc=dc: e.scalar_tensor_tensor(
                        out=C32[:, dc, :], in0=C32[:, dc, :], scalar=eL[:, 0:1], in1=P[4 + dc][:],
                        op0=ALU.mult, op1=ALU.add), reads=[C32_b, eL_b, B[4 + dc]], writes=[C32_b])
                p.op("pool", lambda e: e.tensor_copy(out=Cbf[:], in_=C32[:]), reads=[C32_b], writes=[Cbf_b])
                p.op("dve", lambda e: e.scalar_tensor_tensor(
                    out=n32[:], in0=n32[:], scalar=eL[:, 0:1], in1=P[3][:, 1:3], op0=ALU.mult, op1=ALU.add),
                    reads=[n32_b, eL_b, ps3n_b], writes=[n32_b])
                p.op("dve", lambda e: e.tensor_copy(out=nbf[:], in_=n32[:]), reads=[n32_b], writes=[nbf_b])
            for blk in range(4):
                r0 = h * 512 + blk * 128
                p.dma("sp", mixT_out[r0:r0 + 128, :], mTh[:, blk, :], osem, reads=[mTh_b], writes=[mix_b])
        p.barrier()


def emit_sb(p, cx, hc, d, mixT_out, mix_b, nheads=12):
    SC = 128 ** -0.5
    NQ = SEQ // 512
    P, B = cx.ps, cx.psb
    with ExitStack() as st:
        qT = [p.sbuf([128, SEQ], BF16, stack=st) for _ in range(2)]
        kT = [p.sbuf([128, SEQ], BF16, stack=st) for _ in range(2)]
        V = [p.sbuf([128, SEQ // 128, 128], BF16, stack=st) for _ in range(2)]
        hd_b = [p.buf() for _ in range(2)]
        hsem = [p.new_dma_sem() for _ in range(2)]
        oT = [p.sbuf([128, SEQ], BF16, stack=st) for _ in range(2)]
        oT_b = [p.buf() for _ in range(2)]
        osem = p.new_dma_sem()
        NS = 2
        ex = [p.sbuf([128, 512], F32, stack=st) for _ in range(NS)]; ex_b = [p.buf() for _ in range(NS)]
        l_ = [p.sbuf([128, 512], F32, stack=st) for _ in range(NS)]; l_b = [p.buf() for _ in range(NS)]
        zl = [p.sbuf([128, 512], F32, stack=st) for _ in range(NS)]; zl_b = [p.buf() for _ in range(NS)]
        AT = [p.sbuf([128, 512], BF16, stack=st) for _ in range(NS)]; AT_b = [p.buf() for _ in range(NS)]
        R = p.sbuf([128, 512], F32, stack=st); R_b = p.buf()
        zeros = p.sbuf([128, 128], BF16, stack=st); z_b = p.buf()
        p.op("pool", lambda e: e.memset(zeros[:], 0.0), writes=[z_b])
        it = 0
        for h in range(nheads):
            hs = h % 2
            r0 = h * 128
            p.dma("sp", qT[hs][:], d["qT"][r0:r0 + 128, :], hsem[hs], writes=[hd_b[hs]])
            p.dma("sp", kT[hs][:], d["kT"][r0:r0 + 128, :], hsem[hs], writes=[hd_b[hs]])
            p.dma("pool", V[hs][:], d["v_tm"][:, r0:r0 + 128].rearrange("(c p) d -> p c d", p=128),
                  hsem[hs], writes=[hd_b[hs]])
            for Q in range(NQ):
                ob = 6 + (Q % 2)
                p.op("pe", lambda e, ob=ob, hs=hs: e.matmul(P[ob][:], lhsT=zeros[:], rhs=qT[hs][:, 0:512],
                                                            start=True, stop=False),
                     reads=[z_b, hd_b[hs]], writes=[B[ob]], signal=False)
                p.op("pool", lambda e: e.memset(R[:], 0.0), writes=[R_b])
                amax = 4 * Q + 3
                for a in range(amax, -1, -1):
                    s = it % NS
                    zb = (it % 2)
                    pb = 2 + (it % 2)
                    it += 1
                    c0 = max(0, 128 * a - 512 * Q)
                    diag = a >= 4 * Q
                    cs = slice(c0, 512)
                    qs = slice(512 * Q + c0, 512 * Q + 512)
                    p.op("pe", lambda e, zb=zb, hs=hs, a=a, cs=cs, qs=qs: e.matmul(
                        P[zb][:, cs], lhsT=kT[hs][:, a * 128:(a + 1) * 128], rhs=qT[hs][:, qs],
                        start=True, stop=True), reads=[hd_b[hs]], writes=[B[zb]])
                    p.op("act", lambda e, s=s, zb=zb, cs=cs: e.activation(
                        out=ex[s][:, cs], in_=P[zb][:, cs], func=AF.Exp, scale=SC),
                        reads=[B[zb]], writes=[ex_b[s]])
                    p.op("act", lambda e, s=s, cs=cs: e.activation(
                        out=l_[s][:, cs], in_=ex[s][:, cs], func=AF.Ln, bias=1.0),
                        reads=[ex_b[s]], writes=[l_b[s]])
                    if diag:
                        p.op("pool", lambda e, s=s, c0=c0: e.tensor_tensor(
                            out=l_[s][:, c0:c0 + 128], in0=l_[s][:, c0:c0 + 128], in1=hc.c32("maskS"),
                            op=ALU.mult), reads=[l_b[s], hc.b], writes=[l_b[s]])
                    p.op("dve", lambda e, s=s, zb=zb, cs=cs: e.scalar_tensor_tensor(
                        out=zl[s][:, cs], in0=P[zb][:, cs], scalar=SC, in1=l_[s][:, cs],
                        op0=ALU.mult, op1=ALU.subtract), reads=[B[zb], l_b[s]], writes=[zl_b[s]])
                    first = a == amax
                    p.op("pe", lambda e, pb=pb, s=s, cs=cs, first=first: e.matmul(
                        P[pb][:, cs], lhsT=hc.c32("Tri"), rhs=l_[s][:, cs], start=True, stop=first),
                        reads=[hc.b, l_b[s]], writes=[B[pb]], signal=first)
                    if not first:
                        p.op("pe", lambda e, pb=pb, cs=cs: e.matmul(
                            P[pb][:, cs], lhsT=hc.c32("ones"), rhs=R[:, cs], start=False, stop=True),
                            reads=[hc.b, R_b], writes=[B[pb]])
                    p.op("dve", lambda e, s=s, pb=pb, cs=cs: e.tensor_tensor(
                        out=zl[s][:, cs], in0=zl[s][:, cs], in1=P[pb][:, cs], op=ALU.subtract),
                        reads=[zl_b[s], B[pb]], writes=[zl_b[s]])
                    if diag:
                        p.op("pool", lambda e, s=s, c0=c0: e.tensor_tensor(
                            out=zl[s][:, c0:c0 + 128], in0=zl[s][:, c0:c0 + 128], in1=hc.c32("negS"),
                            op=ALU.add), reads=[zl_b[s], hc.b], writes=[zl_b[s]])
                    p.op("act", lambda e, s=s, cs=cs: e.activation(out=AT[s][:, cs], in_=zl[s][:, cs], func=AF.Exp),
                         reads=[zl_b[s]], writes=[AT_b[s]])
                    last = a == 0
                    p.op("pe", lambda e, ob=ob, hs=hs, a=a, s=s, cs=cs, last=last: e.matmul(
                        P[ob][:, cs], lhsT=V[hs][:, a, :], rhs=AT[s][:, cs], start=False, stop=last),
                        reads=[hd_b[hs], AT_b[s]], writes=[B[ob]])
                    if not last:
                        p.op("pool", lambda e, s=s, cs=cs: e.tensor_tensor(
                            out=R[:, cs], in0=R[:, cs], in1=l_[s][:, cs], op=ALU.add),
                            reads=[R_b, l_b[s]], writes=[R_b])
                p.op("act", lambda e, ob=ob, hs=hs, Q=Q: e.activation(
                    out=oT[hs][:, Q * 512:(Q + 1) * 512], in_=P[ob][:], func=AF.Copy),
                    reads=[B[ob]], writes=[oT_b[hs]])
            p.dma("sp", mixT_out[r0:r0 + 128, :], oT[hs][:], osem, reads=[oT_b[hs]], writes=[mix_b])
        p.barrier()


FCN = D_FF // 128
A_FM, A_TM = 32, 16
B_FM, B_TM = 56, 6
N_NORMS = 2 + 3 * DEPTH
NTP = SEQ // NT


def build_fused():
    p = Prog()
    EI, EO = "ExternalInput", "ExternalOutput"
    consts = p.dram("consts", [128, len(CONST_NAMES), 128], F32, kind=EI)
    xT = p.dram("xT", [D_MODEL, SEQ], F32, kind=EI)
    memT = p.dram("memT", [D_MODEL, MEM_TOKENS], F32, kind=EI)
    g_d = p.dram("g", [128, N_NORMS * KC], F32, kind=EI)
    w_mk = p.dram("w_mk", [8, KC // KG, 128, KG, 128], F32, kind=EI)
    w_mv = p.dram("w_mv", [2, KC // KG, 128, KG, 512], F32, kind=EI)
    W = []
    for i in range(DEPTH):
        n_fm, n_tm = (A_FM, A_TM) if i % 2 == 0 else (B_FM, B_TM)
        W.append({
            "w1_in": p.dram(f"w1_in{i}", [FCN, KC // KG, 128, KG, 256], F32, kind=EI),
            "w1_out": p.dram(f"w1_out{i}", [KC, FCN // KG, 128, KG, 128], F32, kind=EI),
            "w_fm": p.dram(f"w_fm{i}", [n_fm, KC // KG, 128, KG, 128], F32, kind=EI),
            "w_tm": p.dram(f"w_tm{i}", [n_tm, KC // KG, 128, KG, 512], F32, kind=EI),
            "w_mo": p.dram(f"w_mo{i}", [KC, KC // KG, 128, KG, 128], F32, kind=EI),
            "w2_in": p.dram(f"w2_in{i}", [FCN, KC // KG, 128, KG, 256], F32, kind=EI),
            "w2_out": p.dram(f"w2_out{i}", [KC, FCN // KG, 128, KG, 128], F32, kind=EI),
        })
    gbs = [p.dram(f"gb{j}", [128, 12], F32, kind=EI) for j in range(2)]
    gains = [p.dram(f"gain{j}", [128, 3072], F32, kind=EI) for j in range(2)]
    outT = p.dram("outT", [D_MODEL, SEQ], F32, kind=EO)
    hs = [[p.dram(f"hs{tp}_{k}", [D_MODEL, NT], F32) for k in range(3)] for tp in range(NTP)]
    fmT = {"A": p.dram("fmT_A", [A_FM * 128, SEQ], BF16), "B": p.dram("fmT_B", [B_FM * 128, SEQ], BF16)}
    tm = {"A": p.dram("tm_A", [SEQ, A_TM * 512], F32), "B": p.dram("tm_B", [SEQ, B_TM * 512], F32)}
    mixT = p.dram("mixT", [D_MODEL, SEQ], BF16)
    mk_d = p.dram("mk_d", [128, 8 * MEM_TOKENS], BF16)
    mv_d = p.dram("mv_d", [128, 2 * 1024], BF16)

    cx = Ctx(p, consts)
    hc = HConsts(p, consts)
    g_sb = p.sbuf([128, N_NORMS * KC], F32)
    g_b = p.buf()
    p.dma("sp", g_sb[:], g_d[:, :], p.new_dma_sem(), writes=[g_b])

    with ExitStack() as st:
        mkT = p.sbuf([128, 8, MEM_TOKENS], BF16, stack=st)
        mv = p.sbuf([128, 2, 1024], BF16, stack=st)
        mkT_b, mv_b = p.buf(), p.buf()
        emit_memkv(p, cx, memT, g_sb, g_b, w_mk, w_mv, mkT, mkT_b, mv, mv_b, nmh=4, gcol=0)
        sem = p.new_dma_sem()
        p.dma("sp", mk_d[:, :], mkT[:].rearrange("p a b -> p (a b)"), sem, reads=[mkT_b])
        p.dma("sp", mv_d[:, :], mv[:].rearrange("p a b -> p (a b)"), sem, reads=[mv_b])
        p.barrier()

    def tsl(tp):
        return slice(tp * NT, (tp + 1) * NT)

    cur = [None] * NTP
    for i in range(DEPTH + 1):
        mx = ("A" if i % 2 == 0 else "B") if i < DEPTH else None
        for tp in range(NTP):
            if i == 0:
                src = xT[:, tsl(tp)]
                ci = 0
            else:
                ci = cur[tp]
                src = hs[tp][ci]
                a, b = (ci + 1) % 3, (ci + 2) % 3
                emit_mixout(p, cx, src, hs[tp][a], mixT[:, tsl(tp)], W[i - 1]["w_mo"], NT)
                emit_ffn(p, cx, hs[tp][a], hs[tp][b], g_sb, g_b, (3 * (i - 1) + 3) * KC,
                         W[i - 1]["w2_in"], W[i - 1]["w2_out"], NT)
                src = hs[tp][b]
            if i < DEPTH:
                emit_ffn(p, cx, src, hs[tp][ci], g_sb, g_b, (3 * i + 1) * KC, W[i]["w1_in"], W[i]["w1_out"], NT)
                cur[tp] = ci
                n_fm, n_tm = (A_FM, A_TM) if mx == "A" else (B_FM, B_TM)
                emit_inproj(p, cx, hs[tp][ci], g_sb, g_b, (3 * i + 2) * KC, W[i]["w_fm"], n_fm,
                            fmT[mx][:, tsl(tp)], W[i]["w_tm"], n_tm, tm[mx][tsl(tp), :], NT)
            else:
                emit_final_norm(p, cx, src, g_sb, g_b, (N_NORMS - 1) * KC, outT[:, tsl(tp)], NT)
        if i == DEPTH:
            break
        mix_b = p.buf()
        with ExitStack() as st:
            mkT = p.sbuf([128, 8, MEM_TOKENS], BF16, stack=st)
            mv = p.sbuf([128, 2, 1024], BF16, stack=st)
            mkT_b, mv_b = p.buf(), p.buf()
            sem = p.new_dma_sem()
            p.dma("sp", mkT[:].rearrange("p a b -> p (a b)"), mk_d[:, :], sem, writes=[mkT_b])
            p.dma("sp", mv[:].rearrange("p a b -> p (a b)"), mv_d[:, :], sem, writes=[mv_b])
            f, t = fmT[mx], tm[mx]
            if mx == "A":
                emit_memattn(p, cx, hc, f[3072:4096, :], mkT, mkT_b, mv, mv_b, mixT, 3072, mix_b, nmh=4)
                d = {"qT": f[0:1536, :], "kT": f[1536:3072, :], "k_tm": t[:, 0:1536], "v_tm": t[:, 1536:4608],
                     "o_tm": t[:, 4608:7680], "gates": t[:, 7680:7692], "gb": gbs[i // 2], "gain": gains[i // 2]}
                emit_mlstm(p, cx, hc, d, mixT, mix_b, nheads=6)
            else:
                emit_memattn(p, cx, hc, f[6144:7168, :], mkT, mkT_b, mv, mv_b, mixT, 3072, mix_b, nmh=4)
                d = {"qT": f[0:3072, :], "kT": f[3072:6144, :], "v_tm": t[:, 0:3072]}
                emit_sb(p, cx, hc, d, mixT, mix_b, nheads=24)
    return p.finish()


_PROG = {}


def _glay(*norms):
    return np.ascontiguousarray(np.concatenate([np.asarray(n, np.float32).reshape(KC, 128).T for n in norms], axis=1))


def _tile_ffn(w_in, w_out):
    wi = tile_w(w_in, 256, [[(j * 128, 128), (D_FF + j * 128, 128)] for j in range(FCN)])
    wo = tile_w(w_out, 128, [[(m * 128, 128)] for m in range(KC)])
    return wi, wo


def _tile_inproj(w, mixer):
    if mixer == "A":
        fm_cols = [(c, 128) for c in range(0, 3072, 128)] + [(9228 + c, 128) for c in range(0, 1024, 128)]
        tm_cols = [[(1536 + c, 512)] for c in range(0, 1536, 512)] + \
                  [[(3072 + c, 512)] for c in range(0, 6144, 512)] + [[(9216, 12)]]
    else:
        fm_cols = [(c, 128) for c in range(0, 6144, 128)] + [(9216 + c, 128) for c in range(0, 1024, 128)]
        tm_cols = [[(6144 + c, 512)] for c in range(0, 3072, 512)]
    return tile_w(w, 128, [[c] for c in fm_cols]), tile_w(w, 512, tm_cols)


def kernel(**inputs):
    f32 = np.float32
    x = np.asarray(inputs["x"], f32)
    mem = np.asarray(inputs["mem"], f32)
    shared = {"consts": host_consts()}
    norms = [inputs["mem_norm"]]
    for i in range(DEPTH):
        norms += [inputs["norm_ffn1"][i], inputs["norm_mix"][i], inputs["norm_ffn2"][i]]
    norms.append(inputs["norm_final"])
    shared["g"] = _glay(*[np.asarray(n, f32) for n in norms])
    wkv = np.asarray(inputs["w_mem_kv"], f32)
    shared["w_mk"] = tile_w(wkv[:, 0:1024], 128, [[(i * 128, 128)] for i in range(8)])
    shared["w_mv"] = tile_w(wkv[:, 1024:2048], 512, [[(0, 512)], [(512, 512)]])
    for i in range(DEPTH):
        shared[f"w1_in{i}"], shared[f"w1_out{i}"] = _tile_ffn(np.asarray(inputs["ffn1_w_in"][i], f32),
                                                              np.asarray(inputs["ffn1_w_out"][i], f32))
        shared[f"w2_in{i}"], shared[f"w2_out{i}"] = _tile_ffn(np.asarray(inputs["ffn2_w_in"][i], f32),
                                                              np.asarray(inputs["ffn2_w_out"][i], f32))
        w = np.asarray(inputs["a_w_in" if i % 2 == 0 else "b_w_in"][i // 2], f32)
        shared[f"w_fm{i}"], shared[f"w_tm{i}"] = _tile_inproj(w, "A" if i % 2 == 0 else "B")
        shared[f"w_mo{i}"] = tile_w(np.asarray(inputs["w_out"][i], f32), 128, [[(m * 128, 128)] for m in range(KC)])
    for j in range(2):
        gb = np.concatenate([np.asarray(inputs["a_b_igate"], f32)[j], np.asarray(inputs["a_b_fgate"], f32)[j]])
        shared[f"gb{j}"] = np.ascontiguousarray(np.broadcast_to(gb[None, :], (128, 12)))
        shared[f"gain{j}"] = np.ascontiguousarray(
            np.broadcast_to(np.asarray(inputs["a_head_gain"], f32)[j][None, :], (128, 3072)))
    maps = []
    for c in range(NCORES):
        b = c // 2
        m = dict(shared)
        m["xT"] = np.ascontiguousarray(x[b].T)
        m["memT"] = np.ascontiguousarray(mem[b].T)
        maps.append(m)
    if "nc" not in _PROG:
        _PROG["nc"] = build_fused()
    res = run_bass_kernel_spmd(_PROG["nc"], maps, core_ids=list(range(NCORES))).results
    del maps, shared
    out = np.empty((BATCH, SEQ, D_MODEL), f32)
    for b in range(BATCH):
        out[b, :NT] = res[2 * b]["outT"][:, :NT].T
        out[b, NT:] = res[2 * b + 1]["outT"][:, NT:].T
    return out
```

```python
import numpy as np
import ml_dtypes
from contextlib import ExitStack
import concourse.bass as bass
import concourse.mybir as mybir
from concourse.bass_utils import run_bass_kernel_spmd

F32 = mybir.dt.float32
BF16 = mybir.dt.bfloat16
AF = mybir.ActivationFunctionType
ALU = mybir.AluOpType

D_MODEL = 4096
BATCH = 4
SEQ = 2048
DEPTH = 4
MEM_TOKENS = 256
D_FF = 6144
RMS_EPS = 1e-6
NCORES = 8
NT = 1024
KC = D_MODEL // 128


class Buf:
    __slots__ = ("name", "last_write", "reads")

    def __init__(self, name):
        self.name = name
        self.last_write = None
        self.reads = []


class Prog:
    ENGS = ("pe", "act", "dve", "pool", "sp")

    def __init__(self):
        self.nc = bass.Bass("TRN2", target_bir_lowering=False)
        self.es = ExitStack()
        self.ops = {e: [] for e in self.ENGS}
        self.count = {e: 0 for e in self.ENGS}
        self.sem = {}
        for e in ("pe", "act", "dve", "pool"):
            self.sem[e] = self.es.enter_context(self.nc.semaphore("sem_" + e))
        self.seen = {}
        self.dma_sems = []
        self.free_dma_sems = []
        self.live_dma_sems = []
        self.nbuf = 0
        self.tensors = 0

    def sbuf(self, shape, dtype, name=None, stack=None):
        self.tensors += 1
        name = name or f"sb{self.tensors}"
        t = (stack or self.es).enter_context(self.nc.sbuf_tensor(name, list(shape), dtype))
        return t

    def psum(self, shape, dtype, name=None, stack=None):
        self.tensors += 1
        name = name or f"ps{self.tensors}"
        t = (stack or self.es).enter_context(self.nc.psum_tensor(name, list(shape), dtype))
        return t

    def dram(self, name, shape, dtype, kind=None):
        if kind is None:
            return self.nc.dram_tensor(name, list(shape), dtype)
        return self.nc.dram_tensor(name, list(shape), dtype, kind=kind)

    def buf(self, name=None):
        self.nbuf += 1
        return Buf(name or f"b{self.nbuf}")

    def new_dma_sem(self):
        if self.free_dma_sems:
            ent = self.free_dma_sems.pop()
        else:
            s = self.es.enter_context(self.nc.semaphore(f"dsem{len(self.dma_sems)}"))
            ent = [s, 0]
            self.dma_sems.append(ent)
        self.live_dma_sems.append(ent)
        return ent

    def _wait(self, eng, tok):
        if tok is None:
            return
        kind, key, val = tok
        if kind == "eng":
            if key == eng and eng == "pe":
                return
            sem = self.sem[key]
            skey = (eng, "e" + key)
        else:
            sem = key[0]
            skey = (eng, id(key))
        if self.seen.get(skey, 0) >= val:
            return
        self.seen[skey] = val
        self.ops[eng].append(lambda e, sem=sem, val=val: e.wait_ge(sem, val))

    def _deps(self, eng, reads, writes, dsem=None):
        toks = []
        for b in reads:
            toks.append(b.last_write)
        for b in writes:
            lw = b.last_write
            if not (dsem is not None and lw is not None and lw[0] == "dma" and lw[1] is dsem):
                toks.append(lw)
            toks.extend(b.reads)
        for t in toks:
            self._wait(eng, t)

    def _commit(self, tok, reads, writes):
        for b in reads:
            b.reads.append(tok)
        for b in writes:
            b.last_write = tok
            b.reads = []

    def op(self, eng, fn, reads=(), writes=(), signal=True):
        self._deps(eng, reads, writes)
        if signal:
            self.count[eng] += 1
            n = self.count[eng]
            sem = self.sem[eng]
            self.ops[eng].append(lambda e, fn=fn, sem=sem: fn(e).then_inc(sem, 1))
        else:
            n = self.count[eng] + 1
            self.ops[eng].append(lambda e, fn=fn: fn(e))
        self._commit(("eng", eng, n), reads, writes)

    def dma(self, eng, out, in_, dsem, reads=(), writes=(), **kw):
        self._deps(eng, reads, writes, dsem)
        dsem[1] += 16
        sem = dsem[0]
        self.ops[eng].append(
            lambda e, out=out, in_=in_, sem=sem, kw=kw: e.dma_start(out=out, in_=in_, **kw).then_inc(sem, 16))
        self._commit(("dma", dsem, dsem[1]), reads, writes)

    def barrier(self):
        for eng in self.ENGS:
            for other in ("pe", "act", "dve", "pool"):
                if other != eng and self.count[other] > 0:
                    self._wait(eng, ("eng", other, self.count[other]))
            for ent in self.dma_sems:
                if ent[1] > 0:
                    self._wait(eng, ("dma", ent, ent[1]))
        self.free_dma_sems.extend(self.live_dma_sems)
        self.live_dma_sems = []

    def finish(self):
        self.barrier()
        nc = self.nc
        with nc.Block() as block:
            @block.tensor
            def _(e):
                for f in self.ops["pe"]:
                    f(e)

            @block.scalar
            def _(e):
                for f in self.ops["act"]:
                    f(e)

            @block.vector
            def _(e):
                for f in self.ops["dve"]:
                    f(e)

            @block.gpsimd
            def _(e):
                for f in self.ops["pool"]:
                    f(e)

            @block.sync
            def _(e):
                for f in self.ops["sp"]:
                    f(e)
        self.es.close()
        return nc


KG = 8
CONST_NAMES_IDX_ONES = 5


class Ctx:
    def __init__(self, p, consts_dram):
        self.p = p
        nc = p.nc
        self.ps = [p.psum([128, 512], F32, name=f"psb{i}") for i in range(8)]
        self.psb = [p.buf(f"psb{i}") for i in range(8)]
        self.ones_bf = p.sbuf([128, 128], BF16, name="ones_bf")
        self.ones_bf_b = p.buf("ones_bf")
        self.csem = p.new_dma_sem()
        p.dma("pool", self.ones_bf[:], consts_dram[:, CONST_NAMES_IDX_ONES, :], self.csem, writes=[self.ones_bf_b])


def tile_w(W, ncols, col_blocks):
    K = W.shape[0]
    kcn = K // 128
    kgn = kcn // KG
    MT = len(col_blocks)
    out = np.zeros((MT, kgn, 128, KG, ncols), np.float32)
    Wr = W.reshape(kgn, KG, 128, W.shape[1])
    for m, blocks in enumerate(col_blocks):
        c0 = 0
        for (s, w) in blocks:
            out[m, :, :, :, c0:c0 + w] = Wr[:, :, :, s:s + w].transpose(0, 2, 1, 3)
            c0 += w
    return out


def emit_rmsnorm(p, cx, hT_dram, g_sb, g_buf, gcol, xnT, xnT_buf, nt, kcn=KC):
    with ExitStack() as st:
        hs = [p.sbuf([128, nt], F32, stack=st) for _ in range(2)]
        hsb = [p.buf() for _ in range(2)]
        hsem = [p.new_dma_sem() for _ in range(2)]
        sq = [p.sbuf([128, nt], BF16, stack=st) for _ in range(2)]
        sqb = [p.buf() for _ in range(2)]
        rstd = p.sbuf([128, nt], F32, stack=st)
        rstdb = p.buf()
        tw = min(512, nt)
        ntt = nt // tw
        for kc in range(kcn):
            s = kc % 2
            p.dma("sp", hs[s][:], hT_dram[kc * 128:(kc + 1) * 128, :], hsem[s], writes=[hsb[s]])
            p.op("act", lambda e, s=s: e.activation(out=sq[s][:], in_=hs[s][:], func=AF.Square),
                 reads=[hsb[s]], writes=[sqb[s]])
            for tt in range(ntt):
                last = kc == kcn - 1
                p.op("pe", lambda e, s=s, tt=tt, kc=kc, last=last: e.matmul(
                    cx.ps[tt][:, 0:tw], lhsT=cx.ones_bf[:], rhs=sq[s][:, tt * tw:(tt + 1) * tw],
                    start=(kc == 0), stop=last),
                    reads=[sqb[s], cx.ones_bf_b], writes=[cx.psb[tt]], signal=(last or tt == ntt - 1))
        for tt in range(ntt):
            p.op("act", lambda e, tt=tt: e.activation(
                out=rstd[:, tt * tw:(tt + 1) * tw], in_=cx.ps[tt][:, 0:tw], func=AF.Sqrt,
                bias=RMS_EPS, scale=1.0 / (kcn * 128)),
                reads=[cx.psb[tt]], writes=[rstdb])
        p.op("dve", lambda e: e.reciprocal(out=rstd[:], in_=rstd[:]), reads=[rstdb], writes=[rstdb])
        for kc in range(kcn):
            s = kc % 2
            p.dma("sp", hs[s][:], hT_dram[kc * 128:(kc + 1) * 128, :], hsem[s], writes=[hsb[s]])
            p.op("dve", lambda e, s=s, kc=kc: e.scalar_tensor_tensor(
                out=xnT[:, kc, :], in0=hs[s][:], scalar=g_sb[:, gcol + kc:gcol + kc + 1], in1=rstd[:],
                op0=ALU.mult, op1=ALU.mult),
                reads=[hsb[s], rstdb, g_buf], writes=[xnT_buf])
        p.barrier()


class WStream:
    def __init__(self, p, nslots, ncols, stack):
        self.p = p
        self.ncols = ncols
        self.nslots = nslots
        self.slots = [p.sbuf([128, KG, ncols], BF16, stack=stack) for _ in range(nslots)]
        self.bufs = [p.buf() for _ in range(nslots)]
        self.sems = [p.new_dma_sem() for _ in range(nslots)]
        self.i = 0

    def load(self, src_ap):
        s = self.i % self.nslots
        self.i += 1
        self.p.dma("pool", self.slots[s][:], src_ap, self.sems[s], writes=[self.bufs[s]])
        return self.slots[s], self.bufs[s]


def emit_lin_fm(p, cx, ws, xT, xT_buf, kcn, w_dram, mt, nblk, nt, evac, ps_base=0, nsets=2):
    tw = min(512, nt)
    ntt = nt // tw
    per_set = nblk * ntt
    kgn = kcn // KG
    for m in range(mt):
        st_ = m % nsets
        banks = [[ps_base + st_ * per_set + b * ntt + tt for tt in range(ntt)] for b in range(nblk)]
        for kg in range(kgn):
            wt, wb = ws.load(w_dram[m, kg])
            for ki in range(KG):
                kc = kg * KG + ki
                for b in range(nblk):
                    for tt in range(ntt):
                        last = kc == kcn - 1
                        bk = banks[b][tt]
                        p.op("pe", lambda e, wt=wt, ki=ki, b=b, tt=tt, kc=kc, bk=bk, last=last: e.matmul(
                            cx.ps[bk][:, 0:tw], lhsT=wt[:, ki, b * 128:(b + 1) * 128],
                            rhs=xT[:, kc, tt * tw:(tt + 1) * tw], start=(kc == 0), stop=last),
                            reads=[wb, xT_buf], writes=[cx.psb[bk]],
                            signal=(last or (ki == KG - 1 and b == nblk - 1 and tt == ntt - 1)))
        evac(m, banks)


def emit_lin_tm(p, cx, ws, xT, xT_buf, kcn, w_dram, ct_n, ncols, nt, evac, ps_base=0):
    kgn = kcn // KG
    ntg = nt // 128
    i = 0
    for ct in range(ct_n):
        tiles = [ws.load(w_dram[ct, kg]) for kg in range(kgn)]
        for tg in range(ntg):
            bk = ps_base + (i % 2)
            i += 1
            for kc in range(kcn):
                wt, wb = tiles[kc // KG]
                last = kc == kcn - 1
                p.op("pe", lambda e, wt=wt, kc=kc, tg=tg, bk=bk, last=last: e.matmul(
                    cx.ps[bk][:, 0:ncols], lhsT=xT[:, kc, tg * 128:(tg + 1) * 128],
                    rhs=wt[:, kc % KG, :], start=(kc == 0), stop=last),
                    reads=[wb, xT_buf], writes=[cx.psb[bk]], signal=(last or kc % KG == KG - 1))
            evac(ct, tg, bk)


def emit_ffn(p, cx, hT_in, hT_out, g_sb, g_buf, gcol, w_in_dram, w_out_dram, nt, kcn=KC, fcn=D_FF // 128,
             h_in_buf=None, h_out_buf=None):
    h_in_buf = h_in_buf or p.buf()
    h_out_buf = h_out_buf or p.buf()
    with ExitStack() as st:
        xnT = p.sbuf([128, kcn, nt], BF16, stack=st)
        xnT_b = p.buf()
        emit_rmsnorm(p, cx, hT_in, g_sb, g_buf, gcol, xnT, xnT_b, nt, kcn)
        aT = p.sbuf([128, fcn, nt], BF16, stack=st)
        aT_b = p.buf()
        ntt = nt // 512
        with ExitStack() as st2:
            ws = WStream(p, 3, 256, st2)
            sg = [p.sbuf([128, 512], F32, stack=st2) for _ in range(2)]
            sgb = [p.buf() for _ in range(2)]
            cnt = [0]

            def evac_in(j, banks):
                for tt in range(ntt):
                    i = cnt[0] % 2
                    cnt[0] += 1
                    bg, bu = banks[0][tt], banks[1][tt]
                    p.op("act", lambda e, i=i, bg=bg: e.activation(out=sg[i][:], in_=cx.ps[bg][:], func=AF.Silu),
                         reads=[cx.psb[bg]], writes=[sgb[i]])
                    p.op("dve", lambda e, i=i, bu=bu, j=j, tt=tt: e.tensor_tensor(
                        out=aT[:, j, tt * 512:(tt + 1) * 512], in0=sg[i][:], in1=cx.ps[bu][:], op=ALU.mult),
                        reads=[sgb[i], cx.psb[bu]], writes=[aT_b])
            emit_lin_fm(p, cx, ws, xnT, xnT_b, kcn, w_in_dram, fcn, 2, nt, evac_in)
            p.barrier()
        with ExitStack() as st3:
            ws = WStream(p, 4, 128, st3)
            hs = [p.sbuf([128, nt], F32, stack=st3) for _ in range(2)]
            hsb = [p.buf() for _ in range(2)]
            hsem = [p.new_dma_sem() for _ in range(2)]
            osem = p.new_dma_sem()

            def evac_out(m, banks):
                s = m % 2
                p.dma("pool", hs[s][:], hT_in[m * 128:(m + 1) * 128, :], hsem[s],
                      reads=[h_in_buf], writes=[hsb[s]])
                for tt in range(ntt):
                    bk = banks[0][tt]
                    p.op("dve", lambda e, s=s, bk=bk, tt=tt: e.scalar_tensor_tensor(
                        out=hs[s][:, tt * 512:(tt + 1) * 512], in0=cx.ps[bk][:], scalar=0.5,
                        in1=hs[s][:, tt * 512:(tt + 1) * 512], op0=ALU.mult, op1=ALU.add),
                        reads=[cx.psb[bk], hsb[s]], writes=[hsb[s]])
                p.dma("sp", hT_out[m * 128:(m + 1) * 128, :], hs[s][:], osem, reads=[hsb[s]], writes=[h_out_buf])
            emit_lin_fm(p, cx, ws, aT, aT_b, fcn, w_out_dram, kcn, 1, nt, evac_out)
            p.barrier()
    return h_out_buf


def emit_inproj(p, cx, hT_in, g_sb, g_buf, gcol, w_fm_dram, n_fm, fmT_out, w_tm_dram, n_tm, tm_out, nt,
                kcn=KC):
    fm_b, tm_b = p.buf(), p.buf()
    with ExitStack() as st:
        xnT = p.sbuf([128, kcn, nt], BF16, stack=st)
        xnT_b = p.buf()
        emit_rmsnorm(p, cx, hT_in, g_sb, g_buf, gcol, xnT, xnT_b, nt, kcn)
        ntt = nt // 512
        with ExitStack() as st2:
            ws = WStream(p, 4, 128, st2)
            og = [p.sbuf([128, nt], BF16, stack=st2) for _ in range(2)]
            ogb = [p.buf() for _ in range(2)]
            osem = p.new_dma_sem()

            def evac_fm(m, banks):
                s = m % 2
                for tt in range(ntt):
                    bk = banks[0][tt]
                    eng = "act" if tt % 2 == 0 else "dve"
                    if eng == "act":
                        p.op("act", lambda e, s=s, bk=bk, tt=tt: e.activation(
                            out=og[s][:, tt * 512:(tt + 1) * 512], in_=cx.ps[bk][:], func=AF.Copy),
                            reads=[cx.psb[bk]], writes=[ogb[s]])
                    else:
                        p.op("dve", lambda e, s=s, bk=bk, tt=tt: e.tensor_copy(
                            out=og[s][:, tt * 512:(tt + 1) * 512], in_=cx.ps[bk][:]),
                            reads=[cx.psb[bk]], writes=[ogb[s]])
                p.dma("sp", fmT_out[m * 128:(m + 1) * 128, :], og[s][:], osem, reads=[ogb[s]], writes=[fm_b])
            emit_lin_fm(p, cx, ws, xnT, xnT_b, kcn, w_fm_dram, n_fm, 1, nt, evac_fm)
            p.barrier()
        with ExitStack() as st3:
            kgn = kcn // KG
            ws = WStream(p, 2 * kgn, 512, st3)
            ot = [p.sbuf([128, 512], F32, stack=st3) for _ in range(3)]
            otb = [p.buf() for _ in range(3)]
            osem = p.new_dma_sem()
            cnt = [0]

            def evac_tm(ct, tg, bk):
                s = cnt[0] % 3
                eng = "act" if cnt[0] % 2 == 0 else "dve"
                cnt[0] += 1
                if eng == "act":
                    p.op("act", lambda e, s=s, bk=bk: e.activation(out=ot[s][:], in_=cx.ps[bk][:], func=AF.Copy),
                         reads=[cx.psb[bk]], writes=[otb[s]])
                else:
                    p.op("dve", lambda e, s=s, bk=bk: e.tensor_copy(out=ot[s][:], in_=cx.ps[bk][:]),
                         reads=[cx.psb[bk]], writes=[otb[s]])
                p.dma("sp", tm_out[tg * 128:(tg + 1) * 128, ct * 512:(ct + 1) * 512], ot[s][:], osem,
                      reads=[otb[s]], writes=[tm_b])
            emit_lin_tm(p, cx, ws, xnT, xnT_b, kcn, w_tm_dram, n_tm, 512, nt, evac_tm)
            p.barrier()
    return fm_b, tm_b


def emit_mixout(p, cx, hT_in, hT_out, mixT_dram, w_dram, nt, kcn=KC):
    with ExitStack() as st:
        mT = p.sbuf([128, kcn, nt], BF16, stack=st)
        mT_b = p.buf()
        msem = p.new_dma_sem()
        for kc in range(kcn):
            p.dma("sp", mT[:, kc, :], mixT_dram[kc * 128:(kc + 1) * 128, :], msem, writes=[mT_b])
        ws = WStream(p, 4, 128, st)
        hs = [p.sbuf([128, nt], F32, stack=st) for _ in range(2)]
        hsb = [p.buf() for _ in range(2)]
        hsem = [p.new_dma_sem() for _ in range(2)]
        osem = p.new_dma_sem()
        ob = p.buf()
        ntt = nt // 512

        def evac(m, banks):
            s = m % 2
            p.dma("pool", hs[s][:], hT_in[m * 128:(m + 1) * 128, :], hsem[s], writes=[hsb[s]])
            for tt in range(ntt):
                bk = banks[0][tt]
                p.op("dve", lambda e, s=s, bk=bk, tt=tt: e.tensor_tensor(
                    out=hs[s][:, tt * 512:(tt + 1) * 512], in0=cx.ps[bk][:],
                    in1=hs[s][:, tt * 512:(tt + 1) * 512], op=ALU.add),
                    reads=[cx.psb[bk], hsb[s]], writes=[hsb[s]])
            p.dma("sp", hT_out[m * 128:(m + 1) * 128, :], hs[s][:], osem, reads=[hsb[s]], writes=[ob])
        emit_lin_fm(p, cx, ws, mT, mT_b, kcn, w_dram, kcn, 1, nt, evac)
        p.barrier()


def emit_final_norm(p, cx, hT_in, g_sb, g_buf, gcol, outT, nt, kcn=KC):
    with ExitStack() as st:
        hs = [p.sbuf([128, nt], F32, stack=st) for _ in range(2)]
        hsb = [p.buf() for _ in range(2)]
        hsem = [p.new_dma_sem() for _ in range(2)]
        sq = [p.sbuf([128, nt], F32, stack=st) for _ in range(2)]
        sqb = [p.buf() for _ in range(2)]
        rstd = p.sbuf([128, nt], F32, stack=st)
        rstdb = p.buf()
        ones32 = p.sbuf([128, 128], F32, stack=st)
        o32b = p.buf()
        p.op("pool", lambda e: e.memset(ones32[:], 1.0), writes=[o32b])
        ntt = nt // 512
        for kc in range(kcn):
            s = kc % 2
            p.dma("sp", hs[s][:], hT_in[kc * 128:(kc + 1) * 128, :], hsem[s], writes=[hsb[s]])
            p.op("act", lambda e, s=s: e.activation(out=sq[s][:], in_=hs[s][:], func=AF.Square),
                 reads=[hsb[s]], writes=[sqb[s]])
            for tt in range(ntt):
                last = kc == kcn - 1
                p.op("pe", lambda e, s=s, tt=tt, kc=kc, last=last: e.matmul(
                    cx.ps[tt][:], lhsT=ones32[:], rhs=sq[s][:, tt * 512:(tt + 1) * 512],
                    start=(kc == 0), stop=last),
                    reads=[sqb[s], o32b], writes=[cx.psb[tt]], signal=(last or tt == ntt - 1))
        for tt in range(ntt):
            p.op("act", lambda e, tt=tt: e.activation(
                out=rstd[:, tt * 512:(tt + 1) * 512], in_=cx.ps[tt][:], func=AF.Sqrt,
                bias=RMS_EPS, scale=1.0 / (kcn * 128)), reads=[cx.psb[tt]], writes=[rstdb])
        p.op("dve", lambda e: e.reciprocal(out=rstd[:], in_=rstd[:]), reads=[rstdb], writes=[rstdb])
        osem = p.new_dma_sem()
        ob = p.buf()
        for kc in range(kcn):
            s = kc % 2
            p.dma("sp", hs[s][:], hT_in[kc * 128:(kc + 1) * 128, :], hsem[s], writes=[hsb[s]])
            p.op("dve", lambda e, s=s, kc=kc: e.scalar_tensor_tensor(
                out=hs[s][:], in0=hs[s][:], scalar=g_sb[:, gcol + kc:gcol + kc + 1], in1=rstd[:],
                op0=ALU.mult, op1=ALU.mult), reads=[hsb[s], rstdb, g_buf], writes=[hsb[s]])
            p.dma("sp", outT[kc * 128:(kc + 1) * 128, :], hs[s][:], osem, reads=[hsb[s]], writes=[ob])
        p.barrier()


NEG = -30000.0
LN16 = float(np.log(16.0))
CONST_NAMES = ["U", "Tri", "negU", "maskS", "negS", "ones", "ident"]


def host_consts():
    i = np.arange(128)[:, None]
    j = np.arange(128)[None, :]
    c = {
        "U": (i <= j),
        "Tri": (i > j),
        "negU": np.where(i <= j, 0.0, NEG),
        "maskS": (j > i),
        "negS": np.where(j > i, 0.0, NEG),
        "ones": np.ones((128, 128)),
        "ident": (i == j),
    }
    return np.ascontiguousarray(
        np.stack([c[n].astype(np.float32) for n in CONST_NAMES], axis=1))


class HConsts:
    def __init__(self, p, cdram):
        n = len(CONST_NAMES)
        self.f32 = p.sbuf([128, n, 128], F32, name="c32")
        self.bf = p.sbuf([128, n, 128], BF16, name="cbf")
        self.b = p.buf("consts")
        sem = p.new_dma_sem()
        p.dma("sp", self.f32[:], cdram[:, :, :], sem, writes=[self.b])
        p.dma("pool", self.bf[:], cdram[:, :, :], sem, writes=[self.b])

    def c32(self, name):
        return self.f32[:, CONST_NAMES.index(name), :]

    def cbf(self, name):
        return self.bf[:, CONST_NAMES.index(name), :]


def emit_memkv(p, cx, memT_dram, g_sb, g_buf, w_k_dram, w_v_dram, mkT, mkT_b, mv, mv_b, nmh=2, gcol=0):
    with ExitStack() as st:
        mnT = p.sbuf([128, KC, MEM_TOKENS], BF16, stack=st)
        mnT_b = p.buf()
        emit_rmsnorm(p, cx, memT_dram, g_sb, g_buf, gcol, mnT, mnT_b, MEM_TOKENS)
        ws = WStream(p, 4, 128, st)

        def evac_k(m, banks):
            bk = banks[0][0]
            p.op("act", lambda e, m=m, bk=bk: e.activation(out=mkT[:, m, :], in_=cx.ps[bk][:, 0:MEM_TOKENS],
                                                          func=AF.Copy),
                 reads=[cx.psb[bk]], writes=[mkT_b])
        emit_lin_fm(p, cx, ws, mnT, mnT_b, KC, w_k_dram, 2 * nmh, 1, MEM_TOKENS, evac_k)
        ws2 = WStream(p, 2 * (KC // KG), 512, st)

        def evac_v(ct, tg, bk):
            p.op("act", lambda e, ct=ct, tg=tg, bk=bk: e.activation(
                out=mv[:, tg, ct * 512:(ct + 1) * 512], in_=cx.ps[bk][:], func=AF.Copy),
                 reads=[cx.psb[bk]], writes=[mv_b])
        emit_lin_tm(p, cx, ws2, mnT, mnT_b, KC, w_v_dram, nmh // 2, 512, MEM_TOKENS, evac_v)
        p.barrier()


def emit_memattn(p, cx, hc, mqT_dram, mkT, mkT_b, mv, mv_b, mixT_out, row0, mix_b, nmh=2):
    with ExitStack() as st:
        mq = p.sbuf([128, 2 * nmh, SEQ], BF16, stack=st)
        mq_b = p.buf()
        sem = p.new_dma_sem()
        for i in range(2 * nmh):
            p.dma("sp", mq[:, i, :], mqT_dram[i * 128:(i + 1) * 128, :], sem, writes=[mq_b])
        pT = [p.sbuf([128, 2, 512], BF16, stack=st) for _ in range(2)]
        pTb = [p.buf() for _ in range(2)]
        rd = [p.sbuf([128, 512], F32, stack=st) for _ in range(2)]
        rdb = [p.buf() for _ in range(2)]
        oT = p.sbuf([128, 2 * nmh, SEQ], BF16, stack=st)
        oT_b = p.buf()
        it = 0
        for mh in range(nmh):
            for tq in range(SEQ // 512):
                s = it % 2
                it += 1
                tok = slice(tq * 512, (tq + 1) * 512)
                for mc in range(2):
                    for dc in range(2):
                        p.op("pe", lambda e, mc=mc, dc=dc, mh=mh, tok=tok: e.matmul(
                            cx.ps[mc][:], lhsT=mkT[:, mh * 2 + dc, mc * 128:(mc + 1) * 128],
                            rhs=mq[:, mh * 2 + dc, tok], start=(dc == 0), stop=(dc == 1)),
                            reads=[mkT_b, mq_b], writes=[cx.psb[mc]], signal=(dc == 1))
                    p.op("act", lambda e, mc=mc, s=s: e.activation(
                        out=pT[s][:, mc, :], in_=cx.ps[mc][:], func=AF.Exp, scale=1.0 / 16.0),
                        reads=[cx.psb[mc]], writes=[pTb[s]])
                for mc in range(2):
                    p.op("pe", lambda e, mc=mc, s=s: e.matmul(
                        cx.ps[2][:], lhsT=hc.cbf("ones"), rhs=pT[s][:, mc, :], start=(mc == 0), stop=(mc == 1)),
                        reads=[hc.b, pTb[s]], writes=[cx.psb[2]], signal=(mc == 1))
                p.op("dve", lambda e, s=s: e.reciprocal(out=rd[s][:], in_=cx.ps[2][:]),
                     reads=[cx.psb[2]], writes=[rdb[s]])
                for dvc in range(2):
                    bk = 3 + dvc
                    for mc in range(2):
                        p.op("pe", lambda e, mc=mc, dvc=dvc, mh=mh, s=s, bk=bk: e.matmul(
                            cx.ps[bk][:], lhsT=mv[:, mc, mh * 256 + dvc * 128:mh * 256 + (dvc + 1) * 128],
                            rhs=pT[s][:, mc, :], start=(mc == 0), stop=(mc == 1)),
                            reads=[mv_b, pTb[s]], writes=[cx.psb[bk]], signal=(mc == 1))
                    p.op("dve", lambda e, bk=bk, s=s, mh=mh, dvc=dvc, tok=tok: e.tensor_tensor(
                        out=oT[:, mh * 2 + dvc, tok], in0=cx.ps[bk][:], in1=rd[s][:], op=ALU.mult),
                        reads=[cx.psb[bk], rdb[s]], writes=[oT_b])
        osem = p.new_dma_sem()
        for i in range(2 * nmh):
            p.dma("sp", mixT_out[row0 + i * 128:row0 + (i + 1) * 128, :], oT[:, i, :], osem,
                  reads=[oT_b], writes=[mix_b])
        p.barrier()


def emit_mlstm(p, cx, hc, d, mixT_out, mix_b, nheads=3):
    NCH = SEQ // 128
    NI = nheads * NCH
    with ExitStack() as st:
        G = p.sbuf([128, NCH, 2 * nheads], F32, stack=st)
        gb = p.sbuf([128, 2 * nheads], F32, stack=st)
        gb15 = p.sbuf([128, 2 * nheads], F32, stack=st)
        gain = p.sbuf([128, nheads * 512], F32, stack=st)
        setup_b = p.buf()
        sem0 = p.new_dma_sem()
        p.dma("sp", G[:], d["gates"].rearrange("(c p) j -> p c j", p=128), sem0, writes=[setup_b])
        p.dma("sp", gb[:], d["gb"][:, :], sem0, writes=[setup_b])
        p.dma("sp", gain[:], d["gain"][:, :], sem0, writes=[setup_b])
        T1 = p.sbuf([128, NI], F32, stack=st)
        T2 = p.sbuf([128, NI], F32, stack=st)
        IG = p.sbuf([128, NI], F32, stack=st)
        LF = p.sbuf([128, NI], F32, stack=st)
        WB = p.sbuf([128, NI], F32, stack=st)
        DB = p.sbuf([128, NI], F32, stack=st)
        gt_b = p.buf()
        p.op("dve", lambda e: e.tensor_scalar_mul(out=gb15[:], in0=gb[:], scalar1=1.0 / 15.0),
             reads=[setup_b], writes=[gt_b])
        for h in range(nheads):
            hs_ = slice(h * NCH, (h + 1) * NCH)
            p.op("act", lambda e, h=h, hs_=hs_: e.activation(
                out=T1[:, hs_], in_=G[:, :, h], func=AF.Tanh, bias=gb15[:, h:h + 1], scale=1.0 / 15.0),
                reads=[setup_b, gt_b], writes=[gt_b])
            p.op("act", lambda e, h=h, hs_=hs_: e.activation(
                out=T2[:, hs_], in_=G[:, :, nheads + h], func=AF.Tanh,
                bias=gb15[:, nheads + h:nheads + h + 1], scale=1.0 / 15.0),
                reads=[setup_b, gt_b], writes=[gt_b])
        p.op("dve", lambda e: e.tensor_scalar_mul(out=IG[:], in0=T1[:], scalar1=15.0), reads=[gt_b], writes=[gt_b])
        p.op("act", lambda e: e.activation(out=T2[:], in_=T2[:], func=AF.Exp, scale=-15.0),
             reads=[gt_b], writes=[gt_b])
        p.op("act", lambda e: e.activation(out=T2[:], in_=T2[:], func=AF.Ln, bias=1.0),
             reads=[gt_b], writes=[gt_b])
        p.op("dve", lambda e: e.tensor_scalar_mul(out=LF[:], in0=T2[:], scalar1=-1.0), reads=[gt_b], writes=[gt_b])
        p.op("pe", lambda e: e.matmul(cx.ps[7][:, 0:NI], lhsT=hc.c32("U"), rhs=LF[:], start=True, stop=True),
             reads=[gt_b, hc.b], writes=[cx.psb[7]])
        p.op("dve", lambda e: e.tensor_tensor(out=WB[:], in0=IG[:], in1=cx.ps[7][:, 0:NI], op=ALU.subtract),
             reads=[gt_b, cx.psb[7]], writes=[gt_b])
        p.op("dve", lambda e: e.tensor_scalar_add(out=DB[:], in0=WB[:], scalar1=-LN16), reads=[gt_b], writes=[gt_b])

        qTs = p.sbuf([128, 2, SEQ], BF16, stack=st)
        kTs = p.sbuf([128, 2, SEQ], BF16, stack=st)
        ktm = p.sbuf([128, NCH, 256], BF16, stack=st)
        V = p.sbuf([128, NCH, 512], BF16, stack=st)
        O = p.sbuf([128, NCH, 512], F32, stack=st)
        hd_b = p.buf()
        hsem = p.new_dma_sem()
        C32 = p.sbuf([128, 2, 512], F32, stack=st)
        Cbf = p.sbuf([128, 2, 512], BF16, stack=st)
        n32 = p.sbuf([128, 2], F32, stack=st)
        nbf = p.sbuf([128, 2], BF16, stack=st)
        C32_b, Cbf_b, n32_b, nbf_b = p.buf(), p.buf(), p.buf(), p.buf()
        mTh = p.sbuf([128, 4, SEQ], BF16, stack=st)
        mTh_b = p.buf()
        osem = p.new_dma_sem()
        lfb = p.sbuf([128, 128], F32, stack=st); lfb_b = p.buf()
        bl = p.sbuf([128, 1], F32, stack=st); bl_b = p.buf()
        tt_ = p.sbuf([128, 128], F32, stack=st); tt_b = p.buf()
        DT = p.sbuf([128, 128], F32, stack=st); DT_b = p.buf()
        EB = p.sbuf([128, 128], F32, stack=st); EB_b = p.buf()
        qp = p.sbuf([128, 2, 128], BF16, stack=st); qp_b = p.buf()
        ST = p.sbuf([128, 128], BF16, stack=st); ST_b = p.buf()
        w_ = p.sbuf([128, 1], F32, stack=st); w_b = p.buf()
        eL = p.sbuf([128, 1], F32, stack=st); eL_b = p.buf()
        kw = p.sbuf([128, 256], BF16, stack=st); kw_b = p.buf()
        den = p.sbuf([128, 1], F32, stack=st); den_b = p.buf()
        hn = p.sbuf([128, 512], F32, stack=st); hn_b = p.buf()
        junk = p.sbuf([128, 512], F32, stack=st); junk_b = p.buf()
        ss = p.sbuf([128, 1], F32, stack=st); ss_b = p.buf()
        y = p.sbuf([128, 512], F32, stack=st); y_b = p.buf()
        sig = p.sbuf([128, 512], F32, stack=st); sig_b = p.buf()
        y2 = p.sbuf([128, 512], BF16, stack=st); y2_b = p.buf()
        ps3n_b = p.buf()
        ones_c = hc.cbf("ones")[:, 0:1]
        P = cx.ps
        B = cx.psb

        for h in range(nheads):
            for dc in range(2):
                r0 = h * 256 + dc * 128
                p.dma("sp", qTs[:, dc, :], d["qT"][r0:r0 + 128, :], hsem, writes=[hd_b])
                p.dma("sp", kTs[:, dc, :], d["kT"][r0:r0 + 128, :], hsem, writes=[hd_b])
            p.dma("pool", ktm[:], d["k_tm"][:, h * 256:(h + 1) * 256].rearrange("(c p) d -> p c d", p=128),
                  hsem, writes=[hd_b])
            p.dma("pool", V[:], d["v_tm"][:, h * 512:(h + 1) * 512].rearrange("(c p) d -> p c d", p=128),
                  hsem, writes=[hd_b])
            p.dma("sp", O[:], d["o_tm"][:, h * 512:(h + 1) * 512].rearrange("(c p) d -> p c d", p=128),
                  hsem, writes=[hd_b])
            p.op("pool", lambda e: e.memset(C32[:], 0.0), writes=[C32_b])
            p.op("pool", lambda e: e.memset(Cbf[:], 0.0), writes=[Cbf_b])
            p.op("pool", lambda e: e.memset(n32[:], 0.0), writes=[n32_b])
            p.op("pool", lambda e: e.memset(nbf[:], 0.0), writes=[nbf_b])
            for c in range(NCH):
                idx = h * NCH + c
                cols = slice(c * 128, (c + 1) * 128)
                p.op("pool", lambda e, idx=idx: e.tensor_scalar_mul(
                    out=lfb[:], in0=hc.c32("ones"), scalar1=LF[:, idx:idx + 1]),
                    reads=[gt_b, hc.b], writes=[lfb_b])
                p.op("pe", lambda e: e.matmul(P[0][:, 0:128], lhsT=lfb[:], rhs=hc.c32("U"), start=True, stop=True),
                     reads=[lfb_b, hc.b], writes=[B[0]])
                p.op("dve", lambda e: e.tensor_copy(out=bl[:], in_=P[0][:, 127:128]), reads=[B[0]], writes=[bl_b])
                p.op("dve", lambda e: e.tensor_tensor(out=tt_[:], in0=P[0][:, 0:128], in1=hc.c32("negU"),
                                                      op=ALU.add), reads=[B[0], hc.b], writes=[tt_b])
                p.op("act", lambda e, idx=idx: e.activation(out=DT[:], in_=tt_[:], func=AF.Exp,
                                                            bias=DB[:, idx:idx + 1]),
                     reads=[tt_b, gt_b], writes=[DT_b])
                p.op("act", lambda e: e.activation(out=EB[:], in_=P[0][:, 0:128], func=AF.Exp, bias=-LN16),
                     reads=[B[0]], writes=[EB_b])
                for dc in range(2):
                    p.op("dve", lambda e, dc=dc, cols=cols: e.tensor_tensor(
                        out=qp[:, dc, :], in0=qTs[:, dc, cols], in1=EB[:], op=ALU.mult),
                        reads=[hd_b, EB_b], writes=[qp_b])
                for dc in range(2):
                    p.op("pe", lambda e, dc=dc, cols=cols: e.matmul(
                        P[1][:, 0:128], lhsT=kTs[:, dc, cols], rhs=qTs[:, dc, cols], start=(dc == 0), stop=(dc == 1)),
                        reads=[hd_b], writes=[B[1]], signal=(dc == 1))
                p.op("dve", lambda e: e.tensor_tensor(out=ST[:], in0=P[1][:, 0:128], in1=DT[:], op=ALU.mult),
                     reads=[B[1], DT_b], writes=[ST_b])
                p.op("pe", lambda e, c=c: e.matmul(P[2][:], lhsT=ST[:], rhs=V[:, c, :], start=True, stop=False),
                     reads=[ST_b, hd_b], writes=[B[2]], signal=False)
                for dc in range(2):
                    p.op("pe", lambda e, dc=dc: e.matmul(P[2][:], lhsT=qp[:, dc, :], rhs=Cbf[:, dc, :],
                                                         start=False, stop=(dc == 1)),
                         reads=[qp_b, Cbf_b], writes=[B[2]], signal=(dc == 1))
                p.op("pe", lambda e: e.matmul(P[3][:, 0:1], lhsT=ST[:], rhs=ones_c, start=True, stop=False),
                     reads=[ST_b, hc.b], writes=[B[3]], signal=False)
                for dc in range(2):
                    p.op("pe", lambda e, dc=dc: e.matmul(P[3][:, 0:1], lhsT=qp[:, dc, :], rhs=nbf[:, dc:dc + 1],
                                                         start=False, stop=(dc == 1)),
                         reads=[qp_b, nbf_b], writes=[B[3]], signal=(dc == 1))
                p.op("act", lambda e, idx=idx: e.activation(out=w_[:], in_=WB[:, idx:idx + 1], func=AF.Exp,
                                                            bias=bl[:, 0:1]),
                     reads=[gt_b, bl_b], writes=[w_b])
                p.op("act", lambda e: e.activation(out=eL[:], in_=bl[:], func=AF.Exp), reads=[bl_b], writes=[eL_b])
                p.op("dve", lambda e, c=c: e.tensor_scalar_mul(out=kw[:], in0=ktm[:, c, :], scalar1=w_[:, 0:1]),
                     reads=[hd_b, w_b], writes=[kw_b])
                for dc in range(2):
                    p.op("pe", lambda e, dc=dc, c=c: e.matmul(
                        P[4 + dc][:], lhsT=kw[:, dc * 128:(dc + 1) * 128], rhs=V[:, c, :], start=True, stop=True),
                        reads=[kw_b, hd_b], writes=[B[4 + dc]])
                    p.op("pe", lambda e, dc=dc: e.matmul(
                        P[3][:, 1 + dc:2 + dc], lhsT=kw[:, dc * 128:(dc + 1) * 128], rhs=ones_c,
                        start=True, stop=True), reads=[kw_b, hc.b], writes=[ps3n_b])
                p.op("act", lambda e: e.activation(out=den[:], in_=P[3][:, 0:1], func=AF.Abs),
                     reads=[B[3]], writes=[den_b])
                p.op("dve", lambda e: e.tensor_scalar_max(out=den[:], in0=den[:], scalar1=1.0),
                     reads=[den_b], writes=[den_b])
                p.op("dve", lambda e: e.reciprocal(out=den[:], in_=den[:]), reads=[den_b], writes=[den_b])
                p.op("act", lambda e: e.activation(out=hn[:], in_=P[2][:], func=AF.Copy, scale=den[:, 0:1]),
                     reads=[B[2], den_b], writes=[hn_b])
                p.op("act", lambda e: e.activation(out=junk[:], in_=hn[:], func=AF.Square, accum_out=ss[:, 0:1]),
                     reads=[hn_b], writes=[junk_b, ss_b])
                p.op("act", lambda e: e.activation(out=ss[:], in_=ss[:], func=AF.Sqrt, bias=RMS_EPS,
                                                   scale=1.0 / 512.0), reads=[ss_b], writes=[ss_b])
                p.op("dve", lambda e: e.reciprocal(out=ss[:], in_=ss[:]), reads=[ss_b], writes=[ss_b])
                p.op("dve", lambda e, h=h: e.scalar_tensor_tensor(
                    out=y[:], in0=hn[:], scalar=ss[:, 0:1], in1=gain[:, h * 512:(h + 1) * 512],
                    op0=ALU.mult, op1=ALU.mult), reads=[hn_b, ss_b, setup_b], writes=[y_b])
                p.op("act", lambda e, c=c: e.activation(out=sig[:], in_=O[:, c, :], func=AF.Sigmoid),
                     reads=[hd_b], writes=[sig_b])
                p.op("pool", lambda e: e.tensor_tensor(out=y2[:], in0=y[:], in1=sig[:], op=ALU.mult),
                     reads=[y_b, sig_b], writes=[y2_b])
                for blk in range(4):
                    p.op("pe", lambda e, blk=blk: e.matmul(
                        P[6][:, blk * 128:(blk + 1) * 128], lhsT=y2[:, blk * 128:(blk + 1) * 128],
                        rhs=hc.cbf("ident"), start=True, stop=True),
                        reads=[y2_b, hc.b], writes=[B[6]], signal=(blk == 3))
                p.op("act", lambda e, cols=cols: e.activation(
                    out=mTh[:, :, cols], in_=P[6][:].rearrange("p (b j) -> p b j", b=4), func=AF.Copy),
                    reads=[B[6]], writes=[mTh_b])
                for dc in range(2):
                    p.op("dve", lambda e, dc=dc: e.scalar_tensor_tensor(
                        out=C32[:, dc, :], in0=C32[:, dc, :], scalar=eL[:, 0:1], in1=P[4 + dc][:],
                        op0=ALU.mult, op1=ALU.add), reads=[C32_b, eL_b, B[4 + dc]], writes=[C32_b])
                p.op("pool", lambda e: e.tensor_copy(out=Cbf[:], in_=C32[:]), reads=[C32_b], writes=[Cbf_b])
                p.op("dve", lambda e: e.scalar_tensor_tensor(
                    out=n32[:], in0=n32[:], scalar=eL[:, 0:1], in1=P[3][:, 1:3], op0=ALU.mult, op1=ALU.add),
                    reads=[n32_b, eL_b, ps3n_b], writes=[n32_b])
                p.op("dve", lambda e: e.tensor_copy(out=nbf[:], in_=n32[:]), reads=[n32_b], writes=[nbf_b])
            for blk in range(4):
                r0 = h * 512 + blk * 128
                p.dma("sp", mixT_out[r0:r0 + 128, :], mTh[:, blk, :], osem, reads=[mTh_b], writes=[mix_b])
        p.barrier()


def emit_sb(p, cx, hc, d, mixT_out, mix_b, nheads=12):
    SC = 128 ** -0.5
    NQ = SEQ // 512
    P, B = cx.ps, cx.psb
    with ExitStack() as st:
        NH = 3
        qT = [p.sbuf([128, SEQ], BF16, stack=st) for _ in range(NH)]
        kT = [p.sbuf([128, SEQ], BF16, stack=st) for _ in range(NH)]
        V = [p.sbuf([128, SEQ // 128, 128], BF16, stack=st) for _ in range(NH)]
        hd_b = [p.buf() for _ in range(NH)]
        hsem = [p.new_dma_sem() for _ in range(NH)]
        oT = [p.sbuf([128, SEQ], BF16, stack=st) for _ in range(2)]
        oT_b = [p.buf() for _ in range(2)]
        osem = p.new_dma_sem()
        NS = 4
        ex = [p.sbuf([128, 512], F32, stack=st) for _ in range(NS)]; ex_b = [p.buf() for _ in range(NS)]
        l_ = [p.sbuf([128, 512], F32, stack=st) for _ in range(NS)]; l_b = [p.buf() for _ in range(NS)]
        zl = [p.sbuf([128, 512], F32, stack=st) for _ in range(NS)]; zl_b = [p.buf() for _ in range(NS)]
        AT = [p.sbuf([128, 512], BF16, stack=st) for _ in range(NS)]; AT_b = [p.buf() for _ in range(NS)]
        R = [p.sbuf([128, 512], F32, stack=st) for _ in range(2)]; R_b = [p.buf() for _ in range(2)]
        zeros = p.sbuf([128, 128], BF16, stack=st); z_b = p.buf()
        p.op("pool", lambda e: e.memset(zeros[:], 0.0), writes=[z_b])

        steps = []
        k = 0
        for h in range(nheads):
            for Q in range(NQ):
                amax = 4 * Q + 3
                for a in range(amax, -1, -1):
                    c0 = max(0, 128 * a - 512 * Q)
                    steps.append(dict(k=k, h=h, hs=h % NH, Q=Q, a=a, c0=c0, diag=(a >= 4 * Q), first=(a == amax),
                                      last=(a == 0), s=k % NS, zb=k % 2, pb=2 + k % 2, ob=6 + (Q % 2),
                                      rq=(h * NQ + Q) % 2))
                    k += 1
        n = len(steps)

        def load_head(h):
            hs = h % NH
            r0 = h * 128
            p.dma("sp", qT[hs][:], d["qT"][r0:r0 + 128, :], hsem[hs], writes=[hd_b[hs]])
            p.dma("sp", kT[hs][:], d["kT"][r0:r0 + 128, :], hsem[hs], writes=[hd_b[hs]])
            p.dma("pool", V[hs][:], d["v_tm"][:, r0:r0 + 128].rearrange("(c p) d -> p c d", p=128),
                  hsem[hs], writes=[hd_b[hs]])

        def S1(t):
            s, zb, hs, a, c0 = t["s"], t["zb"], t["hs"], t["a"], t["c0"]
            cs = slice(c0, 512)
            qs = slice(512 * t["Q"] + c0, 512 * t["Q"] + 512)
            if t["first"]:
                if t["Q"] == 0 and t["h"] + 2 < nheads:
                    load_head(t["h"] + 2)
                ob, rq = t["ob"], t["rq"]
                p.op("pe", lambda e, ob=ob, hs=hs: e.matmul(P[ob][:], lhsT=zeros[:], rhs=qT[hs][:, 0:512],
                                                            start=True, stop=False),
                     reads=[z_b, hd_b[hs]], writes=[B[ob]], signal=False)
                p.op("pool", lambda e, rq=rq: e.memset(R[rq][:], 0.0), writes=[R_b[rq]])
            p.op("pe", lambda e: e.matmul(P[zb][:, cs], lhsT=kT[hs][:, a * 128:(a + 1) * 128], rhs=qT[hs][:, qs],
                                          start=True, stop=True), reads=[hd_b[hs]], writes=[B[zb]])
            p.op("act", lambda e: e.activation(out=ex[s][:, cs], in_=P[zb][:, cs], func=AF.Exp, scale=SC),
                 reads=[B[zb]], writes=[ex_b[s]])
            p.op("act", lambda e: e.activation(out=l_[s][:, cs], in_=ex[s][:, cs], func=AF.Ln, bias=1.0),
                 reads=[ex_b[s]], writes=[l_b[s]])
            if t["diag"]:
                p.op("pool", lambda e: e.tensor_tensor(out=l_[s][:, c0:c0 + 128], in0=l_[s][:, c0:c0 + 128],
                                                       in1=hc.c32("maskS"), op=ALU.mult),
                     reads=[l_b[s], hc.b], writes=[l_b[s]])

        def S2(t):
            s, zb, pb, c0, rq = t["s"], t["zb"], t["pb"], t["c0"], t["rq"]
            cs = slice(c0, 512)
            first = t["first"]
            p.op("dve", lambda e: e.scalar_tensor_tensor(out=zl[s][:, cs], in0=P[zb][:, cs], scalar=SC,
                                                         in1=l_[s][:, cs], op0=ALU.mult, op1=ALU.subtract),
                 reads=[B[zb], l_b[s]], writes=[zl_b[s]])
            p.op("pe", lambda e: e.matmul(P[pb][:, cs], lhsT=hc.c32("Tri"), rhs=l_[s][:, cs], start=True, stop=first),
                 reads=[hc.b, l_b[s]], writes=[B[pb]], signal=first)
            if not first:
                p.op("pe", lambda e: e.matmul(P[pb][:, cs], lhsT=hc.c32("ones"), rhs=R[rq][:, cs],
                                              start=False, stop=True), reads=[hc.b, R_b[rq]], writes=[B[pb]])
            if not t["last"]:
                p.op("pool", lambda e: e.tensor_tensor(out=R[rq][:, cs], in0=R[rq][:, cs], in1=l_[s][:, cs],
                                                       op=ALU.add), reads=[R_b[rq], l_b[s]], writes=[R_b[rq]])
            p.op("dve", lambda e: e.tensor_tensor(out=zl[s][:, cs], in0=zl[s][:, cs], in1=P[pb][:, cs],
                                                  op=ALU.subtract), reads=[zl_b[s], B[pb]], writes=[zl_b[s]])
            if t["diag"]:
                p.op("pool", lambda e: e.tensor_tensor(out=zl[s][:, c0:c0 + 128], in0=zl[s][:, c0:c0 + 128],
                                                       in1=hc.c32("negS"), op=ALU.add),
                     reads=[zl_b[s], hc.b], writes=[zl_b[s]])

        def S3(t):
            s, hs, a, c0, ob, Q, h = t["s"], t["hs"], t["a"], t["c0"], t["ob"], t["Q"], t["h"]
            cs = slice(c0, 512)
            last = t["last"]
            p.op("act", lambda e: e.activation(out=AT[s][:, cs], in_=zl[s][:, cs], func=AF.Exp),
                 reads=[zl_b[s]], writes=[AT_b[s]])
            p.op("pe", lambda e: e.matmul(P[ob][:, cs], lhsT=V[hs][:, a, :], rhs=AT[s][:, cs], start=False, stop=last),
                 reads=[hd_b[hs], AT_b[s]], writes=[B[ob]])
            if last:
                oh = h % 2
                p.op("act", lambda e: e.activation(out=oT[oh][:, Q * 512:(Q + 1) * 512], in_=P[ob][:], func=AF.Copy),
                     reads=[B[ob]], writes=[oT_b[oh]])
                if Q == NQ - 1:
                    r0 = h * 128
                    p.dma("sp", mixT_out[r0:r0 + 128, :], oT[oh][:], osem, reads=[oT_b[oh]], writes=[mix_b])

        for h0 in range(min(2, nheads)):
            load_head(h0)
        for i in range(n + 2):
            if i < n:
                S1(steps[i])
            if 0 <= i - 1 < n:
                S2(steps[i - 1])
            if 0 <= i - 2 < n:
                S3(steps[i - 2])
        p.barrier()


FCN = D_FF // 128
A_FM, A_TM = 32, 16
B_FM, B_TM = 56, 6
N_NORMS = 2 + 3 * DEPTH
NTP = SEQ // NT


def build_fused():
    p = Prog()
    EI, EO = "ExternalInput", "ExternalOutput"
    consts = p.dram("consts", [128, len(CONST_NAMES), 128], F32, kind=EI)
    xT = p.dram("xT", [D_MODEL, SEQ], F32, kind=EI)
    memT = p.dram("memT", [D_MODEL, MEM_TOKENS], F32, kind=EI)
    g_d = p.dram("g", [128, N_NORMS * KC], F32, kind=EI)
    w_mk = p.dram("w_mk", [8, KC // KG, 128, KG, 128], F32, kind=EI)
    w_mv = p.dram("w_mv", [2, KC // KG, 128, KG, 512], F32, kind=EI)
    W = []
    for i in range(DEPTH):
        n_fm, n_tm = (A_FM, A_TM) if i % 2 == 0 else (B_FM, B_TM)
        W.append({
            "w1_in": p.dram(f"w1_in{i}", [FCN, KC // KG, 128, KG, 256], F32, kind=EI),
            "w1_out": p.dram(f"w1_out{i}", [KC, FCN // KG, 128, KG, 128], F32, kind=EI),
            "w_fm": p.dram(f"w_fm{i}", [n_fm, KC // KG, 128, KG, 128], F32, kind=EI),
            "w_tm": p.dram(f"w_tm{i}", [n_tm, KC // KG, 128, KG, 512], F32, kind=EI),
            "w_mo": p.dram(f"w_mo{i}", [KC, KC // KG, 128, KG, 128], F32, kind=EI),
            "w2_in": p.dram(f"w2_in{i}", [FCN, KC // KG, 128, KG, 256], F32, kind=EI),
            "w2_out": p.dram(f"w2_out{i}", [KC, FCN // KG, 128, KG, 128], F32, kind=EI),
        })
    gbs = [p.dram(f"gb{j}", [128, 12], F32, kind=EI) for j in range(2)]
    gains = [p.dram(f"gain{j}", [128, 3072], F32, kind=EI) for j in range(2)]
    outT = p.dram("outT", [D_MODEL, SEQ], F32, kind=EO)
    hs = [[p.dram(f"hs{tp}_{k}", [D_MODEL, NT], F32) for k in range(3)] for tp in range(NTP)]
    fmT = {"A": p.dram("fmT_A", [A_FM * 128, SEQ], BF16), "B": p.dram("fmT_B", [B_FM * 128, SEQ], BF16)}
    tm = {"A": p.dram("tm_A", [SEQ, A_TM * 512], F32), "B": p.dram("tm_B", [SEQ, B_TM * 512], F32)}
    mixT = p.dram("mixT", [D_MODEL, SEQ], BF16)
    mk_d = p.dram("mk_d", [128, 8 * MEM_TOKENS], BF16)
    mv_d = p.dram("mv_d", [128, 2 * 1024], BF16)

    cx = Ctx(p, consts)
    hc = HConsts(p, consts)
    g_sb = p.sbuf([128, N_NORMS * KC], F32)
    g_b = p.buf()
    p.dma("sp", g_sb[:], g_d[:, :], p.new_dma_sem(), writes=[g_b])

    with ExitStack() as st:
        mkT = p.sbuf([128, 8, MEM_TOKENS], BF16, stack=st)
        mv = p.sbuf([128, 2, 1024], BF16, stack=st)
        mkT_b, mv_b = p.buf(), p.buf()
        emit_memkv(p, cx, memT, g_sb, g_b, w_mk, w_mv, mkT, mkT_b, mv, mv_b, nmh=4, gcol=0)
        sem = p.new_dma_sem()
        p.dma("sp", mk_d[:, :], mkT[:].rearrange("p a b -> p (a b)"), sem, reads=[mkT_b])
        p.dma("sp", mv_d[:, :], mv[:].rearrange("p a b -> p (a b)"), sem, reads=[mv_b])
        p.barrier()

    def tsl(tp):
        return slice(tp * NT, (tp + 1) * NT)

    cur = [None] * NTP
    for i in range(DEPTH + 1):
        mx = ("A" if i % 2 == 0 else "B") if i < DEPTH else None
        for tp in range(NTP):
            if i == 0:
                src = xT[:, tsl(tp)]
                ci = 0
            else:
                ci = cur[tp]
                src = hs[tp][ci]
                a, b = (ci + 1) % 3, (ci + 2) % 3
                emit_mixout(p, cx, src, hs[tp][a], mixT[:, tsl(tp)], W[i - 1]["w_mo"], NT)
                emit_ffn(p, cx, hs[tp][a], hs[tp][b], g_sb, g_b, (3 * (i - 1) + 3) * KC,
                         W[i - 1]["w2_in"], W[i - 1]["w2_out"], NT)
                src = hs[tp][b]
            if i < DEPTH:
                emit_ffn(p, cx, src, hs[tp][ci], g_sb, g_b, (3 * i + 1) * KC, W[i]["w1_in"], W[i]["w1_out"], NT)
                cur[tp] = ci
                n_fm, n_tm = (A_FM, A_TM) if mx == "A" else (B_FM, B_TM)
                emit_inproj(p, cx, hs[tp][ci], g_sb, g_b, (3 * i + 2) * KC, W[i]["w_fm"], n_fm,
                            fmT[mx][:, tsl(tp)], W[i]["w_tm"], n_tm, tm[mx][tsl(tp), :], NT)
            else:
                emit_final_norm(p, cx, src, g_sb, g_b, (N_NORMS - 1) * KC, outT[:, tsl(tp)], NT)
        if i == DEPTH:
            break
        mix_b = p.buf()
        with ExitStack() as st:
            mkT = p.sbuf([128, 8, MEM_TOKENS], BF16, stack=st)
            mv = p.sbuf([128, 2, 1024], BF16, stack=st)
            mkT_b, mv_b = p.buf(), p.buf()
            sem = p.new_dma_sem()
            p.dma("sp", mkT[:].rearrange("p a b -> p (a b)"), mk_d[:, :], sem, writes=[mkT_b])
            p.dma("sp", mv[:].rearrange("p a b -> p (a b)"), mv_d[:, :], sem, writes=[mv_b])
            f, t = fmT[mx], tm[mx]
            if mx == "A":
                emit_memattn(p, cx, hc, f[3072:4096, :], mkT, mkT_b, mv, mv_b, mixT, 3072, mix_b, nmh=4)
                d = {"qT": f[0:1536, :], "kT": f[1536:3072, :], "k_tm": t[:, 0:1536], "v_tm": t[:, 1536:4608],
                     "o_tm": t[:, 4608:7680], "gates": t[:, 7680:7692], "gb": gbs[i // 2], "gain": gains[i // 2]}
                emit_mlstm(p, cx, hc, d, mixT, mix_b, nheads=6)
            else:
                emit_memattn(p, cx, hc, f[6144:7168, :], mkT, mkT_b, mv, mv_b, mixT, 3072, mix_b, nmh=4)
                d = {"qT": f[0:3072, :], "kT": f[3072:6144, :], "v_tm": t[:, 0:3072]}
                emit_sb(p, cx, hc, d, mixT, mix_b, nheads=24)
    return p.finish()


_PROG = {}


def _glay(*norms):
    return np.ascontiguousarray(np.concatenate([np.asarray(n, np.float32).reshape(KC, 128).T for n in norms], axis=1))


def _tile_ffn(w_in, w_out):
    wi = tile_w(w_in, 256, [[(j * 128, 128), (D_FF + j * 128, 128)] for j in range(FCN)])
    wo = tile_w(w_out, 128, [[(m * 128, 128)] for m in range(KC)])
    return wi, wo


def _tile_inproj(w, mixer):
    if mixer == "A":
        fm_cols = [(c, 128) for c in range(0, 3072, 128)] + [(9228 + c, 128) for c in range(0, 1024, 128)]
        tm_cols = [[(1536 + c, 512)] for c in range(0, 1536, 512)] + \
                  [[(3072 + c, 512)] for c in range(0, 6144, 512)] + [[(9216, 12)]]
    else:
        fm_cols = [(c, 128) for c in range(0, 6144, 128)] + [(9216 + c, 128) for c in range(0, 1024, 128)]
        tm_cols = [[(6144 + c, 512)] for c in range(0, 3072, 512)]
    return tile_w(w, 128, [[c] for c in fm_cols]), tile_w(w, 512, tm_cols)


def kernel(**inputs):
    f32 = np.float32
    x = np.asarray(inputs["x"], f32)
    mem = np.asarray(inputs["mem"], f32)
    shared = {"consts": host_consts()}
    norms = [inputs["mem_norm"]]
    for i in range(DEPTH):
        norms += [inputs["norm_ffn1"][i], inputs["norm_mix"][i], inputs["norm_ffn2"][i]]
    norms.append(inputs["norm_final"])
    shared["g"] = _glay(*[np.asarray(n, f32) for n in norms])
    wkv = np.asarray(inputs["w_mem_kv"], f32)
    shared["w_mk"] = tile_w(wkv[:, 0:1024], 128, [[(i * 128, 128)] for i in range(8)])
    shared["w_mv"] = tile_w(wkv[:, 1024:2048], 512, [[(0, 512)], [(512, 512)]])
    for i in range(DEPTH):
        shared[f"w1_in{i}"], shared[f"w1_out{i}"] = _tile_ffn(np.asarray(inputs["ffn1_w_in"][i], f32),
                                                              np.asarray(inputs["ffn1_w_out"][i], f32))
        shared[f"w2_in{i}"], shared[f"w2_out{i}"] = _tile_ffn(np.asarray(inputs["ffn2_w_in"][i], f32),
                                                              np.asarray(inputs["ffn2_w_out"][i], f32))
        w = np.asarray(inputs["a_w_in" if i % 2 == 0 else "b_w_in"][i // 2], f32)
        shared[f"w_fm{i}"], shared[f"w_tm{i}"] = _tile_inproj(w, "A" if i % 2 == 0 else "B")
        shared[f"w_mo{i}"] = tile_w(np.asarray(inputs["w_out"][i], f32), 128, [[(m * 128, 128)] for m in range(KC)])
    for j in range(2):
        gb = np.concatenate([np.asarray(inputs["a_b_igate"], f32)[j], np.asarray(inputs["a_b_fgate"], f32)[j]])
        shared[f"gb{j}"] = np.ascontiguousarray(np.broadcast_to(gb[None, :], (128, 12)))
        shared[f"gain{j}"] = np.ascontiguousarray(
            np.broadcast_to(np.asarray(inputs["a_head_gain"], f32)[j][None, :], (128, 3072)))
    maps = []
    for c in range(NCORES):
        b = c // 2
        m = dict(shared)
        m["xT"] = np.ascontiguousarray(x[b].T)
        m["memT"] = np.ascontiguousarray(mem[b].T)
        maps.append(m)
    if "nc" not in _PROG:
        _PROG["nc"] = build_fused()
    res = run_bass_kernel_spmd(_PROG["nc"], maps, core_ids=list(range(NCORES))).results
    del maps, shared
    out = np.empty((BATCH, SEQ, D_MODEL), f32)
    for b in range(BATCH):
        out[b, :NT] = res[2 * b]["outT"][:, :NT].T
        out[b, NT:] = res[2 * b + 1]["outT"][:, NT:].T
    return out
```

```python
import numpy as np
import ml_dtypes
from contextlib import ExitStack
import concourse.bass as bass
import concourse.mybir as mybir
from concourse.bass_utils import run_bass_kernel_spmd

F32 = mybir.dt.float32
BF16 = mybir.dt.bfloat16
AF = mybir.ActivationFunctionType
ALU = mybir.AluOpType

D_MODEL = 4096
BATCH = 4
SEQ = 2048
DEPTH = 4
MEM_TOKENS = 256
D_FF = 6144
RMS_EPS = 1e-6
NCORES = 8
NT = 1024
KC = D_MODEL // 128


class Buf:
    __slots__ = ("name", "last_write", "reads")

    def __init__(self, name):
        self.name = name
        self.last_write = None
        self.reads = []


class Prog:
    ENGS = ("pe", "act", "dve", "pool", "sp")

    def __init__(self):
        self.nc = bass.Bass("TRN2", target_bir_lowering=False)
        self.es = ExitStack()
        self.ops = {e: [] for e in self.ENGS}
        self.count = {e: 0 for e in self.ENGS}
        self.sem = {}
        for e in ("pe", "act", "dve", "pool"):
            self.sem[e] = self.es.enter_context(self.nc.semaphore("sem_" + e))
        self.seen = {}
        self.dma_sems = []
        self.free_dma_sems = []
        self.live_dma_sems = []
        self.nbuf = 0
        self.tensors = 0

    def sbuf(self, shape, dtype, name=None, stack=None):
        self.tensors += 1
        name = name or f"sb{self.tensors}"
        t = (stack or self.es).enter_context(self.nc.sbuf_tensor(name, list(shape), dtype))
        return t

    def psum(self, shape, dtype, name=None, stack=None):
        self.tensors += 1
        name = name or f"ps{self.tensors}"
        t = (stack or self.es).enter_context(self.nc.psum_tensor(name, list(shape), dtype))
        return t

    def dram(self, name, shape, dtype, kind=None):
        if kind is None:
            return self.nc.dram_tensor(name, list(shape), dtype)
        return self.nc.dram_tensor(name, list(shape), dtype, kind=kind)

    def buf(self, name=None):
        self.nbuf += 1
        return Buf(name or f"b{self.nbuf}")

    def new_dma_sem(self):
        if self.free_dma_sems:
            ent = self.free_dma_sems.pop()
        else:
            s = self.es.enter_context(self.nc.semaphore(f"dsem{len(self.dma_sems)}"))
            ent = [s, 0]
            self.dma_sems.append(ent)
        self.live_dma_sems.append(ent)
        return ent

    def _wait(self, eng, tok):
        if tok is None:
            return
        kind, key, val = tok
        if kind == "eng":
            if key == eng and eng == "pe":
                return
            sem = self.sem[key]
            skey = (eng, "e" + key)
        else:
            sem = key[0]
            skey = (eng, id(key))
        if self.seen.get(skey, 0) >= val:
            return
        self.seen[skey] = val
        self.ops[eng].append(lambda e, sem=sem, val=val: e.wait_ge(sem, val))

    def _deps(self, eng, reads, writes, dsem=None):
        toks = []
        for b in reads:
            toks.append(b.last_write)
        for b in writes:
            lw = b.last_write
            if not (dsem is not None and lw is not None and lw[0] == "dma" and lw[1] is dsem):
                toks.append(lw)
            toks.extend(b.reads)
        for t in toks:
            self._wait(eng, t)

    def _commit(self, tok, reads, writes):
        for b in reads:
            b.reads.append(tok)
        for b in writes:
            b.last_write = tok
            b.reads = []

    def op(self, eng, fn, reads=(), writes=(), signal=True):
        self._deps(eng, reads, writes)
        if signal:
            self.count[eng] += 1
            n = self.count[eng]
            sem = self.sem[eng]
            self.ops[eng].append(lambda e, fn=fn, sem=sem: fn(e).then_inc(sem, 1))
        else:
            n = self.count[eng] + 1
            self.ops[eng].append(lambda e, fn=fn: fn(e))
        self._commit(("eng", eng, n), reads, writes)

    def dma(self, eng, out, in_, dsem, reads=(), writes=(), **kw):
        self._deps(eng, reads, writes, dsem)
        dsem[1] += 16
        sem = dsem[0]
        self.ops[eng].append(
            lambda e, out=out, in_=in_, sem=sem, kw=kw: e.dma_start(out=out, in_=in_, **kw).then_inc(sem, 16))
        self._commit(("dma", dsem, dsem[1]), reads, writes)

    def barrier(self):
        for eng in self.ENGS:
            for other in ("pe", "act", "dve", "pool"):
                if other != eng and self.count[other] > 0:
                    self._wait(eng, ("eng", other, self.count[other]))
            for ent in self.dma_sems:
                if ent[1] > 0:
                    self._wait(eng, ("dma", ent, ent[1]))
        self.free_dma_sems.extend(self.live_dma_sems)
        self.live_dma_sems = []

    def finish(self):
        self.barrier()
        nc = self.nc
        with nc.Block() as block:
            @block.tensor
            def _(e):
                for f in self.ops["pe"]:
                    f(e)

            @block.scalar
            def _(e):
                for f in self.ops["act"]:
                    f(e)

            @block.vector
            def _(e):
                for f in self.ops["dve"]:
                    f(e)

            @block.gpsimd
            def _(e):
                for f in self.ops["pool"]:
                    f(e)

            @block.sync
            def _(e):
                for f in self.ops["sp"]:
                    f(e)
        self.es.close()
        return nc


KG = 8
CONST_NAMES_IDX_ONES = 5


class Ctx:
    def __init__(self, p, consts_dram):
        self.p = p
        nc = p.nc
        self.ps = [p.psum([128, 512], F32, name=f"psb{i}") for i in range(8)]
        self.psb = [p.buf(f"psb{i}") for i in range(8)]
        self.ones_bf = p.sbuf([128, 128], BF16, name="ones_bf")
        self.ones_bf_b = p.buf("ones_bf")
        self.csem = p.new_dma_sem()
        p.dma("pool", self.ones_bf[:], consts_dram[:, CONST_NAMES_IDX_ONES, :], self.csem, writes=[self.ones_bf_b])


def tile_w(W, ncols, col_blocks):
    K = W.shape[0]
    kcn = K // 128
    kgn = kcn // KG
    MT = len(col_blocks)
    out = np.zeros((MT, kgn, 128, KG, ncols), np.float32)
    Wr = W.reshape(kgn, KG, 128, W.shape[1])
    for m, blocks in enumerate(col_blocks):
        c0 = 0
        for (s, w) in blocks:
            out[m, :, :, :, c0:c0 + w] = Wr[:, :, :, s:s + w].transpose(0, 2, 1, 3)
            c0 += w
    return out


def emit_rmsnorm(p, cx, hT_dram, g_sb, g_buf, gcol, xnT, xnT_buf, nt, kcn=KC):
    with ExitStack() as st:
        hs = [p.sbuf([128, nt], F32, stack=st) for _ in range(2)]
        hsb = [p.buf() for _ in range(2)]
        hsem = [p.new_dma_sem() for _ in range(2)]
        sq = [p.sbuf([128, nt], BF16, stack=st) for _ in range(2)]
        sqb = [p.buf() for _ in range(2)]
        rstd = p.sbuf([128, nt], F32, stack=st)
        rstdb = p.buf()
        tw = min(512, nt)
        ntt = nt // tw
        for kc in range(kcn):
            s = kc % 2
            p.dma("sp", hs[s][:], hT_dram[kc * 128:(kc + 1) * 128, :], hsem[s], writes=[hsb[s]])
            p.op("act", lambda e, s=s: e.activation(out=sq[s][:], in_=hs[s][:], func=AF.Square),
                 reads=[hsb[s]], writes=[sqb[s]])
            for tt in range(ntt):
                last = kc == kcn - 1
                p.op("pe", lambda e, s=s, tt=tt, kc=kc, last=last: e.matmul(
                    cx.ps[tt][:, 0:tw], lhsT=cx.ones_bf[:], rhs=sq[s][:, tt * tw:(tt + 1) * tw],
                    start=(kc == 0), stop=last),
                    reads=[sqb[s], cx.ones_bf_b], writes=[cx.psb[tt]], signal=(last or tt == ntt - 1))
        for tt in range(ntt):
            p.op("act", lambda e, tt=tt: e.activation(
                out=rstd[:, tt * tw:(tt + 1) * tw], in_=cx.ps[tt][:, 0:tw], func=AF.Sqrt,
                bias=RMS_EPS, scale=1.0 / (kcn * 128)),
                reads=[cx.psb[tt]], writes=[rstdb])
        p.op("dve", lambda e: e.reciprocal(out=rstd[:], in_=rstd[:]), reads=[rstdb], writes=[rstdb])
        for kc in range(kcn):
            s = kc % 2
            p.dma("sp", hs[s][:], hT_dram[kc * 128:(kc + 1) * 128, :], hsem[s], writes=[hsb[s]])
            p.op("dve", lambda e, s=s, kc=kc: e.scalar_tensor_tensor(
                out=xnT[:, kc, :], in0=hs[s][:], scalar=g_sb[:, gcol + kc:gcol + kc + 1], in1=rstd[:],
                op0=ALU.mult, op1=ALU.mult),
                reads=[hsb[s], rstdb, g_buf], writes=[xnT_buf])
        p.barrier()


class WStream:
    def __init__(self, p, nslots, ncols, stack):
        self.p = p
        self.ncols = ncols
        self.nslots = nslots
        self.slots = [p.sbuf([128, KG, ncols], BF16, stack=stack) for _ in range(nslots)]
        self.bufs = [p.buf() for _ in range(nslots)]
        self.sems = [p.new_dma_sem() for _ in range(nslots)]
        self.i = 0

    def load(self, src_ap):
        s = self.i % self.nslots
        self.i += 1
        self.p.dma("pool", self.slots[s][:], src_ap, self.sems[s], writes=[self.bufs[s]])
        return self.slots[s], self.bufs[s]


def emit_lin_fm(p, cx, ws, xT, xT_buf, kcn, w_dram, mt, nblk, nt, evac, ps_base=0, nsets=2):
    tw = min(512, nt)
    ntt = nt // tw
    per_set = nblk * ntt
    kgn = kcn // KG
    for m in range(mt):
        st_ = m % nsets
        banks = [[ps_base + st_ * per_set + b * ntt + tt for tt in range(ntt)] for b in range(nblk)]
        for kg in range(kgn):
            wt, wb = ws.load(w_dram[m, kg])
            for ki in range(KG):
                kc = kg * KG + ki
                for b in range(nblk):
                    for tt in range(ntt):
                        last = kc == kcn - 1
                        bk = banks[b][tt]
                        p.op("pe", lambda e, wt=wt, ki=ki, b=b, tt=tt, kc=kc, bk=bk, last=last: e.matmul(
                            cx.ps[bk][:, 0:tw], lhsT=wt[:, ki, b * 128:(b + 1) * 128],
                            rhs=xT[:, kc, tt * tw:(tt + 1) * tw], start=(kc == 0), stop=last),
                            reads=[wb, xT_buf], writes=[cx.psb[bk]],
                            signal=(last or (ki == KG - 1 and b == nblk - 1 and tt == ntt - 1)))
        evac(m, banks)


def emit_lin_tm(p, cx, ws, xT, xT_buf, kcn, w_dram, ct_n, ncols, nt, evac, ps_base=0):
    kgn = kcn // KG
    ntg = nt // 128
    i = 0
    for ct in range(ct_n):
        tiles = [ws.load(w_dram[ct, kg]) for kg in range(kgn)]
        for tg in range(ntg):
            bk = ps_base + (i % 2)
            i += 1
            for kc in range(kcn):
                wt, wb = tiles[kc // KG]
                last = kc == kcn - 1
                p.op("pe", lambda e, wt=wt, kc=kc, tg=tg, bk=bk, last=last: e.matmul(
                    cx.ps[bk][:, 0:ncols], lhsT=xT[:, kc, tg * 128:(tg + 1) * 128],
                    rhs=wt[:, kc % KG, :], start=(kc == 0), stop=last),
                    reads=[wb, xT_buf], writes=[cx.psb[bk]], signal=(last or kc % KG == KG - 1))
            evac(ct, tg, bk)


def emit_ffn(p, cx, hT_in, hT_out, g_sb, g_buf, gcol, w_in_dram, w_out_dram, nt, kcn=KC, fcn=D_FF // 128,
             h_in_buf=None, h_out_buf=None):
    h_in_buf = h_in_buf or p.buf()
    h_out_buf = h_out_buf or p.buf()
    with ExitStack() as st:
        xnT = p.sbuf([128, kcn, nt], BF16, stack=st)
        xnT_b = p.buf()
        emit_rmsnorm(p, cx, hT_in, g_sb, g_buf, gcol, xnT, xnT_b, nt, kcn)
        aT = p.sbuf([128, fcn, nt], BF16, stack=st)
        aT_b = p.buf()
        ntt = nt // 512
        with ExitStack() as st2:
            ws = WStream(p, 3, 256, st2)
            sg = [p.sbuf([128, 512], F32, stack=st2) for _ in range(2)]
            sgb = [p.buf() for _ in range(2)]
            cnt = [0]

            def evac_in(j, banks):
                for tt in range(ntt):
                    i = cnt[0] % 2
                    cnt[0] += 1
                    bg, bu = banks[0][tt], banks[1][tt]
                    p.op("act", lambda e, i=i, bg=bg: e.activation(out=sg[i][:], in_=cx.ps[bg][:], func=AF.Silu),
                         reads=[cx.psb[bg]], writes=[sgb[i]])
                    p.op("dve", lambda e, i=i, bu=bu, j=j, tt=tt: e.tensor_tensor(
                        out=aT[:, j, tt * 512:(tt + 1) * 512], in0=sg[i][:], in1=cx.ps[bu][:], op=ALU.mult),
                        reads=[sgb[i], cx.psb[bu]], writes=[aT_b])
            emit_lin_fm(p, cx, ws, xnT, xnT_b, kcn, w_in_dram, fcn, 2, nt, evac_in)
            p.barrier()
        with ExitStack() as st3:
            ws = WStream(p, 4, 128, st3)
            hs = [p.sbuf([128, nt], F32, stack=st3) for _ in range(2)]
            hsb = [p.buf() for _ in range(2)]
            hsem = [p.new_dma_sem() for _ in range(2)]
            osem = p.new_dma_sem()

            def evac_out(m, banks):
                s = m % 2
                p.dma("pool", hs[s][:], hT_in[m * 128:(m + 1) * 128, :], hsem[s],
                      reads=[h_in_buf], writes=[hsb[s]])
                for tt in range(ntt):
                    bk = banks[0][tt]
                    p.op("dve", lambda e, s=s, bk=bk, tt=tt: e.scalar_tensor_tensor(
                        out=hs[s][:, tt * 512:(tt + 1) * 512], in0=cx.ps[bk][:], scalar=0.5,
                        in1=hs[s][:, tt * 512:(tt + 1) * 512], op0=ALU.mult, op1=ALU.add),
                        reads=[cx.psb[bk], hsb[s]], writes=[hsb[s]])
                p.dma("sp", hT_out[m * 128:(m + 1) * 128, :], hs[s][:], osem, reads=[hsb[s]], writes=[h_out_buf])
            emit_lin_fm(p, cx, ws, aT, aT_b, fcn, w_out_dram, kcn, 1, nt, evac_out)
            p.barrier()
    return h_out_buf


def emit_inproj(p, cx, hT_in, g_sb, g_buf, gcol, w_fm_dram, n_fm, fmT_out, w_tm_dram, n_tm, tm_out, nt,
                kcn=KC):
    fm_b, tm_b = p.buf(), p.buf()
    with ExitStack() as st:
        xnT = p.sbuf([128, kcn, nt], BF16, stack=st)
        xnT_b = p.buf()
        emit_rmsnorm(p, cx, hT_in, g_sb, g_buf, gcol, xnT, xnT_b, nt, kcn)
        ntt = nt // 512
        with ExitStack() as st2:
            ws = WStream(p, 4, 128, st2)
            og = [p.sbuf([128, nt], BF16, stack=st2) for _ in range(2)]
            ogb = [p.buf() for _ in range(2)]
            osem = p.new_dma_sem()

            def evac_fm(m, banks):
                s = m % 2
                for tt in range(ntt):
                    bk = banks[0][tt]
                    eng = "act" if tt % 2 == 0 else "dve"
                    if eng == "act":
                        p.op("act", lambda e, s=s, bk=bk, tt=tt: e.activation(
                            out=og[s][:, tt * 512:(tt + 1) * 512], in_=cx.ps[bk][:], func=AF.Copy),
                            reads=[cx.psb[bk]], writes=[ogb[s]])
                    else:
                        p.op("dve", lambda e, s=s, bk=bk, tt=tt: e.tensor_copy(
                            out=og[s][:, tt * 512:(tt + 1) * 512], in_=cx.ps[bk][:]),
                            reads=[cx.psb[bk]], writes=[ogb[s]])
                p.dma("sp", fmT_out[m * 128:(m + 1) * 128, :], og[s][:], osem, reads=[ogb[s]], writes=[fm_b])
            emit_lin_fm(p, cx, ws, xnT, xnT_b, kcn, w_fm_dram, n_fm, 1, nt, evac_fm)
            p.barrier()
        with ExitStack() as st3:
            kgn = kcn // KG
            ws = WStream(p, 2 * kgn, 512, st3)
            ot = [p.sbuf([128, 512], F32, stack=st3) for _ in range(3)]
            otb = [p.buf() for _ in range(3)]
            osem = p.new_dma_sem()
            cnt = [0]

            def evac_tm(ct, tg, bk):
                s = cnt[0] % 3
                eng = "act" if cnt[0] % 2 == 0 else "dve"
                cnt[0] += 1
                if eng == "act":
                    p.op("act", lambda e, s=s, bk=bk: e.activation(out=ot[s][:], in_=cx.ps[bk][:], func=AF.Copy),
                         reads=[cx.psb[bk]], writes=[otb[s]])
                else:
                    p.op("dve", lambda e, s=s, bk=bk: e.tensor_copy(out=ot[s][:], in_=cx.ps[bk][:]),
                         reads=[cx.psb[bk]], writes=[otb[s]])
                p.dma("sp", tm_out[tg * 128:(tg + 1) * 128, ct * 512:(ct + 1) * 512], ot[s][:], osem,
                      reads=[otb[s]], writes=[tm_b])
            emit_lin_tm(p, cx, ws, xnT, xnT_b, kcn, w_tm_dram, n_tm, 512, nt, evac_tm)
            p.barrier()
    return fm_b, tm_b


def emit_mixout(p, cx, hT_in, hT_out, mixT_dram, w_dram, nt, kcn=KC):
    with ExitStack() as st:
        mT = p.sbuf([128, kcn, nt], BF16, stack=st)
        mT_b = p.buf()
        msem = p.new_dma_sem()
        for kc in range(kcn):
            p.dma("sp", mT[:, kc, :], mixT_dram[kc * 128:(kc + 1) * 128, :], msem, writes=[mT_b])
        ws = WStream(p, 4, 128, st)
        hs = [p.sbuf([128, nt], F32, stack=st) for _ in range(2)]
        hsb = [p.buf() for _ in range(2)]
        hsem = [p.new_dma_sem() for _ in range(2)]
        osem = p.new_dma_sem()
        ob = p.buf()
        ntt = nt // 512

        def evac(m, banks):
            s = m % 2
            p.dma("pool", hs[s][:], hT_in[m * 128:(m + 1) * 128, :], hsem[s], writes=[hsb[s]])
            for tt in range(ntt):
                bk = banks[0][tt]
                p.op("dve", lambda e, s=s, bk=bk, tt=tt: e.tensor_tensor(
                    out=hs[s][:, tt * 512:(tt + 1) * 512], in0=cx.ps[bk][:],
                    in1=hs[s][:, tt * 512:(tt + 1) * 512], op=ALU.add),
                    reads=[cx.psb[bk], hsb[s]], writes=[hsb[s]])
            p.dma("sp", hT_out[m * 128:(m + 1) * 128, :], hs[s][:], osem, reads=[hsb[s]], writes=[ob])
        emit_lin_fm(p, cx, ws, mT, mT_b, kcn, w_dram, kcn, 1, nt, evac)
        p.barrier()


def emit_final_norm(p, cx, hT_in, g_sb, g_buf, gcol, outT, nt, kcn=KC):
    with ExitStack() as st:
        hs = [p.sbuf([128, nt], F32, stack=st) for _ in range(2)]
        hsb = [p.buf() for _ in range(2)]
        hsem = [p.new_dma_sem() for _ in range(2)]
        sq = [p.sbuf([128, nt], F32, stack=st) for _ in range(2)]
        sqb = [p.buf() for _ in range(2)]
        rstd = p.sbuf([128, nt], F32, stack=st)
        rstdb = p.buf()
        ones32 = p.sbuf([128, 128], F32, stack=st)
        o32b = p.buf()
        p.op("pool", lambda e: e.memset(ones32[:], 1.0), writes=[o32b])
        ntt = nt // 512
        for kc in range(kcn):
            s = kc % 2
            p.dma("sp", hs[s][:], hT_in[kc * 128:(kc + 1) * 128, :], hsem[s], writes=[hsb[s]])
            p.op("act", lambda e, s=s: e.activation(out=sq[s][:], in_=hs[s][:], func=AF.Square),
                 reads=[hsb[s]], writes=[sqb[s]])
            for tt in range(ntt):
                last = kc == kcn - 1
                p.op("pe", lambda e, s=s, tt=tt, kc=kc, last=last: e.matmul(
                    cx.ps[tt][:], lhsT=ones32[:], rhs=sq[s][:, tt * 512:(tt + 1) * 512],
                    start=(kc == 0), stop=last),
                    reads=[sqb[s], o32b], writes=[cx.psb[tt]], signal=(last or tt == ntt - 1))
        for tt in range(ntt):
            p.op("act", lambda e, tt=tt: e.activation(
                out=rstd[:, tt * 512:(tt + 1) * 512], in_=cx.ps[tt][:], func=AF.Sqrt,
                bias=RMS_EPS, scale=1.0 / (kcn * 128)), reads=[cx.psb[tt]], writes=[rstdb])
        p.op("dve", lambda e: e.reciprocal(out=rstd[:], in_=rstd[:]), reads=[rstdb], writes=[rstdb])
        osem = p.new_dma_sem()
        ob = p.buf()
        for kc in range(kcn):
            s = kc % 2
            p.dma("sp", hs[s][:], hT_in[kc * 128:(kc + 1) * 128, :], hsem[s], writes=[hsb[s]])
            p.op("dve", lambda e, s=s, kc=kc: e.scalar_tensor_tensor(
                out=hs[s][:], in0=hs[s][:], scalar=g_sb[:, gcol + kc:gcol + kc + 1], in1=rstd[:],
                op0=ALU.mult, op1=ALU.mult), reads=[hsb[s], rstdb, g_buf], writes=[hsb[s]])
            p.dma("sp", outT[kc * 128:(kc + 1) * 128, :], hs[s][:], osem, reads=[hsb[s]], writes=[ob])
        p.barrier()


NEG = -30000.0
LN16 = float(np.log(16.0))
CONST_NAMES = ["U", "Tri", "negU", "maskS", "negS", "ones", "ident"]


def host_consts():
    i = np.arange(128)[:, None]
    j = np.arange(128)[None, :]
    c = {
        "U": (i <= j),
        "Tri": (i > j),
        "negU": np.where(i <= j, 0.0, NEG),
        "maskS": (j > i),
        "negS": np.where(j > i, 0.0, NEG),
        "ones": np.ones((128, 128)),
        "ident": (i == j),
    }
    return np.ascontiguousarray(
        np.stack([c[n].astype(np.float32) for n in CONST_NAMES], axis=1))


class HConsts:
    def __init__(self, p, cdram):
        n = len(CONST_NAMES)
        self.f32 = p.sbuf([128, n, 128], F32, name="c32")
        self.bf = p.sbuf([128, n, 128], BF16, name="cbf")
        self.b = p.buf("consts")
        sem = p.new_dma_sem()
        p.dma("sp", self.f32[:], cdram[:, :, :], sem, writes=[self.b])
        p.dma("pool", self.bf[:], cdram[:, :, :], sem, writes=[self.b])

    def c32(self, name):
        return self.f32[:, CONST_NAMES.index(name), :]

    def cbf(self, name):
        return self.bf[:, CONST_NAMES.index(name), :]


def emit_memkv(p, cx, memT_dram, g_sb, g_buf, w_k_dram, w_v_dram, mkT, mkT_b, mv, mv_b, nmh=2, gcol=0):
    with ExitStack() as st:
        mnT = p.sbuf([128, KC, MEM_TOKENS], BF16, stack=st)
        mnT_b = p.buf()
        emit_rmsnorm(p, cx, memT_dram, g_sb, g_buf, gcol, mnT, mnT_b, MEM_TOKENS)
        ws = WStream(p, 4, 128, st)

        def evac_k(m, banks):
            bk = banks[0][0]
            p.op("act", lambda e, m=m, bk=bk: e.activation(out=mkT[:, m, :], in_=cx.ps[bk][:, 0:MEM_TOKENS],
                                                          func=AF.Copy),
                 reads=[cx.psb[bk]], writes=[mkT_b])
        emit_lin_fm(p, cx, ws, mnT, mnT_b, KC, w_k_dram, 2 * nmh, 1, MEM_TOKENS, evac_k)
        ws2 = WStream(p, 2 * (KC // KG), 512, st)

        def evac_v(ct, tg, bk):
            p.op("act", lambda e, ct=ct, tg=tg, bk=bk: e.activation(
                out=mv[:, tg, ct * 512:(ct + 1) * 512], in_=cx.ps[bk][:], func=AF.Copy),
                 reads=[cx.psb[bk]], writes=[mv_b])
        emit_lin_tm(p, cx, ws2, mnT, mnT_b, KC, w_v_dram, nmh // 2, 512, MEM_TOKENS, evac_v)
        p.barrier()


def emit_memattn(p, cx, hc, mqT_dram, mkT, mkT_b, mv, mv_b, mixT_out, row0, mix_b, nmh=2):
    with ExitStack() as st:
        mq = p.sbuf([128, 2 * nmh, SEQ], BF16, stack=st)
        mq_b = p.buf()
        sem = p.new_dma_sem()
        for i in range(2 * nmh):
            p.dma("sp", mq[:, i, :], mqT_dram[i * 128:(i + 1) * 128, :], sem, writes=[mq_b])
        pT = [p.sbuf([128, 2, 512], BF16, stack=st) for _ in range(2)]
        pTb = [p.buf() for _ in range(2)]
        rd = [p.sbuf([128, 512], F32, stack=st) for _ in range(2)]
        rdb = [p.buf() for _ in range(2)]
        oT = p.sbuf([128, 2 * nmh, SEQ], BF16, stack=st)
        oT_b = p.buf()
        it = 0
        for mh in range(nmh):
            for tq in range(SEQ // 512):
                s = it % 2
                it += 1
                tok = slice(tq * 512, (tq + 1) * 512)
                for mc in range(2):
                    for dc in range(2):
                        p.op("pe", lambda e, mc=mc, dc=dc, mh=mh, tok=tok: e.matmul(
                            cx.ps[mc][:], lhsT=mkT[:, mh * 2 + dc, mc * 128:(mc + 1) * 128],
                            rhs=mq[:, mh * 2 + dc, tok], start=(dc == 0), stop=(dc == 1)),
                            reads=[mkT_b, mq_b], writes=[cx.psb[mc]], signal=(dc == 1))
                    p.op("act", lambda e, mc=mc, s=s: e.activation(
                        out=pT[s][:, mc, :], in_=cx.ps[mc][:], func=AF.Exp, scale=1.0 / 16.0),
                        reads=[cx.psb[mc]], writes=[pTb[s]])
                for mc in range(2):
                    p.op("pe", lambda e, mc=mc, s=s: e.matmul(
                        cx.ps[2][:], lhsT=hc.cbf("ones"), rhs=pT[s][:, mc, :], start=(mc == 0), stop=(mc == 1)),
                        reads=[hc.b, pTb[s]], writes=[cx.psb[2]], signal=(mc == 1))
                p.op("dve", lambda e, s=s: e.reciprocal(out=rd[s][:], in_=cx.ps[2][:]),
                     reads=[cx.psb[2]], writes=[rdb[s]])
                for dvc in range(2):
                    bk = 3 + dvc
                    for mc in range(2):
                        p.op("pe", lambda e, mc=mc, dvc=dvc, mh=mh, s=s, bk=bk: e.matmul(
                            cx.ps[bk][:], lhsT=mv[:, mc, mh * 256 + dvc * 128:mh * 256 + (dvc + 1) * 128],
                            rhs=pT[s][:, mc, :], start=(mc == 0), stop=(mc == 1)),
                            reads=[mv_b, pTb[s]], writes=[cx.psb[bk]], signal=(mc == 1))
                    p.op("dve", lambda e, bk=bk, s=s, mh=mh, dvc=dvc, tok=tok: e.tensor_tensor(
                        out=oT[:, mh * 2 + dvc, tok], in0=cx.ps[bk][:], in1=rd[s][:], op=ALU.mult),
                        reads=[cx.psb[bk], rdb[s]], writes=[oT_b])
        osem = p.new_dma_sem()
        for i in range(2 * nmh):
            p.dma("sp", mixT_out[row0 + i * 128:row0 + (i + 1) * 128, :], oT[:, i, :], osem,
                  reads=[oT_b], writes=[mix_b])
        p.barrier()


def emit_mlstm_v1(p, cx, hc, d, mixT_out, mix_b, nheads=3):
    NCH = SEQ // 128
    NI = nheads * NCH
    with ExitStack() as st:
        G = p.sbuf([128, NCH, 2 * nheads], F32, stack=st)
        gb = p.sbuf([128, 2 * nheads], F32, stack=st)
        gb15 = p.sbuf([128, 2 * nheads], F32, stack=st)
        gain = p.sbuf([128, nheads * 512], F32, stack=st)
        setup_b = p.buf()
        sem0 = p.new_dma_sem()
        p.dma("sp", G[:], d["gates"].rearrange("(c p) j -> p c j", p=128), sem0, writes=[setup_b])
        p.dma("sp", gb[:], d["gb"][:, :], sem0, writes=[setup_b])
        p.dma("sp", gain[:], d["gain"][:, :], sem0, writes=[setup_b])
        T1 = p.sbuf([128, NI], F32, stack=st)
        T2 = p.sbuf([128, NI], F32, stack=st)
        IG = p.sbuf([128, NI], F32, stack=st)
        LF = p.sbuf([128, NI], F32, stack=st)
        WB = p.sbuf([128, NI], F32, stack=st)
        DB = p.sbuf([128, NI], F32, stack=st)
        gt_b = p.buf()
        p.op("dve", lambda e: e.tensor_scalar_mul(out=gb15[:], in0=gb[:], scalar1=1.0 / 15.0),
             reads=[setup_b], writes=[gt_b])
        for h in range(nheads):
            hs_ = slice(h * NCH, (h + 1) * NCH)
            p.op("act", lambda e, h=h, hs_=hs_: e.activation(
                out=T1[:, hs_], in_=G[:, :, h], func=AF.Tanh, bias=gb15[:, h:h + 1], scale=1.0 / 15.0),
                reads=[setup_b, gt_b], writes=[gt_b])
            p.op("act", lambda e, h=h, hs_=hs_: e.activation(
                out=T2[:, hs_], in_=G[:, :, nheads + h], func=AF.Tanh,
                bias=gb15[:, nheads + h:nheads + h + 1], scale=1.0 / 15.0),
                reads=[setup_b, gt_b], writes=[gt_b])
        p.op("dve", lambda e: e.tensor_scalar_mul(out=IG[:], in0=T1[:], scalar1=15.0), reads=[gt_b], writes=[gt_b])
        p.op("act", lambda e: e.activation(out=T2[:], in_=T2[:], func=AF.Exp, scale=-15.0),
             reads=[gt_b], writes=[gt_b])
        p.op("act", lambda e: e.activation(out=T2[:], in_=T2[:], func=AF.Ln, bias=1.0),
             reads=[gt_b], writes=[gt_b])
        p.op("dve", lambda e: e.tensor_scalar_mul(out=LF[:], in0=T2[:], scalar1=-1.0), reads=[gt_b], writes=[gt_b])
        p.op("pe", lambda e: e.matmul(cx.ps[7][:, 0:NI], lhsT=hc.c32("U"), rhs=LF[:], start=True, stop=True),
             reads=[gt_b, hc.b], writes=[cx.psb[7]])
        p.op("dve", lambda e: e.tensor_tensor(out=WB[:], in0=IG[:], in1=cx.ps[7][:, 0:NI], op=ALU.subtract),
             reads=[gt_b, cx.psb[7]], writes=[gt_b])
        p.op("dve", lambda e: e.tensor_scalar_add(out=DB[:], in0=WB[:], scalar1=-LN16), reads=[gt_b], writes=[gt_b])

        qTs = p.sbuf([128, 2, SEQ], BF16, stack=st)
        kTs = p.sbuf([128, 2, SEQ], BF16, stack=st)
        ktm = p.sbuf([128, NCH, 256], BF16, stack=st)
        V = p.sbuf([128, NCH, 512], BF16, stack=st)
        O = p.sbuf([128, NCH, 512], F32, stack=st)
        hd_b = p.buf()
        hsem = p.new_dma_sem()
        C32 = p.sbuf([128, 2, 512], F32, stack=st)
        Cbf = p.sbuf([128, 2, 512], BF16, stack=st)
        n32 = p.sbuf([128, 2], F32, stack=st)
        nbf = p.sbuf([128, 2], BF16, stack=st)
        C32_b, Cbf_b, n32_b, nbf_b = p.buf(), p.buf(), p.buf(), p.buf()
        mTh = p.sbuf([128, 4, SEQ], BF16, stack=st)
        mTh_b = p.buf()
        osem = p.new_dma_sem()
        lfb = p.sbuf([128, 128], F32, stack=st); lfb_b = p.buf()
        bl = p.sbuf([128, 1], F32, stack=st); bl_b = p.buf()
        tt_ = p.sbuf([128, 128], F32, stack=st); tt_b = p.buf()
        DT = p.sbuf([128, 128], F32, stack=st); DT_b = p.buf()
        EB = p.sbuf([128, 128], F32, stack=st); EB_b = p.buf()
        qp = p.sbuf([128, 2, 128], BF16, stack=st); qp_b = p.buf()
        ST = p.sbuf([128, 128], BF16, stack=st); ST_b = p.buf()
        w_ = p.sbuf([128, 1], F32, stack=st); w_b = p.buf()
        eL = p.sbuf([128, 1], F32, stack=st); eL_b = p.buf()
        kw = p.sbuf([128, 256], BF16, stack=st); kw_b = p.buf()
        den = p.sbuf([128, 1], F32, stack=st); den_b = p.buf()
        hn = p.sbuf([128, 512], F32, stack=st); hn_b = p.buf()
        junk = p.sbuf([128, 512], F32, stack=st); junk_b = p.buf()
        ss = p.sbuf([128, 1], F32, stack=st); ss_b = p.buf()
        y = p.sbuf([128, 512], F32, stack=st); y_b = p.buf()
        sig = p.sbuf([128, 512], F32, stack=st); sig_b = p.buf()
        y2 = p.sbuf([128, 512], BF16, stack=st); y2_b = p.buf()
        ps3n_b = p.buf()
        ones_c = hc.cbf("ones")[:, 0:1]
        P = cx.ps
        B = cx.psb

        for h in range(nheads):
            for dc in range(2):
                r0 = h * 256 + dc * 128
                p.dma("sp", qTs[:, dc, :], d["qT"][r0:r0 + 128, :], hsem, writes=[hd_b])
                p.dma("sp", kTs[:, dc, :], d["kT"][r0:r0 + 128, :], hsem, writes=[hd_b])
            p.dma("pool", ktm[:], d["k_tm"][:, h * 256:(h + 1) * 256].rearrange("(c p) d -> p c d", p=128),
                  hsem, writes=[hd_b])
            p.dma("pool", V[:], d["v_tm"][:, h * 512:(h + 1) * 512].rearrange("(c p) d -> p c d", p=128),
                  hsem, writes=[hd_b])
            p.dma("sp", O[:], d["o_tm"][:, h * 512:(h + 1) * 512].rearrange("(c p) d -> p c d", p=128),
                  hsem, writes=[hd_b])
            p.op("pool", lambda e: e.memset(C32[:], 0.0), writes=[C32_b])
            p.op("pool", lambda e: e.memset(Cbf[:], 0.0), writes=[Cbf_b])
            p.op("pool", lambda e: e.memset(n32[:], 0.0), writes=[n32_b])
            p.op("pool", lambda e: e.memset(nbf[:], 0.0), writes=[nbf_b])
            for c in range(NCH):
                idx = h * NCH + c
                cols = slice(c * 128, (c + 1) * 128)
                p.op("pool", lambda e, idx=idx: e.tensor_scalar_mul(
                    out=lfb[:], in0=hc.c32("ones"), scalar1=LF[:, idx:idx + 1]),
                    reads=[gt_b, hc.b], writes=[lfb_b])
                p.op("pe", lambda e: e.matmul(P[0][:, 0:128], lhsT=lfb[:], rhs=hc.c32("U"), start=True, stop=True),
                     reads=[lfb_b, hc.b], writes=[B[0]])
                p.op("dve", lambda e: e.tensor_copy(out=bl[:], in_=P[0][:, 127:128]), reads=[B[0]], writes=[bl_b])
                p.op("dve", lambda e: e.tensor_tensor(out=tt_[:], in0=P[0][:, 0:128], in1=hc.c32("negU"),
                                                      op=ALU.add), reads=[B[0], hc.b], writes=[tt_b])
                p.op("act", lambda e, idx=idx: e.activation(out=DT[:], in_=tt_[:], func=AF.Exp,
                                                            bias=DB[:, idx:idx + 1]),
                     reads=[tt_b, gt_b], writes=[DT_b])
                p.op("act", lambda e: e.activation(out=EB[:], in_=P[0][:, 0:128], func=AF.Exp, bias=-LN16),
                     reads=[B[0]], writes=[EB_b])
                for dc in range(2):
                    p.op("dve", lambda e, dc=dc, cols=cols: e.tensor_tensor(
                        out=qp[:, dc, :], in0=qTs[:, dc, cols], in1=EB[:], op=ALU.mult),
                        reads=[hd_b, EB_b], writes=[qp_b])
                for dc in range(2):
                    p.op("pe", lambda e, dc=dc, cols=cols: e.matmul(
                        P[1][:, 0:128], lhsT=kTs[:, dc, cols], rhs=qTs[:, dc, cols], start=(dc == 0), stop=(dc == 1)),
                        reads=[hd_b], writes=[B[1]], signal=(dc == 1))
                p.op("dve", lambda e: e.tensor_tensor(out=ST[:], in0=P[1][:, 0:128], in1=DT[:], op=ALU.mult),
                     reads=[B[1], DT_b], writes=[ST_b])
                p.op("pe", lambda e, c=c: e.matmul(P[2][:], lhsT=ST[:], rhs=V[:, c, :], start=True, stop=False),
                     reads=[ST_b, hd_b], writes=[B[2]], signal=False)
                for dc in range(2):
                    p.op("pe", lambda e, dc=dc: e.matmul(P[2][:], lhsT=qp[:, dc, :], rhs=Cbf[:, dc, :],
                                                         start=False, stop=(dc == 1)),
                         reads=[qp_b, Cbf_b], writes=[B[2]], signal=(dc == 1))
                p.op("pe", lambda e: e.matmul(P[3][:, 0:1], lhsT=ST[:], rhs=ones_c, start=True, stop=False),
                     reads=[ST_b, hc.b], writes=[B[3]], signal=False)
                for dc in range(2):
                    p.op("pe", lambda e, dc=dc: e.matmul(P[3][:, 0:1], lhsT=qp[:, dc, :], rhs=nbf[:, dc:dc + 1],
                                                         start=False, stop=(dc == 1)),
                         reads=[qp_b, nbf_b], writes=[B[3]], signal=(dc == 1))
                p.op("act", lambda e, idx=idx: e.activation(out=w_[:], in_=WB[:, idx:idx + 1], func=AF.Exp,
                                                            bias=bl[:, 0:1]),
                     reads=[gt_b, bl_b], writes=[w_b])
                p.op("act", lambda e: e.activation(out=eL[:], in_=bl[:], func=AF.Exp), reads=[bl_b], writes=[eL_b])
                p.op("dve", lambda e, c=c: e.tensor_scalar_mul(out=kw[:], in0=ktm[:, c, :], scalar1=w_[:, 0:1]),
                     reads=[hd_b, w_b], writes=[kw_b])
                for dc in range(2):
                    p.op("pe", lambda e, dc=dc, c=c: e.matmul(
                        P[4 + dc][:], lhsT=kw[:, dc * 128:(dc + 1) * 128], rhs=V[:, c, :], start=True, stop=True),
                        reads=[kw_b, hd_b], writes=[B[4 + dc]])
                    p.op("pe", lambda e, dc=dc: e.matmul(
                        P[3][:, 1 + dc:2 + dc], lhsT=kw[:, dc * 128:(dc + 1) * 128], rhs=ones_c,
                        start=True, stop=True), reads=[kw_b, hc.b], writes=[ps3n_b])
                p.op("act", lambda e: e.activation(out=den[:], in_=P[3][:, 0:1], func=AF.Abs),
                     reads=[B[3]], writes=[den_b])
                p.op("dve", lambda e: e.tensor_scalar_max(out=den[:], in0=den[:], scalar1=1.0),
                     reads=[den_b], writes=[den_b])
                p.op("dve", lambda e: e.reciprocal(out=den[:], in_=den[:]), reads=[den_b], writes=[den_b])
                p.op("act", lambda e: e.activation(out=hn[:], in_=P[2][:], func=AF.Copy, scale=den[:, 0:1]),
                     reads=[B[2], den_b], writes=[hn_b])
                p.op("act", lambda e: e.activation(out=junk[:], in_=hn[:], func=AF.Square, accum_out=ss[:, 0:1]),
                     reads=[hn_b], writes=[junk_b, ss_b])
                p.op("act", lambda e: e.activation(out=ss[:], in_=ss[:], func=AF.Sqrt, bias=RMS_EPS,
                                                   scale=1.0 / 512.0), reads=[ss_b], writes=[ss_b])
                p.op("dve", lambda e: e.reciprocal(out=ss[:], in_=ss[:]), reads=[ss_b], writes=[ss_b])
                p.op("dve", lambda e, h=h: e.scalar_tensor_tensor(
                    out=y[:], in0=hn[:], scalar=ss[:, 0:1], in1=gain[:, h * 512:(h + 1) * 512],
                    op0=ALU.mult, op1=ALU.mult), reads=[hn_b, ss_b, setup_b], writes=[y_b])
                p.op("act", lambda e, c=c: e.activation(out=sig[:], in_=O[:, c, :], func=AF.Sigmoid),
                     reads=[hd_b], writes=[sig_b])
                p.op("pool", lambda e: e.tensor_tensor(out=y2[:], in0=y[:], in1=sig[:], op=ALU.mult),
                     reads=[y_b, sig_b], writes=[y2_b])
                for blk in range(4):
                    p.op("pe", lambda e, blk=blk: e.matmul(
                        P[6][:, blk * 128:(blk + 1) * 128], lhsT=y2[:, blk * 128:(blk + 1) * 128],
                        rhs=hc.cbf("ident"), start=True, stop=True),
                        reads=[y2_b, hc.b], writes=[B[6]], signal=(blk == 3))
                p.op("act", lambda e, cols=cols: e.activation(
                    out=mTh[:, :, cols], in_=P[6][:].rearrange("p (b j) -> p b j", b=4), func=AF.Copy),
                    reads=[B[6]], writes=[mTh_b])
                for dc in range(2):
                    p.op("dve", lambda e, dc=dc: e.scalar_tensor_tensor(
                        out=C32[:, dc, :], in0=C32[:, dc, :], scalar=eL[:, 0:1], in1=P[4 + dc][:],
                        op0=ALU.mult, op1=ALU.add), reads=[C32_b, eL_b, B[4 + dc]], writes=[C32_b])
                p.op("pool", lambda e: e.tensor_copy(out=Cbf[:], in_=C32[:]), reads=[C32_b], writes=[Cbf_b])
                p.op("dve", lambda e: e.scalar_tensor_tensor(
                    out=n32[:], in0=n32[:], scalar=eL[:, 0:1], in1=P[3][:, 1:3], op0=ALU.mult, op1=ALU.add),
                    reads=[n32_b, eL_b, ps3n_b], writes=[n32_b])
                p.op("dve", lambda e: e.tensor_copy(out=nbf[:], in_=n32[:]), reads=[n32_b], writes=[nbf_b])
            for blk in range(4):
                r0 = h * 512 + blk * 128
                p.dma("sp", mixT_out[r0:r0 + 128, :], mTh[:, blk, :], osem, reads=[mTh_b], writes=[mix_b])
        p.barrier()


def emit_mlstm(p, cx, hc, d, mixT_out, mix_b, nheads=3):
    NCH = SEQ // 128
    NI = nheads * NCH
    P, B = cx.ps, cx.psb
    with ExitStack() as st:
        G = p.sbuf([128, NCH, 2 * nheads], F32, stack=st)
        gb = p.sbuf([128, 2 * nheads], F32, stack=st)
        gb15 = p.sbuf([128, 2 * nheads], F32, stack=st)
        gain = p.sbuf([128, nheads * 512], F32, stack=st)
        setup_b = p.buf()
        sem0 = p.new_dma_sem()
        p.dma("sp", G[:], d["gates"].rearrange("(c p) j -> p c j", p=128), sem0, writes=[setup_b])
        p.dma("sp", gb[:], d["gb"][:, :], sem0, writes=[setup_b])
        p.dma("sp", gain[:], d["gain"][:, :], sem0, writes=[setup_b])
        T1 = p.sbuf([128, NI], F32, stack=st)
        T2 = p.sbuf([128, NI], F32, stack=st)
        IG = p.sbuf([128, NI], F32, stack=st)
        LF = p.sbuf([128, NI], F32, stack=st)
        WB = p.sbuf([128, NI], F32, stack=st)
        DB = p.sbuf([128, NI], F32, stack=st)
        gt_b = p.buf()
        p.op("dve", lambda e: e.tensor_scalar_mul(out=gb15[:], in0=gb[:], scalar1=1.0 / 15.0),
             reads=[setup_b], writes=[gt_b])
        for h in range(nheads):
            hs_ = slice(h * NCH, (h + 1) * NCH)
            p.op("act", lambda e, h=h, hs_=hs_: e.activation(
                out=T1[:, hs_], in_=G[:, :, h], func=AF.Tanh, bias=gb15[:, h:h + 1], scale=1.0 / 15.0),
                reads=[setup_b, gt_b], writes=[gt_b])
            p.op("act", lambda e, h=h, hs_=hs_: e.activation(
                out=T2[:, hs_], in_=G[:, :, nheads + h], func=AF.Tanh,
                bias=gb15[:, nheads + h:nheads + h + 1], scale=1.0 / 15.0),
                reads=[setup_b, gt_b], writes=[gt_b])
        p.op("dve", lambda e: e.tensor_scalar_mul(out=IG[:], in0=T1[:], scalar1=15.0), reads=[gt_b], writes=[gt_b])
        p.op("act", lambda e: e.activation(out=T2[:], in_=T2[:], func=AF.Exp, scale=-15.0),
             reads=[gt_b], writes=[gt_b])
        p.op("act", lambda e: e.activation(out=T2[:], in_=T2[:], func=AF.Ln, bias=1.0),
             reads=[gt_b], writes=[gt_b])
        p.op("dve", lambda e: e.tensor_scalar_mul(out=LF[:], in0=T2[:], scalar1=-1.0), reads=[gt_b], writes=[gt_b])
        p.op("pe", lambda e: e.matmul(P[7][:, 0:NI], lhsT=hc.c32("U"), rhs=LF[:], start=True, stop=True),
             reads=[gt_b, hc.b], writes=[B[7]])
        p.op("dve", lambda e: e.tensor_tensor(out=WB[:], in0=IG[:], in1=P[7][:, 0:NI], op=ALU.subtract),
             reads=[gt_b, B[7]], writes=[gt_b])
        p.op("dve", lambda e: e.tensor_scalar_add(out=DB[:], in0=WB[:], scalar1=-LN16), reads=[gt_b], writes=[gt_b])

        NHB = 2
        qTs = [p.sbuf([128, 2, SEQ], BF16, stack=st) for _ in range(NHB)]
        kTs = [p.sbuf([128, 2, SEQ], BF16, stack=st) for _ in range(NHB)]
        ktm = [p.sbuf([128, NCH, 256], BF16, stack=st) for _ in range(NHB)]
        V = [p.sbuf([128, NCH, 512], BF16, stack=st) for _ in range(NHB)]
        hd_b = [p.buf() for _ in range(NHB)]
        hsem = [p.new_dma_sem() for _ in range(NHB)]
        mTh = [p.sbuf([128, 4, SEQ], BF16, stack=st) for _ in range(2)]
        mTh_b = [p.buf() for _ in range(2)]
        osem = p.new_dma_sem()
        C32 = p.sbuf([128, 2, 512], F32, stack=st)
        Cbf = p.sbuf([128, 2, 512], BF16, stack=st)
        n32 = p.sbuf([128, 2], F32, stack=st)
        nbf = p.sbuf([128, 2], BF16, stack=st)
        C32_b, Cbf_b, n32_b, nbf_b = p.buf(), p.buf(), p.buf(), p.buf()
        NS = 5

        def mk(shape, dt):
            return [p.sbuf(shape, dt, stack=st) for _ in range(NS)], [p.buf() for _ in range(NS)]
        lfb, lfb_b = mk([128, 128], F32)
        bl, bl_b = mk([128, 1], F32)
        tt_, tt_b = mk([128, 128], F32)
        DT, DT_b = mk([128, 128], F32)
        EB, EB_b = mk([128, 128], F32)
        qp, qp_b = mk([128, 2, 128], BF16)
        ST, ST_b = mk([128, 128], BF16)
        w_, w_b = mk([128, 1], F32)
        eL, eL_b = mk([128, 1], F32)
        kw, kw_b = mk([128, 256], BF16)
        den, den_b = mk([128, 1], F32)
        hn, hn_b = mk([128, 512], F32)
        ss, ss_b = mk([128, 1], F32)
        sig, sig_b = mk([128, 512], F32)
        Ot, Ot_b = mk([128, 512], F32)
        y2, y2_b = mk([128, 512], BF16)
        Osem = [p.new_dma_sem() for _ in range(NS)]
        junk = p.sbuf([128, 512], F32, stack=st); junk_b = p.buf()
        dn_b = [p.buf() for _ in range(4)]
        ones_c = hc.cbf("ones")[:, 0:1]

        steps = []
        for h in range(nheads):
            for c in range(NCH):
                k = len(steps)
                steps.append(dict(k=k, h=h, c=c, hb=h % NHB, s=k % NS, bs=k % 2, nb=2 + k % 2, dg=k % 4,
                                  idx=h * NCH + c))
        n = len(steps)

        def load_head(h):
            hb = h % NHB
            for dc in range(2):
                r0 = h * 256 + dc * 128
                p.dma("sp", qTs[hb][:, dc, :], d["qT"][r0:r0 + 128, :], hsem[hb], writes=[hd_b[hb]])
                p.dma("sp", kTs[hb][:, dc, :], d["kT"][r0:r0 + 128, :], hsem[hb], writes=[hd_b[hb]])
            p.dma("pool", ktm[hb][:], d["k_tm"][:, h * 256:(h + 1) * 256].rearrange("(c p) d -> p c d", p=128),
                  hsem[hb], writes=[hd_b[hb]])
            p.dma("pool", V[hb][:], d["v_tm"][:, h * 512:(h + 1) * 512].rearrange("(c p) d -> p c d", p=128),
                  hsem[hb], writes=[hd_b[hb]])

        def P1(t):
            s, bs, hb, c, idx, h = t["s"], t["bs"], t["hb"], t["c"], t["idx"], t["h"]
            cols = slice(c * 128, (c + 1) * 128)
            p.dma("sp", Ot[s][:], d["o_tm"][c * 128:(c + 1) * 128, h * 512:(h + 1) * 512], Osem[s],
                  writes=[Ot_b[s]])
            p.op("pool", lambda e: e.tensor_scalar_mul(out=lfb[s][:], in0=hc.c32("ones"), scalar1=LF[:, idx:idx + 1]),
                 reads=[gt_b, hc.b], writes=[lfb_b[s]])
            p.op("pe", lambda e: e.matmul(P[bs][:, 0:128], lhsT=lfb[s][:], rhs=hc.c32("U"), start=True, stop=True),
                 reads=[lfb_b[s], hc.b], writes=[B[bs]])
            p.op("dve", lambda e: e.tensor_copy(out=bl[s][:], in_=P[bs][:, 127:128]), reads=[B[bs]], writes=[bl_b[s]])
            p.op("dve", lambda e: e.tensor_tensor(out=tt_[s][:], in0=P[bs][:, 0:128], in1=hc.c32("negU"), op=ALU.add),
                 reads=[B[bs], hc.b], writes=[tt_b[s]])
            p.op("act", lambda e: e.activation(out=DT[s][:], in_=tt_[s][:], func=AF.Exp, bias=DB[:, idx:idx + 1]),
                 reads=[tt_b[s], gt_b], writes=[DT_b[s]])
            p.op("act", lambda e: e.activation(out=EB[s][:], in_=P[bs][:, 0:128], func=AF.Exp, bias=-LN16),
                 reads=[B[bs]], writes=[EB_b[s]])
            p.op("act", lambda e: e.activation(out=w_[s][:], in_=WB[:, idx:idx + 1], func=AF.Exp, bias=bl[s][:, 0:1]),
                 reads=[gt_b, bl_b[s]], writes=[w_b[s]])
            p.op("act", lambda e: e.activation(out=eL[s][:], in_=bl[s][:], func=AF.Exp), reads=[bl_b[s]],
                 writes=[eL_b[s]])
            for dc in range(2):
                p.op("pool", lambda e, dc=dc: e.tensor_tensor(out=qp[s][:, dc, :], in0=qTs[hb][:, dc, cols],
                                                              in1=EB[s][:], op=ALU.mult),
                     reads=[hd_b[hb], EB_b[s]], writes=[qp_b[s]])
            for dc in range(2):
                p.op("pe", lambda e, dc=dc: e.matmul(P[bs][:, 128:256], lhsT=kTs[hb][:, dc, cols],
                                                     rhs=qTs[hb][:, dc, cols], start=(dc == 0), stop=(dc == 1)),
                     reads=[hd_b[hb]], writes=[B[bs]], signal=(dc == 1))
            p.op("dve", lambda e: e.tensor_tensor(out=ST[s][:], in0=P[bs][:, 128:256], in1=DT[s][:], op=ALU.mult),
                 reads=[B[bs], DT_b[s]], writes=[ST_b[s]])
            p.op("pool", lambda e: e.tensor_scalar_mul(out=kw[s][:], in0=ktm[hb][:, c, :], scalar1=w_[s][:, 0:1]),
                 reads=[hd_b[hb], w_b[s]], writes=[kw_b[s]])
            p.op("act", lambda e: e.activation(out=sig[s][:], in_=Ot[s][:], func=AF.Sigmoid),
                 reads=[Ot_b[s]], writes=[sig_b[s]])

        def P2(t):
            s, hb, c, nb, dg = t["s"], t["hb"], t["c"], t["nb"], t["dg"]
            d0 = dg * 4
            if c == 0:
                p.op("pool", lambda e: e.memset(C32[:], 0.0), writes=[C32_b])
                p.op("pool", lambda e: e.memset(Cbf[:], 0.0), writes=[Cbf_b])
                p.op("pool", lambda e: e.memset(n32[:], 0.0), writes=[n32_b])
                p.op("pool", lambda e: e.memset(nbf[:], 0.0), writes=[nbf_b])
            p.op("pe", lambda e: e.matmul(P[nb][:], lhsT=ST[s][:], rhs=V[hb][:, c, :], start=True, stop=False),
                 reads=[ST_b[s], hd_b[hb]], writes=[B[nb]], signal=False)
            for dc in range(2):
                p.op("pe", lambda e, dc=dc: e.matmul(P[nb][:], lhsT=qp[s][:, dc, :], rhs=Cbf[:, dc, :],
                                                     start=False, stop=(dc == 1)),
                     reads=[qp_b[s], Cbf_b], writes=[B[nb]], signal=(dc == 1))
            p.op("pe", lambda e: e.matmul(P[4][:, d0:d0 + 1], lhsT=ST[s][:], rhs=ones_c, start=True, stop=False),
                 reads=[ST_b[s], hc.b], writes=[dn_b[dg]], signal=False)
            for dc in range(2):
                p.op("pe", lambda e, dc=dc: e.matmul(P[4][:, d0:d0 + 1], lhsT=qp[s][:, dc, :], rhs=nbf[:, dc:dc + 1],
                                                     start=False, stop=(dc == 1)),
                     reads=[qp_b[s], nbf_b], writes=[dn_b[dg]], signal=(dc == 1))
            for dc in range(2):
                p.op("pe", lambda e, dc=dc: e.matmul(P[5 + dc][:], lhsT=kw[s][:, dc * 128:(dc + 1) * 128],
                                                     rhs=V[hb][:, c, :], start=True, stop=True),
                     reads=[kw_b[s], hd_b[hb]], writes=[B[5 + dc]])
                p.op("pe", lambda e, dc=dc: e.matmul(P[4][:, d0 + 1 + dc:d0 + 2 + dc],
                                                     lhsT=kw[s][:, dc * 128:(dc + 1) * 128], rhs=ones_c,
                                                     start=True, stop=True),
                     reads=[kw_b[s], hc.b], writes=[dn_b[dg]])
            for dc in range(2):
                p.op("dve", lambda e, dc=dc: e.scalar_tensor_tensor(
                    out=C32[:, dc, :], in0=C32[:, dc, :], scalar=eL[s][:, 0:1], in1=P[5 + dc][:],
                    op0=ALU.mult, op1=ALU.add), reads=[C32_b, eL_b[s], B[5 + dc]], writes=[C32_b])
            p.op("pool", lambda e: e.tensor_copy(out=Cbf[:], in_=C32[:]), reads=[C32_b], writes=[Cbf_b])
            p.op("dve", lambda e: e.scalar_tensor_tensor(
                out=n32[:], in0=n32[:], scalar=eL[s][:, 0:1], in1=P[4][:, d0 + 1:d0 + 3], op0=ALU.mult, op1=ALU.add),
                reads=[n32_b, eL_b[s], dn_b[dg]], writes=[n32_b])
            p.op("pool", lambda e: e.tensor_copy(out=nbf[:], in_=n32[:]), reads=[n32_b], writes=[nbf_b])

        def P3(t):
            s, nb, dg = t["s"], t["nb"], t["dg"]
            d0 = dg * 4
            p.op("act", lambda e: e.activation(out=den[s][:], in_=P[4][:, d0:d0 + 1], func=AF.Abs),
                 reads=[dn_b[dg]], writes=[den_b[s]])
            p.op("dve", lambda e: e.tensor_scalar_max(out=den[s][:], in0=den[s][:], scalar1=1.0),
                 reads=[den_b[s]], writes=[den_b[s]])
            p.op("dve", lambda e: e.reciprocal(out=den[s][:], in_=den[s][:]), reads=[den_b[s]], writes=[den_b[s]])
            p.op("act", lambda e: e.activation(out=hn[s][:], in_=P[nb][:], func=AF.Copy, scale=den[s][:, 0:1]),
                 reads=[B[nb], den_b[s]], writes=[hn_b[s]])
            p.op("act", lambda e: e.activation(out=junk[:], in_=hn[s][:], func=AF.Square, accum_out=ss[s][:, 0:1]),
                 reads=[hn_b[s]], writes=[junk_b, ss_b[s]])
            p.op("act", lambda e: e.activation(out=ss[s][:], in_=ss[s][:], func=AF.Sqrt, bias=RMS_EPS,
                                               scale=1.0 / 512.0), reads=[ss_b[s]], writes=[ss_b[s]])
            p.op("dve", lambda e: e.reciprocal(out=ss[s][:], in_=ss[s][:]), reads=[ss_b[s]], writes=[ss_b[s]])

        def P4(t):
            s, h, c = t["s"], t["h"], t["c"]
            cols = slice(c * 128, (c + 1) * 128)
            mh = h % 2
            p.op("dve", lambda e: e.scalar_tensor_tensor(
                out=hn[s][:], in0=hn[s][:], scalar=ss[s][:, 0:1], in1=gain[:, h * 512:(h + 1) * 512],
                op0=ALU.mult, op1=ALU.mult), reads=[hn_b[s], ss_b[s], setup_b], writes=[hn_b[s]])
            p.op("pool", lambda e: e.tensor_tensor(out=y2[s][:], in0=hn[s][:], in1=sig[s][:], op=ALU.mult),
                 reads=[hn_b[s], sig_b[s]], writes=[y2_b[s]])
            for blk in range(4):
                p.op("pe", lambda e, blk=blk: e.matmul(
                    P[7][:, blk * 128:(blk + 1) * 128], lhsT=y2[s][:, blk * 128:(blk + 1) * 128],
                    rhs=hc.cbf("ident"), start=True, stop=True),
                    reads=[y2_b[s], hc.b], writes=[B[7]], signal=(blk == 3))
            p.op("act", lambda e: e.activation(
                out=mTh[mh][:, :, cols], in_=P[7][:].rearrange("p (b j) -> p b j", b=4), func=AF.Copy),
                reads=[B[7]], writes=[mTh_b[mh]])
            if c == NCH - 1:
                for blk in range(4):
                    r0 = h * 512 + blk * 128
                    p.dma("sp", mixT_out[r0:r0 + 128, :], mTh[mh][:, blk, :], osem, reads=[mTh_b[mh]], writes=[mix_b])

        load_head(0)
        for i in range(n + 3):
            if i < n:
                P1(steps[i])
            if 0 <= i - 1 < n:
                P2(steps[i - 1])
            if 0 <= i - 2 < n:
                P3(steps[i - 2])
            if 0 <= i - 3 < n:
                P4(steps[i - 3])
            if i < n and steps[i]["c"] == 3 and steps[i]["h"] + 1 < nheads:
                load_head(steps[i]["h"] + 1)
        p.barrier()


def emit_sb(p, cx, hc, d, mixT_out, mix_b, nheads=12):
    SC = 128 ** -0.5
    NQ = SEQ // 512
    P, B = cx.ps, cx.psb
    with ExitStack() as st:
        NH = 3
        qT = [p.sbuf([128, SEQ], BF16, stack=st) for _ in range(NH)]
        kT = [p.sbuf([128, SEQ], BF16, stack=st) for _ in range(NH)]
        V = [p.sbuf([128, SEQ // 128, 128], BF16, stack=st) for _ in range(NH)]
        hd_b = [p.buf() for _ in range(NH)]
        hsem = [p.new_dma_sem() for _ in range(NH)]
        oT = [p.sbuf([128, SEQ], BF16, stack=st) for _ in range(2)]
        oT_b = [p.buf() for _ in range(2)]
        osem = p.new_dma_sem()
        NS = 4
        ex = [p.sbuf([128, 512], F32, stack=st) for _ in range(NS)]; ex_b = [p.buf() for _ in range(NS)]
        l_ = [p.sbuf([128, 512], F32, stack=st) for _ in range(NS)]; l_b = [p.buf() for _ in range(NS)]
        zl = [p.sbuf([128, 512], F32, stack=st) for _ in range(NS)]; zl_b = [p.buf() for _ in range(NS)]
        AT = [p.sbuf([128, 512], BF16, stack=st) for _ in range(NS)]; AT_b = [p.buf() for _ in range(NS)]
        R = [p.sbuf([128, 512], F32, stack=st) for _ in range(2)]; R_b = [p.buf() for _ in range(2)]
        zeros = p.sbuf([128, 128], BF16, stack=st); z_b = p.buf()
        p.op("pool", lambda e: e.memset(zeros[:], 0.0), writes=[z_b])

        steps = []
        k = 0
        for h in range(nheads):
            for Q in range(NQ):
                amax = 4 * Q + 3
                for a in range(amax, -1, -1):
                    c0 = max(0, 128 * a - 512 * Q)
                    steps.append(dict(k=k, h=h, hs=h % NH, Q=Q, a=a, c0=c0, diag=(a >= 4 * Q), first=(a == amax),
                                      last=(a == 0), s=k % NS, zb=k % 2, pb=2 + k % 2, ob=6 + (Q % 2),
                                      rq=(h * NQ + Q) % 2))
                    k += 1
        n = len(steps)

        def load_head(h):
            hs = h % NH
            r0 = h * 128
            p.dma("sp", qT[hs][:], d["qT"][r0:r0 + 128, :], hsem[hs], writes=[hd_b[hs]])
            p.dma("sp", kT[hs][:], d["kT"][r0:r0 + 128, :], hsem[hs], writes=[hd_b[hs]])
            p.dma("pool", V[hs][:], d["v_tm"][:, r0:r0 + 128].rearrange("(c p) d -> p c d", p=128),
                  hsem[hs], writes=[hd_b[hs]])

        def S1(t):
            s, zb, hs, a, c0 = t["s"], t["zb"], t["hs"], t["a"], t["c0"]
            cs = slice(c0, 512)
            qs = slice(512 * t["Q"] + c0, 512 * t["Q"] + 512)
            if t["first"]:
                ob, rq = t["ob"], t["rq"]
                p.op("pe", lambda e, ob=ob, hs=hs: e.matmul(P[ob][:], lhsT=zeros[:], rhs=qT[hs][:, 0:512],
                                                            start=True, stop=False),
                     reads=[z_b, hd_b[hs]], writes=[B[ob]], signal=False)
                p.op("pool", lambda e, rq=rq: e.memset(R[rq][:], 0.0), writes=[R_b[rq]])
            p.op("pe", lambda e: e.matmul(P[zb][:, cs], lhsT=kT[hs][:, a * 128:(a + 1) * 128], rhs=qT[hs][:, qs],
                                          start=True, stop=True), reads=[hd_b[hs]], writes=[B[zb]])
            p.op("act", lambda e: e.activation(out=ex[s][:, cs], in_=P[zb][:, cs], func=AF.Exp, scale=SC),
                 reads=[B[zb]], writes=[ex_b[s]])
            p.op("act", lambda e: e.activation(out=l_[s][:, cs], in_=ex[s][:, cs], func=AF.Ln, bias=1.0),
                 reads=[ex_b[s]], writes=[l_b[s]])
            if t["diag"]:
                p.op("pool", lambda e: e.tensor_tensor(out=l_[s][:, c0:c0 + 128], in0=l_[s][:, c0:c0 + 128],
                                                       in1=hc.c32("maskS"), op=ALU.mult),
                     reads=[l_b[s], hc.b], writes=[l_b[s]])

        def S2(t):
            s, zb, pb, c0, rq = t["s"], t["zb"], t["pb"], t["c0"], t["rq"]
            cs = slice(c0, 512)
            first = t["first"]
            p.op("dve", lambda e: e.scalar_tensor_tensor(out=zl[s][:, cs], in0=P[zb][:, cs], scalar=SC,
                                                         in1=l_[s][:, cs], op0=ALU.mult, op1=ALU.subtract),
                 reads=[B[zb], l_b[s]], writes=[zl_b[s]])
            p.op("pe", lambda e: e.matmul(P[pb][:, cs], lhsT=hc.c32("Tri"), rhs=l_[s][:, cs], start=True, stop=first),
                 reads=[hc.b, l_b[s]], writes=[B[pb]], signal=first)
            if not first:
                p.op("pe", lambda e: e.matmul(P[pb][:, cs], lhsT=hc.c32("ones"), rhs=R[rq][:, cs],
                                              start=False, stop=True), reads=[hc.b, R_b[rq]], writes=[B[pb]])
            if not t["last"]:
                p.op("pool", lambda e: e.tensor_tensor(out=R[rq][:, cs], in0=R[rq][:, cs], in1=l_[s][:, cs],
                                                       op=ALU.add), reads=[R_b[rq], l_b[s]], writes=[R_b[rq]])
            p.op("dve", lambda e: e.tensor_tensor(out=zl[s][:, cs], in0=zl[s][:, cs], in1=P[pb][:, cs],
                                                  op=ALU.subtract), reads=[zl_b[s], B[pb]], writes=[zl_b[s]])
            if t["diag"]:
                p.op("pool", lambda e: e.tensor_tensor(out=zl[s][:, c0:c0 + 128], in0=zl[s][:, c0:c0 + 128],
                                                       in1=hc.c32("negS"), op=ALU.add),
                     reads=[zl_b[s], hc.b], writes=[zl_b[s]])

        def S3(t):
            s, hs, a, c0, ob, Q, h = t["s"], t["hs"], t["a"], t["c0"], t["ob"], t["Q"], t["h"]
            cs = slice(c0, 512)
            last = t["last"]
            p.op("act", lambda e: e.activation(out=AT[s][:, cs], in_=zl[s][:, cs], func=AF.Exp),
                 reads=[zl_b[s]], writes=[AT_b[s]])
            p.op("pe", lambda e: e.matmul(P[ob][:, cs], lhsT=V[hs][:, a, :], rhs=AT[s][:, cs], start=False, stop=last),
                 reads=[hd_b[hs], AT_b[s]], writes=[B[ob]])
            if last:
                oh = h % 2
                p.op("act", lambda e: e.activation(out=oT[oh][:, Q * 512:(Q + 1) * 512], in_=P[ob][:], func=AF.Copy),
                     reads=[B[ob]], writes=[oT_b[oh]])
                if Q == NQ - 1:
                    r0 = h * 128
                    p.dma("sp", mixT_out[r0:r0 + 128, :], oT[oh][:], osem, reads=[oT_b[oh]], writes=[mix_b])

        for h0 in range(min(2, nheads)):
            load_head(h0)
        for i in range(n + 2):
            if i < n:
                S1(steps[i])
            if 0 <= i - 1 < n:
                S2(steps[i - 1])
            if 0 <= i - 2 < n:
                S3(steps[i - 2])
            if i < n and steps[i]["Q"] == 0 and steps[i]["a"] == 0 and steps[i]["h"] + 2 < nheads:
                load_head(steps[i]["h"] + 2)
        p.barrier()


FCN = D_FF // 128
A_FM, A_TM = 32, 16
B_FM, B_TM = 56, 6
N_NORMS = 2 + 3 * DEPTH
NTP = SEQ // NT


def build_fused():
    p = Prog()
    EI, EO = "ExternalInput", "ExternalOutput"
    consts = p.dram("consts", [128, len(CONST_NAMES), 128], F32, kind=EI)
    xT = p.dram("xT", [D_MODEL, SEQ], F32, kind=EI)
    memT = p.dram("memT", [D_MODEL, MEM_TOKENS], F32, kind=EI)
    g_d = p.dram("g", [128, N_NORMS * KC], F32, kind=EI)
    w_mk = p.dram("w_mk", [8, KC // KG, 128, KG, 128], F32, kind=EI)
    w_mv = p.dram("w_mv", [2, KC // KG, 128, KG, 512], F32, kind=EI)
    W = []
    for i in range(DEPTH):
        n_fm, n_tm = (A_FM, A_TM) if i % 2 == 0 else (B_FM, B_TM)
        W.append({
            "w1_in": p.dram(f"w1_in{i}", [FCN, KC // KG, 128, KG, 256], F32, kind=EI),
            "w1_out": p.dram(f"w1_out{i}", [KC, FCN // KG, 128, KG, 128], F32, kind=EI),
            "w_fm": p.dram(f"w_fm{i}", [n_fm, KC // KG, 128, KG, 128], F32, kind=EI),
            "w_tm": p.dram(f"w_tm{i}", [n_tm, KC // KG, 128, KG, 512], F32, kind=EI),
            "w_mo": p.dram(f"w_mo{i}", [KC, KC // KG, 128, KG, 128], F32, kind=EI),
            "w2_in": p.dram(f"w2_in{i}", [FCN, KC // KG, 128, KG, 256], F32, kind=EI),
            "w2_out": p.dram(f"w2_out{i}", [KC, FCN // KG, 128, KG, 128], F32, kind=EI),
        })
    gbs = [p.dram(f"gb{j}", [128, 12], F32, kind=EI) for j in range(2)]
    gains = [p.dram(f"gain{j}", [128, 3072], F32, kind=EI) for j in range(2)]
    outT = p.dram("outT", [D_MODEL, SEQ], F32, kind=EO)
    hs = [[p.dram(f"hs{tp}_{k}", [D_MODEL, NT], F32) for k in range(3)] for tp in range(NTP)]
    fmT = {"A": p.dram("fmT_A", [A_FM * 128, SEQ], BF16), "B": p.dram("fmT_B", [B_FM * 128, SEQ], BF16)}
    tm = {"A": p.dram("tm_A", [SEQ, A_TM * 512], F32), "B": p.dram("tm_B", [SEQ, B_TM * 512], F32)}
    mixT = p.dram("mixT", [D_MODEL, SEQ], BF16)
    mk_d = p.dram("mk_d", [128, 8 * MEM_TOKENS], BF16)
    mv_d = p.dram("mv_d", [128, 2 * 1024], BF16)

    cx = Ctx(p, consts)
    hc = HConsts(p, consts)
    g_sb = p.sbuf([128, N_NORMS * KC], F32)
    g_b = p.buf()
    p.dma("sp", g_sb[:], g_d[:, :], p.new_dma_sem(), writes=[g_b])

    with ExitStack() as st:
        mkT = p.sbuf([128, 8, MEM_TOKENS], BF16, stack=st)
        mv = p.sbuf([128, 2, 1024], BF16, stack=st)
        mkT_b, mv_b = p.buf(), p.buf()
        emit_memkv(p, cx, memT, g_sb, g_b, w_mk, w_mv, mkT, mkT_b, mv, mv_b, nmh=4, gcol=0)
        sem = p.new_dma_sem()
        p.dma("sp", mk_d[:, :], mkT[:].rearrange("p a b -> p (a b)"), sem, reads=[mkT_b])
        p.dma("sp", mv_d[:, :], mv[:].rearrange("p a b -> p (a b)"), sem, reads=[mv_b])
        p.barrier()

    def tsl(tp):
        return slice(tp * NT, (tp + 1) * NT)

    cur = [None] * NTP
    for i in range(DEPTH + 1):
        mx = ("A" if i % 2 == 0 else "B") if i < DEPTH else None
        for tp in range(NTP):
            if i == 0:
                src = xT[:, tsl(tp)]
                ci = 0
            else:
                ci = cur[tp]
                src = hs[tp][ci]
                a, b = (ci + 1) % 3, (ci + 2) % 3
                emit_mixout(p, cx, src, hs[tp][a], mixT[:, tsl(tp)], W[i - 1]["w_mo"], NT)
                emit_ffn(p, cx, hs[tp][a], hs[tp][b], g_sb, g_b, (3 * (i - 1) + 3) * KC,
                         W[i - 1]["w2_in"], W[i - 1]["w2_out"], NT)
                src = hs[tp][b]
            if i < DEPTH:
                emit_ffn(p, cx, src, hs[tp][ci], g_sb, g_b, (3 * i + 1) * KC, W[i]["w1_in"], W[i]["w1_out"], NT)
                cur[tp] = ci
                n_fm, n_tm = (A_FM, A_TM) if mx == "A" else (B_FM, B_TM)
                emit_inproj(p, cx, hs[tp][ci], g_sb, g_b, (3 * i + 2) * KC, W[i]["w_fm"], n_fm,
                            fmT[mx][:, tsl(tp)], W[i]["w_tm"], n_tm, tm[mx][tsl(tp), :], NT)
            else:
                emit_final_norm(p, cx, src, g_sb, g_b, (N_NORMS - 1) * KC, outT[:, tsl(tp)], NT)
        if i == DEPTH:
            break
        mix_b = p.buf()
        with ExitStack() as st:
            mkT = p.sbuf([128, 8, MEM_TOKENS], BF16, stack=st)
            mv = p.sbuf([128, 2, 1024], BF16, stack=st)
            mkT_b, mv_b = p.buf(), p.buf()
            sem = p.new_dma_sem()
            p.dma("sp", mkT[:].rearrange("p a b -> p (a b)"), mk_d[:, :], sem, writes=[mkT_b])
            p.dma("sp", mv[:].rearrange("p a b -> p (a b)"), mv_d[:, :], sem, writes=[mv_b])
            f, t = fmT[mx], tm[mx]
            if mx == "A":
                emit_memattn(p, cx, hc, f[3072:4096, :], mkT, mkT_b, mv, mv_b, mixT, 3072, mix_b, nmh=4)
                d = {"qT": f[0:1536, :], "kT": f[1536:3072, :], "k_tm": t[:, 0:1536], "v_tm": t[:, 1536:4608],
                     "o_tm": t[:, 4608:7680], "gates": t[:, 7680:7692], "gb": gbs[i // 2], "gain": gains[i // 2]}
                emit_mlstm(p, cx, hc, d, mixT, mix_b, nheads=6)
            else:
                emit_memattn(p, cx, hc, f[6144:7168, :], mkT, mkT_b, mv, mv_b, mixT, 3072, mix_b, nmh=4)
                d = {"qT": f[0:3072, :], "kT": f[3072:6144, :], "v_tm": t[:, 0:3072]}
                emit_sb(p, cx, hc, d, mixT, mix_b, nheads=24)
    return p.finish()


_PROG = {}


def _glay(*norms):
    return np.ascontiguousarray(np.concatenate([np.asarray(n, np.float32).reshape(KC, 128).T for n in norms], axis=1))


def _tile_ffn(w_in, w_out):
    wi = tile_w(w_in, 256, [[(j * 128, 128), (D_FF + j * 128, 128)] for j in range(FCN)])
    wo = tile_w(w_out, 128, [[(m * 128, 128)] for m in range(KC)])
    return wi, wo


def _tile_inproj(w, mixer):
    if mixer == "A":
        fm_cols = [(c, 128) for c in range(0, 3072, 128)] + [(9228 + c, 128) for c in range(0, 1024, 128)]
        tm_cols = [[(1536 + c, 512)] for c in range(0, 1536, 512)] + \
                  [[(3072 + c, 512)] for c in range(0, 6144, 512)] + [[(9216, 12)]]
    else:
        fm_cols = [(c, 128) for c in range(0, 6144, 128)] + [(9216 + c, 128) for c in range(0, 1024, 128)]
        tm_cols = [[(6144 + c, 512)] for c in range(0, 3072, 512)]
    return tile_w(w, 128, [[c] for c in fm_cols]), tile_w(w, 512, tm_cols)


def kernel(**inputs):
    f32 = np.float32
    x = np.asarray(inputs["x"], f32)
    mem = np.asarray(inputs["mem"], f32)
    shared = {"consts": host_consts()}
    norms = [inputs["mem_norm"]]
    for i in range(DEPTH):
        norms += [inputs["norm_ffn1"][i], inputs["norm_mix"][i], inputs["norm_ffn2"][i]]
    norms.append(inputs["norm_final"])
    shared["g"] = _glay(*[np.asarray(n, f32) for n in norms])
    wkv = np.asarray(inputs["w_mem_kv"], f32)
    shared["w_mk"] = tile_w(wkv[:, 0:1024], 128, [[(i * 128, 128)] for i in range(8)])
    shared["w_mv"] = tile_w(wkv[:, 1024:2048], 512, [[(0, 512)], [(512, 512)]])
    for i in range(DEPTH):
        shared[f"w1_in{i}"], shared[f"w1_out{i}"] = _tile_ffn(np.asarray(inputs["ffn1_w_in"][i], f32),
                                                              np.asarray(inputs["ffn1_w_out"][i], f32))
        shared[f"w2_in{i}"], shared[f"w2_out{i}"] = _tile_ffn(np.asarray(inputs["ffn2_w_in"][i], f32),
                                                              np.asarray(inputs["ffn2_w_out"][i], f32))
        w = np.asarray(inputs["a_w_in" if i % 2 == 0 else "b_w_in"][i // 2], f32)
        shared[f"w_fm{i}"], shared[f"w_tm{i}"] = _tile_inproj(w, "A" if i % 2 == 0 else "B")
        shared[f"w_mo{i}"] = tile_w(np.asarray(inputs["w_out"][i], f32), 128, [[(m * 128, 128)] for m in range(KC)])
    for j in range(2):
        gb = np.concatenate([np.asarray(inputs["a_b_igate"], f32)[j], np.asarray(inputs["a_b_fgate"], f32)[j]])
        shared[f"gb{j}"] = np.ascontiguousarray(np.broadcast_to(gb[None, :], (128, 12)))
        shared[f"gain{j}"] = np.ascontiguousarray(
            np.broadcast_to(np.asarray(inputs["a_head_gain"], f32)[j][None, :], (128, 3072)))
    maps = []
    for c in range(NCORES):
        b = c // 2
        m = dict(shared)
        m["xT"] = np.ascontiguousarray(x[b].T)
        m["memT"] = np.ascontiguousarray(mem[b].T)
        maps.append(m)
    if "nc" not in _PROG:
        _PROG["nc"] = build_fused()
    res = run_bass_kernel_spmd(_PROG["nc"], maps, core_ids=list(range(NCORES))).results
    del maps, shared
    out = np.empty((BATCH, SEQ, D_MODEL), f32)
    for b in range(BATCH):
        out[b, :NT] = res[2 * b]["outT"][:, :NT].T
        out[b, NT:] = res[2 * b + 1]["outT"][:, NT:].T
    return out
```
